# Optimizing a Trainium2 kernel written in Bass

```python
import math
import jax, jax.numpy as jnp
from jax import lax
import numpy as np

D_MODEL = 1024
BATCH = 4
SEQ = 8192
DEPTH = 4

GRID_W = 64
CTX_LEN = 256
N_MIXERS = 3
N_HEADS = 8
HEAD_DIM = D_MODEL // (2 * N_HEADS)
V_DIM = 2 * HEAD_DIM
Q_BLOCK = 128
ROPE_BASE = 10000.0
CONV_WIDTH = 3
GROUP_SIZE = 16
N_GROUPS = D_MODEL // GROUP_SIZE
STATE_DIM = 64
DT_MIN = 1e-3
DT_MAX = 1e-1
D_FF = (D_MODEL * 11) // 4
N_EXPERTS = 8
TOP_K = 2
EPS = 1e-6

kernel_name = 'hybrid_conv_diffattn_s5_moe_prefix_dit'


def rmsnorm(x, g):
    xf = x.astype(jnp.float32)
    y = xf * lax.rsqrt(jnp.mean(xf * xf, axis=-1, keepdims=True) + EPS)
    return (y * g.astype(jnp.float32)).astype(x.dtype)


def adaln(cond, w, b):
    m = (jax.nn.silu(cond) @ w + b).reshape(cond.shape[0], 1, 6, D_MODEL)
    return [m[:, :, j] for j in range(6)]


def modulate(h, g, shift, scale):
    return rmsnorm(h, g) * (1.0 + scale) + shift


def axial_rope_tables(n):
    rows = n // GRID_W
    row = jnp.repeat(jnp.arange(rows, dtype=jnp.float32), GRID_W)
    col = jnp.tile(jnp.arange(GRID_W, dtype=jnp.float32), rows)
    n_freq = HEAD_DIM // 4
    inv = ROPE_BASE ** (-jnp.arange(n_freq, dtype=jnp.float32) / n_freq)
    ang = jnp.concatenate([row[:, None] * inv, col[:, None] * inv], axis=-1)
    return jnp.cos(ang), jnp.sin(ang)


def _rotate(x, cos, sin):
    x1, x2 = jnp.split(x, 2, axis=-1)
    return jnp.concatenate([x1 * cos - x2 * sin, x2 * cos + x1 * sin], axis=-1)


def axial_rope(x, cos, sin):
    f = HEAD_DIM // 4
    c = cos[:, None, None, :]
    s = sin[:, None, None, :]
    xr, xc = jnp.split(x.astype(jnp.float32), 2, axis=-1)
    out = jnp.concatenate([_rotate(xr, c[..., :f], s[..., :f]), _rotate(xc, c[..., f:], s[..., f:])], axis=-1)
    return out.astype(x.dtype)


def short_conv_mixer(h, w_in, w_conv, w_out):
    b_gate, c_gate, v = jnp.split(h @ w_in, 3, axis=-1)
    z = c_gate * v
    L = z.shape[1]
    zp = jnp.pad(z, ((0, 0), (1, 1), (0, 0)))
    y = w_conv[0] * zp[:, :L] + w_conv[1] * zp[:, 1:L + 1] + w_conv[2] * zp[:, 2:]
    return (b_gate * y) @ w_out


def diff_softmax_attend(q, k, v, lam, lam_init, subln):
    s = jnp.einsum('bqhmd,bkhmd->bhmqk', q, k).astype(jnp.float32) * (HEAD_DIM ** -0.5)
    p = jax.nn.softmax(s, axis=-1)
    a = p[:, :, 0] - lam * p[:, :, 1]
    o = jnp.einsum('bhqk,bkhe->bqhe', a, v.astype(jnp.float32))
    o = rmsnorm(o, subln) * (1.0 - lam_init)
    return o.reshape(o.shape[0], o.shape[1], N_HEADS * V_DIM)


def diff_attention_mixer(hc, hl, w_qkv, lam_vecs, subln, w_o, cos, sin, lam_init, need_ctx):
    B, L, _ = hl.shape
    Lc = hc.shape[1]
    lv = lam_vecs.astype(jnp.float32)
    lam = jnp.exp(jnp.sum(lv[0] * lv[1])) - jnp.exp(jnp.sum(lv[2] * lv[3])) + lam_init
    q_l, k_l, v_l = jnp.split(hl @ w_qkv, 3, axis=-1)
    q_l = axial_rope(q_l.reshape(B, L, N_HEADS, 2, HEAD_DIM), cos, sin)
    k_l = axial_rope(k_l.reshape(B, L, N_HEADS, 2, HEAD_DIM), cos, sin)
    v_l = v_l.reshape(B, L, N_HEADS, V_DIM)
    k_c, v_c = jnp.split(hc @ w_qkv[:, D_MODEL:], 2, axis=-1)
    k_c = k_c.reshape(B, Lc, N_HEADS, 2, HEAD_DIM)
    v_c = v_c.reshape(B, Lc, N_HEADS, V_DIM)
    k_all = jnp.concatenate([k_c, k_l], axis=1)
    v_all = jnp.concatenate([v_c, v_l], axis=1)
    n_blocks = L // Q_BLOCK
    q_blocks = q_l.reshape(B, n_blocks, Q_BLOCK, N_HEADS, 2, HEAD_DIM).swapaxes(0, 1)
    o_blocks = lax.map(lambda qb: diff_softmax_attend(qb, k_all, v_all, lam, lam_init, subln), q_blocks)
    y_l = o_blocks.swapaxes(0, 1).reshape(B, L, N_HEADS * V_DIM).astype(hl.dtype) @ w_o
    y_c = None
    if need_ctx:
        q_c = (hc @ w_qkv[:, :D_MODEL]).reshape(B, Lc, N_HEADS, 2, HEAD_DIM)
        y_c = diff_softmax_attend(q_c, k_c, v_c, lam, lam_init, subln).astype(hc.dtype) @ w_o
    return y_c, y_l


def s5_discretise(a_re, a_im, log_dt, b_re, b_im):
    f32 = jnp.float32
    a_re, a_im = a_re.astype(f32), a_im.astype(f32)
    b_re, b_im = b_re.astype(f32), b_im.astype(f32)
    dt = jnp.exp(log_dt.astype(f32))[:, None]
    mag = jnp.exp(dt * a_re)
    ab_re = mag * jnp.cos(dt * a_im)
    ab_im = mag * jnp.sin(dt * a_im)
    den = a_re * a_re + a_im * a_im
    nr = ab_re - 1.0
    co_re = (nr * a_re + ab_im * a_im) / den
    co_im = (ab_im * a_re - nr * a_im) / den
    bb_re = co_re[..., None] * b_re - co_im[..., None] * b_im
    bb_im = co_re[..., None] * b_im + co_im[..., None] * b_re
    return ab_re, ab_im, bb_re, bb_im


def _linear_recurrence_combine(e1, e2):
    a1r, a1i, b1r, b1i = e1
    a2r, a2i, b2r, b2i = e2
    return (a2r * a1r - a2i * a1i, a2r * a1i + a2i * a1r,
            a2r * b1r - a2i * b1i + b2r, a2r * b1i + a2i * b1r + b2i)


def s5_scan(u, disc, h0):
    ab_re, ab_im, bb_re, bb_im = disc
    bu_re = jnp.einsum('blgs,gps->blgp', u, bb_re)
    bu_im = jnp.einsum('blgs,gps->blgp', u, bb_im)
    if h0 is not None:
        h_re, h_im = h0
        bu_re = bu_re.at[:, 0].add(ab_re * h_re - ab_im * h_im)
        bu_im = bu_im.at[:, 0].add(ab_re * h_im + ab_im * h_re)
    L = u.shape[1]
    a_re = jnp.broadcast_to(ab_re, (L,) + ab_re.shape)
    a_im = jnp.broadcast_to(ab_im, (L,) + ab_im.shape)
    def scan_one(br, bi):
        out = lax.associative_scan(_linear_recurrence_combine, (a_re, a_im, br, bi), axis=0)
        return out[2], out[3]
    return jax.vmap(scan_one)(bu_re, bu_im)


def s5_readout(h_re, h_im, c_re, c_im):
    return jnp.einsum('blgp,gsp->blgs', h_re, c_re) - jnp.einsum('blgp,gsp->blgs', h_im, c_im)


def _flip(t, rev):
    return t[:, ::-1] if rev else t


def s5_mixer(hc, hl, a_re, a_im, log_dt, b_re, b_im, c_re, c_im, d_skip, w_glu, need_ctx):
    f32 = jnp.float32
    B, L, _ = hl.shape
    Lc = hc.shape[1]
    u_c = hc.astype(f32).reshape(B, Lc, N_GROUPS, GROUP_SIZE)
    u_l = hl.astype(f32).reshape(B, L, N_GROUPS, GROUP_SIZE)
    d = d_skip.astype(f32)
    y_l = d * hl.astype(f32)
    y_c = d * hc.astype(f32) if need_ctx else None
    for direction in range(2):
        rev = direction == 1
        disc = s5_discretise(a_re[direction], a_im[direction], log_dt[direction], b_re[direction], b_im[direction])
        cr = c_re[direction].astype(f32)
        ci = c_im[direction].astype(f32)
        sc_re, sc_im = s5_scan(_flip(u_c, rev), disc, None)
        sl_re, sl_im = s5_scan(_flip(u_l, rev), disc, (sc_re[:, -1], sc_im[:, -1]))
        y_l = y_l + _flip(s5_readout(sl_re, sl_im, cr, ci), rev).reshape(B, L, D_MODEL)
        if need_ctx:
            y_c = y_c + _flip(s5_readout(sc_re, sc_im, cr, ci), rev).reshape(B, Lc, D_MODEL)
    def glu(y, dtype):
        val, gate = jnp.split(jax.nn.gelu(y).astype(dtype) @ w_glu, 2, axis=-1)
        return val * jax.nn.sigmoid(gate)
    y_c_out = glu(y_c, hc.dtype) if need_ctx else None
    return y_c_out, glu(y_l, hl.dtype)


def swiglu(h, w_gu, w_down):
    g, u = jnp.split(h @ w_gu, 2, axis=-1)
    return (jax.nn.silu(g) * u) @ w_down


def moe_swiglu(h, w_router, w_gu, w_down):
    f32 = jnp.float32
    logits = (h @ w_router).astype(f32)
    top_v, top_i = lax.top_k(logits, TOP_K)
    top_w = jax.nn.softmax(top_v, axis=-1)
    gates = jnp.sum(jax.nn.one_hot(top_i, N_EXPERTS, dtype=f32) * top_w[..., None], axis=-2)
    out = jnp.zeros(h.shape, f32)
    for e in range(N_EXPERTS):
        out = out + gates[..., e:e + 1] * swiglu(h, w_gu[e], w_down[e])
    return out.astype(h.dtype)


def setup_inputs(seed: int = 0) -> dict:
    key = jax.random.key(seed)
    keys = iter(jax.random.split(key, 16 * DEPTH + 8))
    f32 = jnp.float32
    D = D_MODEL
    def nrm(shape, scale=1.0):
        return jax.random.normal(next(keys), shape, f32) * scale
    def gain(n):
        return 1.0 + nrm((n,), 0.02)
    inp = {'x': nrm((BATCH, SEQ, D)), 'c': nrm((BATCH, D)),
           'ctx': nrm((BATCH, CTX_LEN, D)), 'c_ctx': nrm((D,))}
    for i in range(DEPTH):
        p = 'l%d_' % i
        inp[p + 'ada_w'] = nrm((D, 6 * D), 0.5 * D ** -0.5)
        inp[p + 'ada_b'] = nrm((6 * D,), 0.02)
        inp[p + 'norm_mix'] = gain(D)
        inp[p + 'norm_ffn'] = gain(D)
        kind = i % N_MIXERS
        if kind == 0:
            inp[p + 'conv_w_in'] = nrm((D, 3 * D), D ** -0.5)
            inp[p + 'conv_w'] = nrm((CONV_WIDTH, D), CONV_WIDTH ** -0.5)
            inp[p + 'conv_w_out'] = nrm((D, D), D ** -0.5)
        elif kind == 1:
            inp[p + 'attn_w_qkv'] = nrm((D, 3 * D), D ** -0.5)
            inp[p + 'attn_lam'] = nrm((4, HEAD_DIM), 0.1)
            inp[p + 'attn_subln'] = gain(V_DIM)
            inp[p + 'attn_w_o'] = nrm((D, D), D ** -0.5)
        else:
            inp[p + 'ssm_a_re'] = -0.5 + nrm((2, N_GROUPS, STATE_DIM), 0.01)
            inp[p + 'ssm_a_im'] = math.pi * jnp.arange(STATE_DIM, dtype=f32) + nrm((2, N_GROUPS, STATE_DIM), 0.01)
            inp[p + 'ssm_log_dt'] = jax.random.uniform(next(keys), (2, N_GROUPS), f32, math.log(DT_MIN), math.log(DT_MAX))
            inp[p + 'ssm_b_re'] = nrm((2, N_GROUPS, STATE_DIM, GROUP_SIZE), (2 * GROUP_SIZE) ** -0.5)
            inp[p + 'ssm_b_im'] = nrm((2, N_GROUPS, STATE_DIM, GROUP_SIZE), (2 * GROUP_SIZE) ** -0.5)
            inp[p + 'ssm_c_re'] = nrm((2, N_GROUPS, GROUP_SIZE, STATE_DIM), STATE_DIM ** -0.5)
            inp[p + 'ssm_c_im'] = nrm((2, N_GROUPS, GROUP_SIZE, STATE_DIM), STATE_DIM ** -0.5)
            inp[p + 'ssm_d'] = nrm((D,))
            inp[p + 'ssm_w_glu'] = nrm((D, 2 * D), D ** -0.5)
        if i % 2 == 0:
            inp[p + 'ffn_w_gu'] = nrm((D, 2 * D_FF), D ** -0.5)
            inp[p + 'ffn_w_down'] = nrm((D_FF, D), D_FF ** -0.5)
        else:
            inp[p + 'moe_router'] = nrm((D, N_EXPERTS), D ** -0.5)
            inp[p + 'moe_w_gu'] = nrm((N_EXPERTS, D, 2 * D_FF), D ** -0.5)
            inp[p + 'moe_w_down'] = nrm((N_EXPERTS, D_FF, D), D_FF ** -0.5)
    inp['final_norm'] = gain(D)
    return inp


def reference(x, c, ctx, c_ctx,
              l0_ada_w, l0_ada_b, l0_norm_mix, l0_norm_ffn, l0_conv_w_in, l0_conv_w, l0_conv_w_out, l0_ffn_w_gu, l0_ffn_w_down,
              l1_ada_w, l1_ada_b, l1_norm_mix, l1_norm_ffn, l1_attn_w_qkv, l1_attn_lam, l1_attn_subln, l1_attn_w_o, l1_moe_router, l1_moe_w_gu, l1_moe_w_down,
              l2_ada_w, l2_ada_b, l2_norm_mix, l2_norm_ffn, l2_ssm_a_re, l2_ssm_a_im, l2_ssm_log_dt, l2_ssm_b_re, l2_ssm_b_im, l2_ssm_c_re, l2_ssm_c_im, l2_ssm_d, l2_ssm_w_glu, l2_ffn_w_gu, l2_ffn_w_down,
              l3_ada_w, l3_ada_b, l3_norm_mix, l3_norm_ffn, l3_conv_w_in, l3_conv_w, l3_conv_w_out, l3_moe_router, l3_moe_w_gu, l3_moe_w_down,
              final_norm):
    layers = [
        dict(ada=(l0_ada_w, l0_ada_b), norms=(l0_norm_mix, l0_norm_ffn),
             mixer=(l0_conv_w_in, l0_conv_w, l0_conv_w_out), ffn=(l0_ffn_w_gu, l0_ffn_w_down)),
        dict(ada=(l1_ada_w, l1_ada_b), norms=(l1_norm_mix, l1_norm_ffn),
             mixer=(l1_attn_w_qkv, l1_attn_lam, l1_attn_subln, l1_attn_w_o), ffn=(l1_moe_router, l1_moe_w_gu, l1_moe_w_down)),
        dict(ada=(l2_ada_w, l2_ada_b), norms=(l2_norm_mix, l2_norm_ffn),
             mixer=(l2_ssm_a_re, l2_ssm_a_im, l2_ssm_log_dt, l2_ssm_b_re, l2_ssm_b_im, l2_ssm_c_re, l2_ssm_c_im, l2_ssm_d, l2_ssm_w_glu),
             ffn=(l2_ffn_w_gu, l2_ffn_w_down)),
        dict(ada=(l3_ada_w, l3_ada_b), norms=(l3_norm_mix, l3_norm_ffn),
             mixer=(l3_conv_w_in, l3_conv_w, l3_conv_w_out), ffn=(l3_moe_router, l3_moe_w_gu, l3_moe_w_down)),
    ]
    n = x.shape[1]
    n_ctx = ctx.shape[1]
    cos, sin = axial_rope_tables(n)
    kinds = [i % N_MIXERS for i in range(DEPTH)]
    h_ctx = ctx
    for i in range(DEPTH):
        p = layers[i]
        kind = kinds[i]
        reads_ctx = kind != 0
        ctx_next = any(k != 0 for k in kinds[i + 1:])
        use_ctx = reads_ctx or ctx_next
        norm_mix, norm_ffn = p['norms']
        sh1, sc1, g1, sh2, sc2, g2 = adaln(c, *p['ada'])
        hl = modulate(x, norm_mix, sh1, sc1)
        hc = None
        if use_ctx:
            csh1, csc1, cg1, csh2, csc2, cg2 = adaln(c_ctx[None], *p['ada'])
            hc = modulate(h_ctx, norm_mix, csh1, csc1)
        if kind == 0:
            y_l = short_conv_mixer(hl, *p['mixer'])
            y_c = short_conv_mixer(hc, *p['mixer']) if ctx_next else None
        elif kind == 1:
            y_c, y_l = diff_attention_mixer(hc, hl, *p['mixer'], cos=cos, sin=sin,
                                            lam_init=0.8 - 0.6 * math.exp(-0.3 * i), need_ctx=ctx_next)
        else:
            y_c, y_l = s5_mixer(hc, hl, *p['mixer'], need_ctx=ctx_next)
        x = x + g1 * y_l
        if ctx_next:
            h_ctx = h_ctx + cg1 * y_c
        ffn = swiglu if i % 2 == 0 else moe_swiglu
        hl2 = modulate(x, norm_ffn, sh2, sc2)
        if ctx_next:
            hc2 = modulate(h_ctx, norm_ffn, csh2, csc2)
            out = ffn(jnp.concatenate([hc2, hl2], axis=1), *p['ffn'])
            h_ctx = h_ctx + cg2 * out[:, :n_ctx]
            x = x + g2 * out[:, n_ctx:]
        else:
            x = x + g2 * ffn(hl2, *p['ffn'])
    return rmsnorm(x, final_norm)
```

```python
import contextlib
import numpy as np
import concourse.bass as bass
import concourse.mybir as mybir
from concourse.bass_utils import run_bass_kernel_spmd

F32 = mybir.dt.float32
BF16 = mybir.dt.bfloat16
I32 = mybir.dt.int32
AF = mybir.ActivationFunctionType
ALU = mybir.AluOpType
AX = mybir.AxisListType

ENGS = ["tensor", "vector", "scalar", "gpsimd", "sync"]


class Ev:
    __slots__ = ("sem", "val", "eng")

    def __init__(self, sem, val, eng):
        self.sem = sem
        self.val = val
        self.eng = eng


class Buf:
    __slots__ = ("name", "w", "r", "dsem", "dcnt")

    def __init__(self, name=""):
        self.name = name
        self.w = None
        self.r = []
        self.dsem = None
        self.dcnt = 0


class Shared:
    def __init__(self, nc):
        self.nc = nc
        self.es = contextlib.ExitStack()
        self.esem = {e: self.es.enter_context(nc.semaphore("s_" + e)) for e in ENGS}
        self.cnt = {e: 0 for e in ENGS}
        self.pool = []
        self.nstage = 0
        self.nsem = len(ENGS)

    def close(self):
        self.es.close()


class Prog:
    def __init__(self, nc, shared=None):
        self.nc = nc
        self.es = contextlib.ExitStack()
        self.own_shared = shared is None
        self.sh = shared if shared is not None else Shared(nc)
        self.q = {e: [] for e in ENGS}
        self.cnt = self.sh.cnt
        self.pending = {e: [] for e in ENGS}
        self.known = {e: {} for e in ENGS}
        self.esem = self.sh.esem
        self.ninst = 0
        self.dbufs = []
        self.pfx = "g%d_" % self.sh.nstage
        self.sh.nstage += 1

    @property
    def nsem(self):
        return self.sh.nsem

    def sbuf(self, name, shape, dtype):
        return self.es.enter_context(self.nc.sbuf_tensor(self.pfx + name, list(shape), dtype))

    def psum(self, name, shape, dtype=F32):
        return self.es.enter_context(self.nc.psum_tensor(self.pfx + name, list(shape), dtype))

    def newsem(self, buf):
        if self.sh.pool:
            h, c = self.sh.pool.pop()
        else:
            self.sh.nsem += 1
            h, c = self.sh.es.enter_context(self.nc.semaphore("d%d" % self.sh.nsem)), 0
        buf.dsem = h
        buf.dcnt = c
        self.dbufs.append(buf)

    @staticmethod
    def _flat(bs):
        out = []
        for b in bs:
            if isinstance(b, (list, tuple)):
                out.extend(Prog._flat(b))
            else:
                out.append(b)
        return out

    def _collect(self, eng, reads, writes):
        evs = []
        for b in reads:
            if b.w is not None:
                evs.append(b.w)
        for b in writes:
            if b.w is not None:
                evs.append(b.w)
            evs.extend(b.r)
        need = {}
        for ev in evs:
            if ev.eng == eng and eng == "tensor":
                continue
            if ev.val is None:
                raise RuntimeError("waiting on unclosed event (inc=False group not closed)")
            key = id(ev.sem)
            if self.known[eng].get(key, 0) >= ev.val:
                continue
            if key not in need or need[key][1] < ev.val:
                need[key] = (ev.sem, ev.val)
        waits = list(need.values())
        for s, v in waits:
            self.known[eng][id(s)] = v
        return waits

    def op(self, eng, fn, reads=(), writes=(), inc=True):
        reads = self._flat(reads)
        writes = self._flat(writes)
        waits = self._collect(eng, reads, writes)
        if inc:
            self.cnt[eng] += 1
            k = self.cnt[eng]
            ev = Ev(self.esem[eng], k, eng)
            for p in self.pending[eng]:
                p.val = k
            self.pending[eng] = []
        else:
            ev = Ev(self.esem[eng], None, eng)
            self.pending[eng].append(ev)
        sem = self.esem[eng]

        def thunk(e, waits=waits, fn=fn, inc=inc, sem=sem):
            for s, v in waits:
                e.wait_ge(s, v)
            ins = fn(e)
            if inc:
                ins.then_inc(sem, 1)
        self.q[eng].append(thunk)
        self.ninst += 1
        for b in writes:
            b.w = ev
            b.r = []
        for b in reads:
            b.r.append(ev)
            if len(b.r) > 64:
                b.r = self._prune(b.r)
        return ev

    def _prune(self, evs):
        best = {}
        for ev in evs:
            if ev.val is None:
                best[id(ev)] = ev
                continue
            key = id(ev.sem)
            if key not in best or best[key].val < ev.val:
                best[key] = ev
        return list(best.values())

    def dma(self, eng, out_ap, in_ap, reads=(), writes=(), key=None, fn=None):
        if key is None:
            key = writes[0] if writes else reads[0]
        reads = self._flat(reads)
        writes = self._flat(writes)
        if key.dsem is None:
            self.newsem(key)
        waits = self._collect(eng, reads, writes)
        key.dcnt += 16
        ev = Ev(key.dsem, key.dcnt, "dma")
        sem = key.dsem
        if fn is None:
            fn = lambda e: e.dma_start(out=out_ap, in_=in_ap)

        def thunk(e, waits=waits, sem=sem, fn=fn):
            for s, v in waits:
                e.wait_ge(s, v)
            fn(e).then_inc(sem, 16)
        self.q[eng].append(thunk)
        self.ninst += 1
        for b in writes:
            b.w = ev
            b.r = []
        for b in reads:
            b.r.append(ev)
        return ev

    def wait_all(self, eng, evs):
        need = {}
        for ev in evs:
            key = id(ev.sem)
            if key not in need or need[key][1] < ev.val:
                need[key] = (ev.sem, ev.val)
        waits = list(need.values())

        def thunk(e, waits=waits):
            for s, v in waits:
                e.wait_ge(s, v)
        self.q[eng].append(thunk)

    def barrier(self):
        waits = [(self.esem[e], self.cnt[e]) for e in ENGS if self.cnt[e] > 0]
        for b in self.dbufs:
            waits.append((b.dsem, b.dcnt))
        for eng in ENGS:
            def thunk(e, waits=waits):
                for s, v in waits:
                    e.wait_ge(s, v)
            self.q[eng].append(thunk)
        for b in self.dbufs:
            self.sh.pool.append((b.dsem, b.dcnt))
        self.dbufs = []

    def emit(self, final=True):
        nc = self.nc
        with nc.Block() as block:
            @block.tensor
            def _(e):
                for t in self.q["tensor"]:
                    t(e)

            @block.vector
            def _(e):
                for t in self.q["vector"]:
                    t(e)

            @block.scalar
            def _(e):
                for t in self.q["scalar"]:
                    t(e)

            @block.gpsimd
            def _(e):
                for t in self.q["gpsimd"]:
                    t(e)

            @block.sync
            def _(e):
                for t in self.q["sync"]:
                    t(e)
        self.es.close()
        if self.own_shared and final:
            self.sh.close()


D = 1024
KC = 8
DFF = 2816
FC = 22
EPS = 1e-6


class LB:
    def __init__(self, P, nw=5, wslot=4096):
        self.P = P
        self.nw = nw
        self.ps = [(P.psum("ps%d" % i, [128, 1024]), Buf("ps%d" % i)) for i in range(4)]
        self.psi = 0
        self.wr = [(P.sbuf("w%d" % i, [128, wslot], BF16), Buf("w%d" % i)) for i in range(nw)]
        self.wi = 0
        self.ones = P.sbuf("ones", [128, 128], BF16)
        self.bones = Buf("ones")
        P.op("vector", lambda e: e.memset(self.ones[:], 1.0), writes=[self.bones])
        self.dq = 0

    def next_ps(self):
        r = self.ps[self.psi % 4]
        self.psi += 1
        return r

    def load_w(self, W, k0, kch, c0, ncols):
        P = self.P
        t, b = self.wr[self.wi % self.nw]
        self.wi += 1
        view = t[:, 0:kch * ncols].rearrange("p (k n) -> p k n", k=kch)
        src = W[k0 * 128:(k0 + kch) * 128, c0:c0 + ncols].rearrange("(k p) n -> p k n", p=128)
        P.dma("sync" if W.dtype == BF16 else "gpsimd", view, src, writes=[b])
        return view, b

    def mm(self, out_ap, bout, pairs):
        P = self.P
        n = len(pairs)
        for i, (l, r, bufs) in enumerate(pairs):
            P.op("tensor",
                 lambda e, l=l, r=r, i=i: e.matmul(out_ap, lhsT=l, rhs=r, start=(i == 0), stop=(i == n - 1)),
                 reads=bufs, writes=[bout], inc=(i == n - 1))

    def mm_wide(self, ps, bps, W, pairs_fn):
        c0 = 0
        while c0 < W:
            c1 = min(W, c0 + 512)
            self.mm(ps[:, c0:c1], bps, pairs_fn(c0, c1))
            c0 = c1

    def setup_ada(self, condT, ada_w, ada_bT, norm1T, norm2T, ncond=2):
        P = self.P
        cs = P.sbuf("ada_cs", [128, 8, ncond], F32); bcs = Buf("ada_cs")
        csb = P.sbuf("ada_csb", [128, 8, ncond], BF16); bcsb = Buf("ada_csb")
        adab = P.sbuf("ada_b", [128, 48], F32); badab = Buf("ada_b")
        n1 = P.sbuf("n1", [128, 8], F32); bn1 = Buf("n1")
        n2 = P.sbuf("n2", [128, 8], F32); bn2 = Buf("n2")
        P.dma("sync", cs[:], condT.rearrange("(k p) c -> p k c", p=128), writes=[bcs])
        P.dma("sync", adab[:], ada_bT, writes=[badab])
        P.dma("sync", n1[:], norm1T, writes=[bn1])
        P.dma("sync", n2[:], norm2T, writes=[bn2])
        P.op("scalar", lambda e: e.activation(out=csb[:], in_=cs[:], func=AF.Silu), reads=[bcs], writes=[bcsb])
        psA, bpsA = self.next_ps()
        for cg in range(12):
            w, bw = self.load_w(ada_w, 0, 8, cg * 512, 512)
            for m in range(4):
                j = cg * 4 + m
                self.mm(psA[:, j * ncond:(j + 1) * ncond], bpsA,
                        [(w[:, k, m * 128:(m + 1) * 128], csb[:, k, :], [bw, bcsb]) for k in range(8)])
        self.mod = []
        self.gs1 = []
        self.gs2 = []
        self.bmod = Buf("mod")
        psv = psA[:, 0:48 * ncond].rearrange("p (j c) -> p j c", c=ncond)
        for c in range(ncond):
            mod = P.sbuf("mod%d" % c, [128, 48], F32)
            g1 = P.sbuf("gs1_%d" % c, [128, 8], F32)
            g2 = P.sbuf("gs2_%d" % c, [128, 8], F32)
            P.op("vector", lambda e, mod=mod, c=c: e.tensor_tensor(out=mod[:], in0=psv[:, :, c], in1=adab[:], op=ALU.add),
                 reads=[bpsA, badab], writes=[self.bmod])
            P.op("vector", lambda e, mod=mod, g1=g1: e.scalar_tensor_tensor(out=g1[:], in0=mod[:, 8:16], scalar=1.0, in1=n1[:], op0=ALU.add, op1=ALU.mult),
                 reads=[self.bmod, bn1], writes=[self.bmod])
            P.op("vector", lambda e, mod=mod, g2=g2: e.scalar_tensor_tensor(out=g2[:], in0=mod[:, 32:40], scalar=1.0, in1=n2[:], op0=ALU.add, op1=ALU.mult),
                 reads=[self.bmod, bn2], writes=[self.bmod])
            self.mod.append(mod)
            self.gs1.append(g1)
            self.gs2.append(g2)

    def alloc_common(self, WMAX=514):
        P = self.P
        self.WMAX = WMAX
        self.sq = P.sbuf("sq", [128, 8, WMAX], BF16); self.bsq = Buf("sq")
        self.rs = P.sbuf("rs", [128, WMAX], F32); self.brs = Buf("rs")
        self.tmp = [(P.sbuf("tmp%d" % i, [128, WMAX], F32), Buf("tmp%d" % i)) for i in range(2)]
        self.ti = 0
        self.hl = P.sbuf("hl", [128, 8, WMAX], BF16); self.bhl = Buf("hl")

    def alloc_scr(self, n):
        if not hasattr(self, "scr"):
            self.scr = []
        while len(self.scr) < n:
            i = len(self.scr)
            self.scr.append((self.P.sbuf("scr%d" % i, [128, self.WMAX], F32), Buf("scr%d" % i)))

    def next_tmp(self):
        r = self.tmp[self.ti % 2]
        self.ti += 1
        return r

    def rstd(self, xin, bxin, W):
        P = self.P
        sq, bsq, rs, brs = self.sq, self.bsq, self.rs, self.brs
        for k in range(8):
            P.op("scalar", lambda e, k=k: e.activation(out=sq[:, k, 0:W], in_=xin(k), func=AF.Square),
                 reads=[bxin], writes=[bsq])
        ps, bps = self.next_ps()
        self.mm_wide(ps, bps, W, lambda c0, c1: [(self.ones[:], sq[:, k, c0:c1], [self.bones, bsq]) for k in range(8)])
        P.op("vector", lambda e: e.tensor_scalar(out=rs[:, 0:W], in0=ps[:, 0:W], scalar1=1.0 / D, scalar2=EPS, op0=ALU.mult, op1=ALU.add),
             reads=[bps], writes=[brs])
        P.op("scalar", lambda e: e.activation(out=rs[:, 0:W], in_=rs[:, 0:W], func=AF.Sqrt), reads=[brs], writes=[brs])
        P.op("vector", lambda e: e.reciprocal(out=rs[:, 0:W], in_=rs[:, 0:W]), reads=[brs], writes=[brs])

    def norm_mod(self, xin, bxin, W, gs, shift, out=None, bout=None, out32=None, bout32=None):
        P = self.P
        rs, brs = self.rs, self.brs
        if out is None:
            out, bout = self.hl, self.bhl
        self.rstd(xin, bxin, W)
        for k in range(8):
            tmp, btmp = self.next_tmp()
            P.op("vector", lambda e, k=k, tmp=tmp: e.scalar_tensor_tensor(out=tmp[:, 0:W], in0=xin(k), scalar=gs[:, k:k + 1], in1=rs[:, 0:W], op0=ALU.mult, op1=ALU.mult),
                 reads=[bxin, brs, self.bmod], writes=[btmp])
            if out32 is None:
                P.op("scalar", lambda e, k=k, tmp=tmp: e.activation(out=out[:, k, 0:W], in_=tmp[:, 0:W], func=AF.Identity, bias=shift[:, k:k + 1], scale=1.0),
                     reads=[btmp, self.bmod], writes=[bout])
            else:
                P.op("scalar", lambda e, k=k, tmp=tmp: e.activation(out=out32[:, k, 0:W], in_=tmp[:, 0:W], func=AF.Identity, bias=shift[:, k:k + 1], scale=1.0),
                     reads=[btmp, self.bmod], writes=[bout32])
                P.op("vector", lambda e, k=k: e.tensor_copy(out=out[:, k, 0:W], in_=out32[:, k, 0:W]), reads=[bout32], writes=[bout])

    def final_norm(self, x1, bx1, N, fn, bfn, xo, bxo):
        P = self.P
        rs, brs = self.rs, self.brs
        self.rstd(lambda k: x1[:, k, 0:N], bx1, N)
        for k in range(8):
            P.op("vector", lambda e, k=k: e.scalar_tensor_tensor(out=xo[:, k, 0:N], in0=x1[:, k, 0:N], scalar=fn[:, k:k + 1], in1=rs[:, 0:N], op0=ALU.mult, op1=ALU.mult),
                 reads=[bx1, brs, bfn], writes=[bxo])

    def alloc_conv(self):
        P = self.P
        W = self.WMAX
        self.alloc_scr(6)
        self.zc = self.scr[0:2]
        self.z = self.scr[2:4]
        self.yc = self.scr[4:6]
        self.tt = P.sbuf("tt", [128, 8, 512], BF16); self.btt = Buf("tt")
        self.cw = P.sbuf("cw", [128, 3, 8], F32); self.bcw = Buf("cw")
        self.ci = 0

    def load_conv_w(self, conv_wT):
        self.P.dma("sync", self.cw[:], conv_wT, writes=[self.bcw])

    def conv_mixer(self, xs, bxs, N, c, w_in, w_out, maskL, maskR, bmask, x1, bx1):
        P = self.P
        W = N + 2
        mod = self.mod[c]
        self.norm_mod(lambda k: xs[:, k, 0:W], bxs, W, self.gs1[c], mod[:, 0:8])
        hl, bhl = self.hl, self.bhl
        tt, btt = self.tt, self.btt
        for q in range(2):
            wc, bwc = self.load_w(w_in, 0, 8, 1024 + q * 512, 512)
            wv, bwv = self.load_w(w_in, 0, 8, 2048 + q * 512, 512)
            wb, bwb = self.load_w(w_in, 0, 8, q * 512, 512)
            for m in range(4):
                k2 = q * 4 + m
                psC, bpsC = self.next_ps()
                self.mm_wide(psC, bpsC, W, lambda c0, c1: [(wc[:, k, m * 128:(m + 1) * 128], hl[:, k, c0:c1], [bwc, bhl]) for k in range(8)])
                psV, bpsV = self.next_ps()
                self.mm_wide(psV, bpsV, W, lambda c0, c1: [(wv[:, k, m * 128:(m + 1) * 128], hl[:, k, c0:c1], [bwv, bhl]) for k in range(8)])
                zc, bzc = self.zc[self.ci % 2]
                z, bz = self.z[self.ci % 2]
                yc, byc = self.yc[self.ci % 2]
                self.ci += 1
                P.op("scalar", lambda e, zc=zc, psC=psC: e.activation(out=zc[:, 0:W], in_=psC[:, 0:W], func=AF.Copy), reads=[bpsC], writes=[bzc])
                P.op("vector", lambda e, z=z, zc=zc, psV=psV: e.tensor_tensor(out=z[:, 0:W], in0=zc[:, 0:W], in1=psV[:, 0:W], op=ALU.mult), reads=[bzc, bpsV], writes=[bz])
                P.op("vector", lambda e, z=z: e.tensor_scalar(out=z[:, 0:1], in0=z[:, 0:1], scalar1=maskL, scalar2=None, op0=ALU.mult), reads=[bz, bmask], writes=[bz])
                P.op("vector", lambda e, z=z: e.tensor_scalar(out=z[:, W - 1:W], in0=z[:, W - 1:W], scalar1=maskR, scalar2=None, op0=ALU.mult), reads=[bz, bmask], writes=[bz])
                cw = self.cw
                P.op("vector", lambda e, z=z, yc=yc, k2=k2: e.tensor_scalar(out=yc[:, 0:N], in0=z[:, 0:N], scalar1=cw[:, 0, k2:k2 + 1], scalar2=None, op0=ALU.mult), reads=[bz, self.bcw], writes=[byc])
                P.op("vector", lambda e, z=z, yc=yc, k2=k2: e.scalar_tensor_tensor(out=yc[:, 0:N], in0=z[:, 1:N + 1], scalar=cw[:, 1, k2:k2 + 1], in1=yc[:, 0:N], op0=ALU.mult, op1=ALU.add), reads=[bz, byc, self.bcw], writes=[byc])
                P.op("vector", lambda e, z=z, yc=yc, k2=k2: e.scalar_tensor_tensor(out=yc[:, 0:N], in0=z[:, 2:N + 2], scalar=cw[:, 2, k2:k2 + 1], in1=yc[:, 0:N], op0=ALU.mult, op1=ALU.add), reads=[bz, byc, self.bcw], writes=[byc])
                psB, bpsB = self.next_ps()
                self.mm(psB[:, 0:N], bpsB, [(wb[:, k, m * 128:(m + 1) * 128], hl[:, k, 1:N + 1], [bwb, bhl]) for k in range(8)])
                P.op("vector", lambda e, yc=yc, psB=psB, k2=k2: e.tensor_tensor(out=tt[:, k2, 0:N], in0=yc[:, 0:N], in1=psB[:, 0:N], op=ALU.mult), reads=[byc, bpsB], writes=[btt])
        for q in range(2):
            wo, bwo = self.load_w(w_out, 0, 8, q * 512, 512)
            for m in range(4):
                k3 = q * 4 + m
                ps, bps = self.next_ps()
                self.mm(ps[:, 0:N], bps, [(wo[:, k, m * 128:(m + 1) * 128], tt[:, k, 0:N], [bwo, btt]) for k in range(8)])
                P.op("vector", lambda e, ps=ps, k3=k3: e.scalar_tensor_tensor(out=x1[:, k3, 0:N], in0=ps[:, 0:N], scalar=mod[:, 16 + k3:17 + k3], in1=xs[:, k3, 1:N + 1], op0=ALU.mult, op1=ALU.add),
                     reads=[bps, bxs, self.bmod], writes=[bx1])

    def alloc_ffn(self):
        P = self.P
        self.sg = [(P.sbuf("sg%d" % i, [128, 512], F32), Buf("sg%d" % i)) for i in range(2)]
        self.sgi = 0
        self.act = P.sbuf("act", [128, FC, 512], BF16); self.bact = Buf("act")

    def ffn_dense(self, x1, bx1, N, c, w_gu, w_down, x2, bx2):
        P = self.P
        mod = self.mod[c]
        self.norm_mod(lambda k: x1[:, k, 0:N], bx1, N, self.gs2[c], mod[:, 24:32])
        hl, bhl = self.hl, self.bhl
        act, bact = self.act, self.bact
        for jq in range(6):
            nch = 4 if jq < 5 else 2
            wg, bwg = self.load_w(w_gu, 0, 8, jq * 512, nch * 128)
            wu, bwu = self.load_w(w_gu, 0, 8, DFF + jq * 512, nch * 128)
            for m in range(nch):
                j = jq * 4 + m
                psG, bpsG = self.next_ps()
                self.mm(psG[:, 0:N], bpsG, [(wg[:, k, m * 128:(m + 1) * 128], hl[:, k, 0:N], [bwg, bhl]) for k in range(8)])
                psU, bpsU = self.next_ps()
                self.mm(psU[:, 0:N], bpsU, [(wu[:, k, m * 128:(m + 1) * 128], hl[:, k, 0:N], [bwu, bhl]) for k in range(8)])
                sg, bsg = self.sg[self.sgi % 2]
                self.sgi += 1
                P.op("scalar", lambda e, sg=sg, psG=psG: e.activation(out=sg[:, 0:N], in_=psG[:, 0:N], func=AF.Silu), reads=[bpsG], writes=[bsg])
                P.op("vector", lambda e, sg=sg, psU=psU, j=j: e.tensor_tensor(out=act[:, j, 0:N], in0=sg[:, 0:N], in1=psU[:, 0:N], op=ALU.mult), reads=[bsg, bpsU], writes=[bact])
        for dp in range(4):
            wa, bwa = self.load_w(w_down, 0, 11, dp * 256, 256)
            wb, bwb = self.load_w(w_down, 11, 11, dp * 256, 256)
            for h in range(2):
                d = dp * 2 + h
                ps, bps = self.next_ps()
                pairs = [(wa[:, f, h * 128:(h + 1) * 128], act[:, f, 0:N], [bwa, bact]) for f in range(11)]
                pairs += [(wb[:, f, h * 128:(h + 1) * 128], act[:, 11 + f, 0:N], [bwb, bact]) for f in range(11)]
                self.mm(ps[:, 0:N], bps, pairs)
                P.op("vector", lambda e, ps=ps, d=d: e.scalar_tensor_tensor(out=x2[:, d, 0:N], in0=ps[:, 0:N], scalar=mod[:, 40 + d:41 + d], in1=x1[:, d, 0:N], op0=ALU.mult, op1=ALU.add),
                     reads=[bps, bx1, self.bmod], writes=[bx2])

    def alloc_moe(self, router_dram):
        P = self.P
        self.alloc_scr(20)
        self.wr32 = P.sbuf("wr32", [128, 8, 8], F32); self.bwr32 = Buf("wr32")
        P.dma("sync", self.wr32[:], router_dram.rearrange("(k p) e -> p k e", p=128), writes=[self.bwr32])

    def ffn_moe(self, x1, bx1, N, c, w_gu, w_down, hl2f, bhl2f):
        P = self.P
        mod = self.mod[c]
        self.norm_mod(lambda k: x1[:, k, 0:N], bx1, N, self.gs2[c], mod[:, 24:32], out32=hl2f, bout32=bhl2f)
        hl, bhl = self.hl, self.bhl
        act, bact = self.act, self.bact
        wr32, bwr32 = self.wr32, self.bwr32
        Ls = []
        for e_ in range(8):
            ps, bps = self.ps[e_ // 2]
            o = (e_ % 2) * 512
            self.mm(ps[:, o:o + N], bps, [(wr32[:, k, e_:e_ + 1].to_broadcast([128, 128]), hl2f[:, k, 0:N], [bwr32, bhl2f]) for k in range(8)])
            Ls.append((ps[:, o:o + N], bps))
        self.psi = 0
        G = self.scr[0:8]
        LM = self.scr[8:16]
        m1, bm1 = self.scr[16]
        m2, bm2 = self.scr[17]
        w1, bw1 = self.scr[18]
        w2, bw2 = self.scr[19]
        TT = lambda o, a, b, op, r, w: P.op("vector", lambda e: e.tensor_tensor(out=o, in0=a, in1=b, op=op), reads=r, writes=w)
        P.op("scalar", lambda e: e.activation(out=m1[:, 0:N], in_=Ls[0][0], func=AF.Copy), reads=[Ls[0][1]], writes=[bm1])
        for e_ in range(1, 8):
            TT(m1[:, 0:N], m1[:, 0:N], Ls[e_][0], ALU.max, [bm1, Ls[e_][1]], [bm1])
        for e_ in range(8):
            g, bg = G[e_]
            lm, blm = LM[e_]
            TT(g[:, 0:N], Ls[e_][0], m1[:, 0:N], ALU.is_ge, [Ls[e_][1], bm1], [bg])
            P.op("vector", lambda e, g=g, lm=lm, e_=e_: e.scalar_tensor_tensor(out=lm[:, 0:N], in0=g[:, 0:N], scalar=-1e30, in1=Ls[e_][0], op0=ALU.mult, op1=ALU.add),
                 reads=[bg, Ls[e_][1]], writes=[blm])
        P.op("vector", lambda e: e.tensor_copy(out=m2[:, 0:N], in_=LM[0][0][:, 0:N]), reads=[LM[0][1]], writes=[bm2])
        for e_ in range(1, 8):
            TT(m2[:, 0:N], m2[:, 0:N], LM[e_][0][:, 0:N], ALU.max, [bm2, LM[e_][1]], [bm2])
        for e_ in range(8):
            lm, blm = LM[e_]
            TT(lm[:, 0:N], lm[:, 0:N], m2[:, 0:N], ALU.is_ge, [blm, bm2], [blm])
        TT(w1[:, 0:N], m1[:, 0:N], m2[:, 0:N], ALU.subtract, [bm1, bm2], [bw1])
        P.op("scalar", lambda e: e.activation(out=w1[:, 0:N], in_=w1[:, 0:N], func=AF.Sigmoid), reads=[bw1], writes=[bw1])
        P.op("vector", lambda e: e.tensor_scalar(out=w2[:, 0:N], in0=w1[:, 0:N], scalar1=-1.0, scalar2=1.0, op0=ALU.mult, op1=ALU.add), reads=[bw1], writes=[bw2])
        for e_ in range(8):
            g, bg = G[e_]
            lm, blm = LM[e_]
            TT(g[:, 0:N], g[:, 0:N], w1[:, 0:N], ALU.mult, [bg, bw1], [bg])
            TT(lm[:, 0:N], lm[:, 0:N], w2[:, 0:N], ALU.mult, [blm, bw2], [blm])
            TT(g[:, 0:N], g[:, 0:N], lm[:, 0:N], ALU.add, [bg, blm], [bg])
        for e_ in range(8):
            g, bg = G[e_]
            wgu = w_gu[e_]
            wdn = w_down[e_]
            for jq in range(6):
                nch = 4 if jq < 5 else 2
                wg, bwg = self.load_w(wgu, 0, 8, jq * 512, nch * 128)
                wu, bwu = self.load_w(wgu, 0, 8, DFF + jq * 512, nch * 128)
                for m in range(nch):
                    j = jq * 4 + m
                    psG, bpsG = self.next_ps()
                    self.mm(psG[:, 0:N], bpsG, [(wg[:, k, m * 128:(m + 1) * 128], hl[:, k, 0:N], [bwg, bhl]) for k in range(8)])
                    psU, bpsU = self.next_ps()
                    self.mm(psU[:, 0:N], bpsU, [(wu[:, k, m * 128:(m + 1) * 128], hl[:, k, 0:N], [bwu, bhl]) for k in range(8)])
                    sg, bsg = self.sg[self.sgi % 2]
                    self.sgi += 1
                    P.op("scalar", lambda e, sg=sg, psG=psG: e.activation(out=sg[:, 0:N], in_=psG[:, 0:N], func=AF.Silu), reads=[bpsG], writes=[bsg])
                    TT(sg[:, 0:N], sg[:, 0:N], g[:, 0:N], ALU.mult, [bsg, bg], [bsg])
                    TT(act[:, j, 0:N], sg[:, 0:N], psU[:, 0:N], ALU.mult, [bsg, bpsU], [bact])
            for dp in range(4):
                wa, bwa = self.load_w(wdn, 0, 11, dp * 256, 256)
                wb, bwb = self.load_w(wdn, 11, 11, dp * 256, 256)
                for h in range(2):
                    d = dp * 2 + h
                    ps, bps = self.next_ps()
                    pairs = [(wa[:, f, h * 128:(h + 1) * 128], act[:, f, 0:N], [bwa, bact]) for f in range(11)]
                    pairs += [(wb[:, f, h * 128:(h + 1) * 128], act[:, 11 + f, 0:N], [bwb, bact]) for f in range(11)]
                    self.mm(ps[:, 0:N], bps, pairs)
                    P.op("vector", lambda e, ps=ps, d=d: e.scalar_tensor_tensor(out=x1[:, d, 0:N], in0=ps[:, 0:N], scalar=mod[:, 40 + d:41 + d], in1=x1[:, d, 0:N], op0=ALU.mult, op1=ALU.add),
                         reads=[bps, bx1, self.bmod], writes=[bx1])


NT = 4096
NCTX = 256


class Fuse:
    def __init__(self, nc, sh, io):
        self.nc = nc
        self.sh = sh
        self.io = io


def _ctx(F):
    if F is None:
        nc = bass.Bass("TRN2", target_bir_lowering=False)
        dt = lambda n, s, k="ExternalInput", d=F32: nc.dram_tensor(n, list(s), d, kind=k).ap()
        return nc, None, dt

    def dt(n, s, k="ExternalInput", d=F32):
        ap = F.io[n]
        assert list(ap.shape) == list(s), (n, list(ap.shape), list(s))
        return ap
    return F.nc, F.sh, dt


def _finish(P, evs, F):
    if F is None:
        P.wait_all("sync", evs)
        P.emit()
    else:
        P.barrier()
        P.emit(final=False)


def build_conv_layer(moe=False, with_ctx=True, final_norm=False, ntiles=None, F=None):
    nc, sh, dt = _ctx(F)
    if ntiles is None:
        ntiles = NT // 512
    xT = dt("xT", [D, NT + 2])
    condT = dt("condT", [D, 2])
    ada_w = dt("ada_w", [D, 6 * D])
    ada_bT = dt("ada_bT", [128, 48])
    n1T = dt("n1T", [128, 8])
    n2T = dt("n2T", [128, 8])
    conv_wT = dt("conv_wT", [128, 3, 8])
    w_in = dt("w_in", [D, 3 * D])
    w_out = dt("w_out", [D, D])
    mask = dt("mask", [128, 4])
    if with_ctx:
        cT = dt("cT", [D, NCTX + 2])
        coT = dt("coT", [D, NCTX], "ExternalOutput")
    if moe:
        w_gu = dt("w_gu", [8, D, 2 * DFF])
        w_down = dt("w_down", [8, DFF, D])
        router = dt("router", [D, 8])
    else:
        w_gu = dt("w_gu", [D, 2 * DFF])
        w_down = dt("w_down", [DFF, D])
    oT = dt("oT", [D, NT], "ExternalOutput")

    P = Prog(nc, sh)
    L = LB(P, nw=4)
    L.alloc_common(514)
    L.alloc_conv()
    L.alloc_ffn()
    msk = P.sbuf("msk", [128, 4], F32); bmsk = Buf("msk")
    P.dma("sync", msk[:], mask, writes=[bmsk])
    L.load_conv_w(conv_wT)
    L.setup_ada(condT, ada_w, ada_bT, n1T, n2T)
    xs = P.sbuf("xs", [128, 8, 514], F32); bxs = Buf("xs")
    x1 = P.sbuf("x1", [128, 8, 512], F32); bx1 = Buf("x1")
    if moe:
        L.alloc_moe(router)
    if final_norm:
        fnT = dt("fnT", [128, 8])
        fn = P.sbuf("fn", [128, 8], F32); bfn = Buf("fn")
        P.dma("sync", fn[:], fnT, writes=[bfn])
    outs = []
    tiles = []
    if with_ctx:
        tiles.append((cT, coT, 0, NCTX, 1, msk[:, 3:4], msk[:, 3:4]))
    for i in range(ntiles):
        mL = msk[:, 0:1] if i == 0 else msk[:, 2:3]
        mR = msk[:, 1:2] if i == NT // 512 - 1 else msk[:, 2:3]
        tiles.append((xT, oT, i * 512, 512, 0, mL, mR))
    for (src, dst, t0, N, c, mL, mR) in tiles:
        W = N + 2
        P.dma("sync", xs[:, :, 0:W], src[:, t0:t0 + W].rearrange("(k p) n -> p k n", p=128), writes=[bxs])
        L.conv_mixer(xs, bxs, N, c, w_in, w_out, mL, mR, bmsk, x1, bx1)
        if moe:
            L.ffn_moe(x1, bx1, N, c, w_gu, w_down, xs, bxs)
        else:
            L.ffn_dense(x1, bx1, N, c, w_gu, w_down, x1, bx1)
        if final_norm:
            L.final_norm(x1, bx1, N, fn, bfn, xs, bxs)
            ev = P.dma("sync", dst[:, t0:t0 + N].rearrange("(k p) n -> p k n", p=128), xs[:, :, 0:N], reads=[bxs])
        else:
            ev = P.dma("sync", dst[:, t0:t0 + N].rearrange("(k p) n -> p k n", p=128), x1[:, :, 0:N], reads=[bx1])
        outs.append(ev)
    _finish(P, outs, F)
    print("conv layer program: insts", P.ninst, "sems", P.nsem)
    return nc


def fm_vec(v, n):
    return np.ascontiguousarray(v.reshape(n, 128).T)


def host_inputs_conv(xfull, ctxfull, c, c_ctx, pfx, inp, with_ctx=True):
    maps = []
    for core in range(8):
        b, h = core // 2, core % 2
        xe = np.zeros((NT + 2, D), np.float32)
        lo = h * NT - 1
        hi = h * NT + NT + 1
        slo, shi = max(lo, 0), min(hi, xfull.shape[1])
        xe[slo - lo:shi - lo] = xfull[b, slo:shi]
        m = {
            "xT": np.ascontiguousarray(xe.T),
            "condT": np.ascontiguousarray(np.stack([c[b], c_ctx], axis=1)),
            "ada_w": inp[pfx + "ada_w"], "ada_bT": fm_vec(inp[pfx + "ada_b"], 48),
            "n1T": fm_vec(inp[pfx + "norm_mix"], 8), "n2T": fm_vec(inp[pfx + "norm_ffn"], 8),
            "conv_wT": np.ascontiguousarray(inp[pfx + "conv_w"].reshape(3, 8, 128).transpose(2, 0, 1)),
            "w_in": inp[pfx + "conv_w_in"], "w_out": inp[pfx + "conv_w_out"],
            "mask": np.ascontiguousarray(np.tile(np.array([[float(h == 1), float(h == 0), 1.0, 0.0]], np.float32), (128, 1))),
        }
        if with_ctx:
            ce = np.zeros((NCTX + 2, D), np.float32)
            ce[1:NCTX + 1] = ctxfull[b]
            m["cT"] = np.ascontiguousarray(ce.T)
        maps.append(m)
    return maps

import math

NK = 8448
NKT = 66
HD = 64


def rope_partner_perm():
    p = np.arange(128)
    d = p % 64
    lo = (d % 32) < 16
    return np.where(lo, p + 16, p - 16), lo


def rope_tables(tok0, n):
    t = np.arange(tok0, tok0 + n)
    row = (t // 64).astype(np.float32)
    col = (t % 64).astype(np.float32)
    inv = (np.float32(10000.0) ** (-np.arange(16, dtype=np.float32) / np.float32(16))).astype(np.float32)
    ang = np.concatenate([row[:, None] * inv, col[:, None] * inv], axis=-1).astype(np.float32)
    p = np.arange(128)
    d = p % 64
    lo = (d % 32) < 16
    fi = (d % 16) + 16 * (d >= 32)
    C = np.cos(ang[:, fi]).T.astype(np.float32)
    S = np.sin(ang[:, fi]).T.astype(np.float32)
    S = np.where(lo[:, None], -S, S).astype(np.float32)
    return np.ascontiguousarray(C), np.ascontiguousarray(S)


def build_qkv(F=None, fused=False):
    nc, sh, dt = _ctx(F)
    xT = dt("xT", [D, NT]); cT = dt("cT", [D, NCTX])
    condT = dt("condT", [D, 2]); ada_w = dt("ada_w", [D, 6 * D]); ada_bT = dt("ada_bT", [128, 48])
    n1T = dt("n1T", [128, 8]); n2T = dt("n2T", [128, 8])
    w_qkv = dt("w_qkv", [D, 3 * D]); w_perm = dt("w_perm", [D, 2 * D])
    ropeC = dt("ropeC", [128, NT]); ropeS = dt("ropeS", [128, NT])
    if fused:
        kall = dt("kall", [D, NCTX + NT], "ExternalOutput", BF16)
        v_tm = dt("v_tm", [NCTX + NT, D], "ExternalOutput", BF16)
        outs_l = [dt("qT", [D, NT], "ExternalOutput", BF16), kall[:, NCTX:], v_tm[NCTX:, :]]
        outs_c = [dt("qcT", [D, NCTX], "ExternalOutput", BF16), kall[:, 0:NCTX], v_tm[0:NCTX, :]]
    else:
        outs_l = [dt(n, [D, NT], "ExternalOutput", BF16) for n in ("qT", "kT", "vT")]
        outs_c = [dt(n, [D, NCTX], "ExternalOutput", BF16) for n in ("qcT", "kcT", "vcT")]
    P = Prog(nc, sh)
    L = LB(P, nw=4)
    L.alloc_common(512)
    L.alloc_scr(4)
    L.setup_ada(condT, ada_w, ada_bT, n1T, n2T)
    xs = P.sbuf("xs", [128, 8, 512], F32); bxs = Buf("xs")
    rc = P.sbuf("rc", [128, 512], F32); brc = Buf("rc")
    rsn = P.sbuf("rsn", [128, 512], F32); brsn = Buf("rsn")
    ob = [(P.sbuf("ob%d" % i, [128, 8, 512], BF16), Buf("ob%d" % i)) for i in range(3)]
    if fused:
        vb = P.sbuf("vb", [128, 4, 1024], BF16); bvb = Buf("vb")
    evs = []
    tiles = [(cT, outs_c, 0, NCTX, 1, False)] + [(xT, outs_l, i * 512, 512, 0, True) for i in range(NT // 512)]
    def body(src, dsts, t0, N, c, rope):
        P.dma("sync", xs[:, :, 0:N], src[:, t0:t0 + N].rearrange("(k p) n -> p k n", p=128), writes=[bxs])
        if rope:
            P.dma("sync", rc[:, 0:N], ropeC[:, t0:t0 + N], writes=[brc])
            P.dma("sync", rsn[:, 0:N], ropeS[:, t0:t0 + N], writes=[brsn])
        L.norm_mod(lambda k: xs[:, k, 0:N], bxs, N, L.gs1[c], L.mod[c][:, 0:8])
        hl, bhl = L.hl, L.bhl
        for part in range(3):
            o, bo = ob[part]
            if fused and part == 2:
                for q in range(2):
                    w, bw = L.load_w(w_qkv, 0, 8, 2048 + q * 512, 512)
                    for blk in range(N // 128):
                        ps, bps = L.next_ps()
                        L.mm(ps[:, 0:512], bps, [(hl[:, k, blk * 128:(blk + 1) * 128], w[:, k, :], [bw, bhl]) for k in range(8)])
                        P.op("scalar", lambda e, ps=ps, blk=blk, q=q: e.activation(out=vb[:, blk, q * 512:(q + 1) * 512], in_=ps[:, 0:512], func=AF.Copy), reads=[bps], writes=[bvb])
                nb_ = N // 128
                evs.append(P.dma("sync", dsts[2][t0:t0 + N, :].rearrange("(b p) c -> p b c", p=128), vb[:, 0:nb_, :], reads=[bvb]))
                continue
            for q in range(2):
                w, bw = L.load_w(w_qkv, 0, 8, part * 1024 + q * 512, 512)
                if rope and part < 2:
                    wp, bwp = L.load_w(w_perm, 0, 8, part * 1024 + q * 512, 512)
                for m in range(4):
                    k2 = q * 4 + m
                    ps, bps = L.next_ps()
                    L.mm(ps[:, 0:N], bps, [(w[:, k, m * 128:(m + 1) * 128], hl[:, k, 0:N], [bw, bhl]) for k in range(8)])
                    if rope and part < 2:
                        ps2, bps2 = L.next_ps()
                        L.mm(ps2[:, 0:N], bps2, [(wp[:, k, m * 128:(m + 1) * 128], hl[:, k, 0:N], [bwp, bhl]) for k in range(8)])
                        t1, bt1 = L.scr[(2 * k2) % 4]
                        t2, bt2 = L.scr[(2 * k2 + 1) % 4]
                        P.op("vector", lambda e, t1=t1, ps=ps: e.tensor_tensor(out=t1[:, 0:N], in0=ps[:, 0:N], in1=rc[:, 0:N], op=ALU.mult), reads=[bps, brc], writes=[bt1])
                        P.op("vector", lambda e, t2=t2, ps2=ps2: e.tensor_tensor(out=t2[:, 0:N], in0=ps2[:, 0:N], in1=rsn[:, 0:N], op=ALU.mult), reads=[bps2, brsn], writes=[bt2])
                        P.op("vector", lambda e, t1=t1, t2=t2, o=o, k2=k2: e.tensor_tensor(out=o[:, k2, 0:N], in0=t1[:, 0:N], in1=t2[:, 0:N], op=ALU.add), reads=[bt1, bt2], writes=[bo])
                    else:
                        P.op("scalar", lambda e, ps=ps, o=o, k2=k2: e.activation(out=o[:, k2, 0:N], in_=ps[:, 0:N], func=AF.Copy), reads=[bps], writes=[bo])
            evs.append(P.dma("sync", dsts[part][:, t0:t0 + N].rearrange("(k p) n -> p k n", p=128), o[:, :, 0:N], reads=[bo]))
    for tl in tiles:
        body(*tl)
    _finish(P, evs, F)
    print("qkv program: insts", P.ninst, "sems", P.nsem)
    return nc


def build_attn(lam_init, F=None, fused=False):
    nc, sh, dt = _ctx(F)
    qT = dt("qT", [D, NT], d=BF16)
    qcT = dt("qcT", [D, NCTX], d=BF16)
    kT = dt("kT", [D, NK], d=BF16)
    if fused:
        v_tm = dt("v_tm", [NK, D], d=BF16)
    else:
        vr = dt("vr", [8, 128, NKT * 128], d=BF16)
    lamb = dt("lamb", [128, 4, HD])
    sublnT = dt("sublnT", [128, 1])
    oT = dt("oT", [D, NT], "ExternalOutput", BF16)
    ocT = dt("ocT", [D, NCTX], "ExternalOutput", BF16)
    P = Prog(nc, sh)
    ones = P.sbuf("ones", [128, 128], BF16); bones = Buf("ones")
    P.op("vector", lambda e: e.memset(ones[:], 1.0), writes=[bones])
    lv = P.sbuf("lv", [128, 4, HD], F32); blv = Buf("lv")
    sl = P.sbuf("sl", [128, 1], F32); bsl = Buf("sl")
    P.dma("sync", lv[:], lamb, writes=[blv])
    P.dma("sync", sl[:], sublnT, writes=[bsl])
    pr = P.sbuf("pr", [128, 2, HD], F32); bpr = Buf("pr")
    sm = P.sbuf("sm", [128, 2], F32); bsm = Buf("sm")
    nlam = P.sbuf("nlam", [128, 1], F32); bnl = Buf("nlam")
    P.op("vector", lambda e: e.tensor_tensor(out=pr[:, 0, :], in0=lv[:, 0, :], in1=lv[:, 1, :], op=ALU.mult), reads=[blv], writes=[bpr])
    P.op("vector", lambda e: e.tensor_tensor(out=pr[:, 1, :], in0=lv[:, 2, :], in1=lv[:, 3, :], op=ALU.mult), reads=[blv, bpr], writes=[bpr])
    P.op("vector", lambda e: e.reduce_sum(out=sm[:], in_=pr[:], axis=AX.X), reads=[bpr], writes=[bsm])
    P.op("scalar", lambda e: e.activation(out=sm[:], in_=sm[:], func=AF.Exp), reads=[bsm], writes=[bsm])
    P.op("vector", lambda e: e.tensor_tensor(out=nlam[:], in0=sm[:, 1:2], in1=sm[:, 0:1], op=ALU.subtract), reads=[bsm], writes=[bnl])
    P.op("vector", lambda e: e.tensor_scalar(out=nlam[:], in0=nlam[:], scalar1=-float(lam_init), scalar2=None, op0=ALU.add), reads=[bnl], writes=[bnl])
    P.op("vector", lambda e: e.tensor_scalar(out=sl[:], in0=sl[:], scalar1=float(1.0 - lam_init), scalar2=None, op0=ALU.mult), reads=[bsl], writes=[bsl])

    psS = [(P.psum("pS%d" % i, [128, 512]), Buf("pS%d" % i)) for i in range(4)]
    psA = [(P.psum("pA%d" % i, [128, 512]), Buf("pA%d" % i)) for i in range(4)]
    E = [(P.sbuf("E%d" % i, [128, 512], BF16), Buf("E%d" % i)) for i in range(4)]
    kh = [(P.sbuf("kh%d" % i, [128, NK], BF16), Buf("kh%d" % i)) for i in range(2)]
    vh = [(P.sbuf("vh%d" % i, [128, NKT * 128], BF16), Buf("vh%d" % i)) for i in range(2)]
    qh = [(P.sbuf("qh%d" % i, [128, NT], BF16), Buf("qh%d" % i)) for i in range(2)]
    qch = [(P.sbuf("qch%d" % i, [128, NCTX], BF16), Buf("qch%d" % i)) for i in range(2)]
    f = [(P.sbuf("f%d" % i, [128, 512], F32), Buf("f%d" % i)) for i in range(4)]
    sqb = P.sbuf("sqb", [128, 512], BF16); bsqb = Buf("sqb")
    obuf = [(P.sbuf("obf%d" % i, [128, 512], BF16), Buf("obf%d" % i)) for i in range(2)]
    acc0 = [(P.sbuf("acc0_%d" % i, [128, 512], F32), Buf("acc0_%d" % i)) for i in range(2)]
    ones32 = P.sbuf("ones32", [128, 128], F32); bones32 = Buf("ones32")
    P.op("vector", lambda e: e.memset(ones32[:], 1.0), writes=[bones32])
    cnt = {"s": 0, "o": 0, "a": 0}
    evs = []

    def attend(h, q_ap, bq, N, kts, k_t, bk, v_t, bv, dst):
        nk = len(kts)

        def emit_S(i):
            kt = kts[i]
            res = []
            for m in range(2):
                ps, bps = psS[cnt["s"] % 4]
                e_, be = E[cnt["s"] % 4]
                cnt["s"] += 1
                lo = m * 64
                P.op("tensor", lambda e, ps=ps, lo=lo, kt=kt: e.matmul(ps[:, 0:N], lhsT=k_t[lo:lo + 64, kt * 128:(kt + 1) * 128], rhs=q_ap[lo:lo + 64, :], start=True, stop=True),
                     reads=[bk, bq], writes=[bps])
                P.op("scalar", lambda e, ps=ps, e_=e_: e.activation(out=e_[:, 0:N], in_=ps[:, 0:N], func=AF.Exp, scale=0.125), reads=[bps], writes=[be])
                res.append((e_, be))
            return res

        ac, bac = acc0[cnt["a"] % 2]
        cnt["a"] += 1
        cur = emit_S(0)
        for i in range(nk):
            nxt = emit_S(i + 1) if i + 1 < nk else None
            kt = kts[i]
            for m in range(2):
                e_, be = cur[m]
                po, bpo = psA[2 * m]
                pd, bpd = psA[2 * m + 1]
                P.op("tensor", lambda e, po=po, e_=e_, kt=kt, i=i: e.matmul(po[:, 0:N], lhsT=v_t[:, kt * 128:(kt + 1) * 128], rhs=e_[:, 0:N], start=(i == 0), stop=(i == nk - 1)),
                     reads=[bv, be], writes=[bpo], inc=(i == nk - 1))
                if m == 0:
                    if i == 0:
                        P.op("vector", lambda e, e_=e_: e.tensor_copy(out=ac[:, 0:N], in_=e_[:, 0:N]), reads=[be], writes=[bac])
                    else:
                        P.op("vector", lambda e, e_=e_: e.tensor_tensor(out=ac[:, 0:N], in0=ac[:, 0:N], in1=e_[:, 0:N], op=ALU.add), reads=[be, bac], writes=[bac])
                    if i == nk - 1:
                        P.op("tensor", lambda e, pd=pd: e.matmul(pd[:, 0:N], lhsT=ones32[:], rhs=ac[:, 0:N], start=True, stop=True), reads=[bones32, bac], writes=[bpd])
                else:
                    P.op("tensor", lambda e, pd=pd, e_=e_, i=i: e.matmul(pd[:, 0:N], lhsT=ones[:], rhs=e_[:, 0:N], start=(i == 0), stop=(i == nk - 1)),
                         reads=[bones, be], writes=[bpd], inc=True)
            cur = nxt
        (r0, br0), (r1, br1), (a, ba), (b, bb) = f
        P.op("vector", lambda e: e.reciprocal(out=r0[:, 0:N], in_=psA[1][0][:, 0:N]), reads=[psA[1][1]], writes=[br0])
        P.op("vector", lambda e: e.reciprocal(out=r1[:, 0:N], in_=psA[3][0][:, 0:N]), reads=[psA[3][1]], writes=[br1])
        P.op("vector", lambda e: e.tensor_tensor(out=a[:, 0:N], in0=psA[0][0][:, 0:N], in1=r0[:, 0:N], op=ALU.mult), reads=[psA[0][1], br0], writes=[ba])
        P.op("vector", lambda e: e.tensor_tensor(out=b[:, 0:N], in0=psA[2][0][:, 0:N], in1=r1[:, 0:N], op=ALU.mult), reads=[psA[2][1], br1], writes=[bb])
        P.op("vector", lambda e: e.scalar_tensor_tensor(out=a[:, 0:N], in0=b[:, 0:N], scalar=nlam[:, 0:1], in1=a[:, 0:N], op0=ALU.mult, op1=ALU.add), reads=[bb, ba, bnl], writes=[ba])
        P.op("scalar", lambda e: e.activation(out=sqb[:, 0:N], in_=a[:, 0:N], func=AF.Square), reads=[ba], writes=[bsqb])
        ps, bps = psS[cnt["s"] % 4]
        cnt["s"] += 1
        P.op("tensor", lambda e, ps=ps: e.matmul(ps[:, 0:N], lhsT=ones[:], rhs=sqb[:, 0:N], start=True, stop=True), reads=[bones, bsqb], writes=[bps])
        P.op("vector", lambda e, ps=ps: e.tensor_scalar(out=r0[:, 0:N], in0=ps[:, 0:N], scalar1=1.0 / 128, scalar2=EPS, op0=ALU.mult, op1=ALU.add), reads=[bps, br0], writes=[br0])
        P.op("scalar", lambda e: e.activation(out=r0[:, 0:N], in_=r0[:, 0:N], func=AF.Sqrt), reads=[br0], writes=[br0])
        P.op("vector", lambda e: e.reciprocal(out=r0[:, 0:N], in_=r0[:, 0:N]), reads=[br0], writes=[br0])
        ob_, bob = obuf[cnt["o"] % 2]
        cnt["o"] += 1
        P.op("vector", lambda e, ob_=ob_: e.scalar_tensor_tensor(out=ob_[:, 0:N], in0=a[:, 0:N], scalar=sl[:, 0:1], in1=r0[:, 0:N], op0=ALU.mult, op1=ALU.mult), reads=[ba, br0, bsl], writes=[bob])
        evs.append(P.dma("sync", dst, ob_[:, 0:N], reads=[bob]))

    for h in range(8):
        k_t, bk = kh[h % 2]
        v_t, bv = vh[h % 2]
        q_t, bq = qh[h % 2]
        qc_t, bqc = qch[h % 2]
        P.dma("sync", k_t[:], kT[h * 128:(h + 1) * 128, :], writes=[bk])
        if fused:
            P.dma("sync", v_t[:].rearrange("p (kt e) -> p kt e", e=128), v_tm[:, h * 128:(h + 1) * 128].rearrange("(kt p) e -> p kt e", p=128), writes=[bv])
        else:
            P.dma("sync", v_t[:], vr[h], writes=[bv])
        P.dma("sync", q_t[:], qT[h * 128:(h + 1) * 128, :], writes=[bq])
        P.dma("sync", qc_t[:], qcT[h * 128:(h + 1) * 128, :], writes=[bqc])
        attend(h, qc_t[:, 0:NCTX], bqc, NCTX, [0, 1], k_t, bk, v_t, bv, ocT[h * 128:(h + 1) * 128, :])
        for i in range(NT // 512):
            attend(h, q_t[:, i * 512:(i + 1) * 512], bq, 512, list(range(NKT)), k_t, bk, v_t, bv, oT[h * 128:(h + 1) * 128, i * 512:(i + 1) * 512])
    _finish(P, evs, F)
    print("attn program: insts", P.ninst, "sems", P.nsem)
    return nc


def build_oproj_ffn(moe=True, with_ctx=True, ntiles=None, in_bf16=True, name_w="w_o", glu=False, F=None):
    nc, sh, dt = _ctx(F)
    if ntiles is None:
        ntiles = NT // 512
    xT = dt("xT", [D, NT])
    oT = dt("oT", [D, NT], d=BF16 if in_bf16 else F32)
    if with_ctx:
        cT = dt("cT", [D, NCTX]); ocT = dt("ocT", [D, NCTX], d=BF16 if in_bf16 else F32)
        coT = dt("coT", [D, NCTX], "ExternalOutput")
    condT = dt("condT", [D, 2]); ada_w = dt("ada_w", [D, 6 * D]); ada_bT = dt("ada_bT", [128, 48])
    n1T = dt("n1T", [128, 8]); n2T = dt("n2T", [128, 8])
    w_o = dt("w_o", [D, 2 * D if glu else D])
    if moe:
        w_gu = dt("w_gu", [8, D, 2 * DFF]); w_down = dt("w_down", [8, DFF, D]); router = dt("router", [D, 8])
    else:
        w_gu = dt("w_gu", [D, 2 * DFF]); w_down = dt("w_down", [DFF, D])
    outT = dt("outT", [D, NT], "ExternalOutput")
    P = Prog(nc, sh)
    L = LB(P, nw=4)
    L.alloc_common(512)
    L.alloc_ffn()
    L.setup_ada(condT, ada_w, ada_bT, n1T, n2T)
    xs = P.sbuf("xs", [128, 8, 512], F32); bxs = Buf("xs")
    x1 = P.sbuf("x1", [128, 8, 512], F32); bx1 = Buf("x1")
    ob = P.sbuf("ob", [128, 8, 512], BF16); bob = Buf("ob")
    if not in_bf16:
        ob32 = P.sbuf("ob32", [128, 8, 512], F32); bob32 = Buf("ob32")
    if moe:
        L.alloc_moe(router)
    else:
        L.alloc_scr(2)
    evs = []
    tiles = []
    if with_ctx:
        tiles.append((cT, ocT, coT, 0, NCTX, 1))
    tiles += [(xT, oT, outT, i * 512, 512, 0) for i in range(ntiles)]
    def body(src, osrc, dst, t0, N, c):
        mod = L.mod[c]
        P.dma("sync", xs[:, :, 0:N], src[:, t0:t0 + N].rearrange("(k p) n -> p k n", p=128), writes=[bxs])
        if in_bf16:
            P.dma("sync", ob[:, :, 0:N], osrc[:, t0:t0 + N].rearrange("(k p) n -> p k n", p=128), writes=[bob])
        else:
            P.dma("sync", ob32[:, :, 0:N], osrc[:, t0:t0 + N].rearrange("(k p) n -> p k n", p=128), writes=[bob32])
            for k in range(8):
                P.op("scalar", lambda e, k=k: e.activation(out=ob[:, k, 0:N], in_=ob32[:, k, 0:N], func=AF.Gelu), reads=[bob32], writes=[bob])
        for q in range(2):
            if glu:
                wv, bwv = L.load_w(w_o, 0, 8, q * 512, 512)
                wg, bwg = L.load_w(w_o, 0, 8, D + q * 512, 512)
            else:
                wv, bwv = L.load_w(w_o, 0, 8, q * 512, 512)
            for m in range(4):
                k3 = q * 4 + m
                ps, bps = L.next_ps()
                L.mm(ps[:, 0:N], bps, [(wv[:, k, m * 128:(m + 1) * 128], ob[:, k, 0:N], [bwv, bob]) for k in range(8)])
                if glu:
                    ps2, bps2 = L.next_ps()
                    L.mm(ps2[:, 0:N], bps2, [(wg[:, k, m * 128:(m + 1) * 128], ob[:, k, 0:N], [bwg, bob]) for k in range(8)])
                    sg, bsg = L.scr[k3 % 2]
                    P.op("scalar", lambda e, sg=sg, ps2=ps2: e.activation(out=sg[:, 0:N], in_=ps2[:, 0:N], func=AF.Sigmoid), reads=[bps2], writes=[bsg])
                    P.op("vector", lambda e, sg=sg, ps=ps: e.tensor_tensor(out=sg[:, 0:N], in0=sg[:, 0:N], in1=ps[:, 0:N], op=ALU.mult), reads=[bsg, bps], writes=[bsg])
                    P.op("vector", lambda e, sg=sg, k3=k3: e.scalar_tensor_tensor(out=x1[:, k3, 0:N], in0=sg[:, 0:N], scalar=mod[:, 16 + k3:17 + k3], in1=xs[:, k3, 0:N], op0=ALU.mult, op1=ALU.add),
                         reads=[bsg, bxs, L.bmod], writes=[bx1])
                else:
                    P.op("vector", lambda e, ps=ps, k3=k3: e.scalar_tensor_tensor(out=x1[:, k3, 0:N], in0=ps[:, 0:N], scalar=mod[:, 16 + k3:17 + k3], in1=xs[:, k3, 0:N], op0=ALU.mult, op1=ALU.add),
                         reads=[bps, bxs, L.bmod], writes=[bx1])
        if moe:
            L.ffn_moe(x1, bx1, N, c, w_gu, w_down, xs, bxs)
        else:
            L.ffn_dense(x1, bx1, N, c, w_gu, w_down, x1, bx1)
        evs.append(P.dma("sync", dst[:, t0:t0 + N].rearrange("(k p) n -> p k n", p=128), x1[:, :, 0:N], reads=[bx1]))
    for tl in tiles:
        body(*tl)
    _finish(P, evs, F)
    print("oproj_ffn program: insts", P.ninst, "sems", P.nsem)
    return nc


def T(a):
    return np.ascontiguousarray(a.T)


def ada_maps(inp, pfx, c, c_ctx, b):
    return {"condT": np.ascontiguousarray(np.stack([c[b], c_ctx], axis=1)),
            "ada_w": inp[pfx + "ada_w"], "ada_bT": fm_vec(inp[pfx + "ada_b"], 48),
            "n1T": fm_vec(inp[pfx + "norm_mix"], 8), "n2T": fm_vec(inp[pfx + "norm_ffn"], 8)}


def run_layer1(xfull, ctxfull, inp, progs, ntiles=8):
    pfx = "l1_"
    c, c_ctx = inp["c"], inp["c_ctx"]
    perm, lo = rope_partner_perm()
    wq = inp[pfx + "attn_w_qkv"]
    colperm = np.concatenate([part * 1024 + h * 128 + perm for part in range(2) for h in range(8)])
    w_perm = np.ascontiguousarray(wq[:, colperm])
    maps = []
    for core in range(8):
        b, h = core // 2, core % 2
        C, S = rope_tables(h * NT, NT)
        m = ada_maps(inp, pfx, c, c_ctx, b)
        m.update({"xT": T(xfull[b, h * NT:(h + 1) * NT]), "cT": T(ctxfull[b]), "w_qkv": wq, "w_perm": w_perm, "ropeC": C, "ropeS": S})
        maps.append(m)
    r1 = run_bass_kernel_spmd(progs["qkv"], maps, core_ids=list(range(8))).results
    maps = []
    lamb = np.ascontiguousarray(np.broadcast_to(inp[pfx + "attn_lam"][None], (128, 4, HD))).astype(np.float32)
    for core in range(8):
        b, h = core // 2, core % 2
        kall = np.concatenate([r1[2 * b]["kcT"], r1[2 * b]["kT"], r1[2 * b + 1]["kT"]], axis=1)
        vall = np.concatenate([r1[2 * b]["vcT"], r1[2 * b]["vT"], r1[2 * b + 1]["vT"]], axis=1)
        vr = np.ascontiguousarray(vall.reshape(8, 128, NKT, 128).transpose(0, 3, 2, 1)).reshape(8, 128, NKT * 128)
        maps.append({"qT": r1[core]["qT"], "qcT": r1[core]["qcT"], "kT": np.ascontiguousarray(kall), "vr": vr,
                     "lamb": lamb, "sublnT": np.ascontiguousarray(inp[pfx + "attn_subln"].reshape(128, 1))})
    r2 = run_bass_kernel_spmd(progs["attn"], maps, core_ids=list(range(8))).results
    maps = []
    for core in range(8):
        b, h = core // 2, core % 2
        m = ada_maps(inp, pfx, c, c_ctx, b)
        m.update({"xT": T(xfull[b, h * NT:(h + 1) * NT]), "cT": T(ctxfull[b]), "oT": r2[core]["oT"], "ocT": r2[core]["ocT"],
                  "w_o": inp[pfx + "attn_w_o"], "w_gu": inp[pfx + "moe_w_gu"], "w_down": inp[pfx + "moe_w_down"], "router": inp[pfx + "moe_router"]})
        maps.append(m)
    r3 = run_bass_kernel_spmd(progs["oproj_moe"], maps, core_ids=list(range(8))).results
    xo = np.empty_like(xfull)
    co = np.empty_like(ctxfull)
    for core in range(8):
        b, h = core // 2, core % 2
        xo[b, h * NT:(h + 1) * NT] = r3[core]["outT"].T
        if h == 0:
            co[b] = r3[core]["coT"].T
    return xo, co, (r1, r2)


NS = 8448
TWO_PI = 2.0 * math.pi


def build_hl(with_ctx=True, F=None, fused=False):
    nc, sh, dt = _ctx(F)
    xT = dt("xT", [D, NT]); cT = dt("cT", [D, NCTX])
    condT = dt("condT", [D, 2]); ada_w = dt("ada_w", [D, 6 * D]); ada_bT = dt("ada_bT", [128, 48])
    n1T = dt("n1T", [128, 8]); n2T = dt("n2T", [128, 8])
    if fused:
        hT = dt("hT", [D, NCTX + NT], "ExternalOutput")
        hlT = hT[:, NCTX:]; hcT = hT[:, 0:NCTX]
    else:
        hlT = dt("hlT", [D, NT], "ExternalOutput"); hcT = dt("hcT", [D, NCTX], "ExternalOutput")
    P = Prog(nc, sh)
    L = LB(P, nw=2)
    L.alloc_common(512)
    L.setup_ada(condT, ada_w, ada_bT, n1T, n2T)
    xs = P.sbuf("xs", [128, 8, 512], F32); bxs = Buf("xs")
    ho = P.sbuf("ho", [128, 8, 512], F32); bho = Buf("ho")
    evs = []

    def body(src, dst, t0, N, c):
        P.dma("sync", xs[:, :, 0:N], src[:, t0:t0 + N].rearrange("(k p) n -> p k n", p=128), writes=[bxs])
        L.norm_mod(lambda k: xs[:, k, 0:N], bxs, N, L.gs1[c], L.mod[c][:, 0:8], out32=ho, bout32=bho)
        evs.append(P.dma("sync", dst[:, t0:t0 + N].rearrange("(k p) n -> p k n", p=128), ho[:, :, 0:N], reads=[bho]))
    body(cT, hcT, 0, NCTX, 1)
    for i in range(NT // 512):
        body(xT, hlT, i * 512, 512, 0)
    _finish(P, evs, F)
    print("hl program: insts", P.ninst, "sems", P.nsem)
    return nc


def build_s5(nb=4, ndg=8, F=None, fused=False):
    nc, sh, dt = _ctx(F)
    if fused:
        hTc = dt("hTc", [128, NS])
        ySc = dt("ySc", [2, 128, NS - NCTX], "ExternalOutput")
        nb = 1
    else:
        u = dt("u", [2, nb, 128, NS])
    prm_s = dt("prm_s", [128, 3, 8])
    prm_f = dt("prm_f", [128, 5, 2, 64])
    ct = dt("ct", [128, 2, 8, 16])
    gmask = dt("gmask", [128, 8])
    if not fused:
        y = dt("y", [2, nb, 128, NS - NCTX], "ExternalOutput")
    P = Prog(nc, sh)
    TT = lambda eng, o, a, b, op, r, w: P.op(eng, lambda e: e.tensor_tensor(out=o, in0=a, in1=b, op=op), reads=r, writes=w)
    STT = lambda eng, o, a, sc, b, op0, op1, r, w: P.op(eng, lambda e: e.scalar_tensor_tensor(out=o, in0=a, scalar=sc, in1=b, op0=op0, op1=op1), reads=r, writes=w)

    def TS(eng, o, a, s1, s2, op0, op1, r, w):
        if s2 is None:
            return P.op(eng, lambda e: e.tensor_scalar(out=o, in0=a, scalar1=s1, scalar2=None, op0=op0), reads=r, writes=w)
        return P.op(eng, lambda e: e.tensor_scalar(out=o, in0=a, scalar1=s1, scalar2=s2, op0=op0, op1=op1), reads=r, writes=w)
    ACT = lambda o, a, func, r, w, **kw: P.op("scalar", lambda e: e.activation(out=o, in_=a, func=func, **kw), reads=r, writes=w)
    V = "vector"
    G = "gpsimd"

    bprm = Buf("prm")
    ps_ = P.sbuf("ps_", [128, 3, 8], F32)
    pf_ = P.sbuf("pf_", [128, 5, 2, 64], F32)
    ct_ = P.sbuf("ct_", [128, 2, 8, 16], F32)
    gm_ = P.sbuf("gm_", [128, 8], F32)
    P.dma("sync", ps_[:], prm_s, writes=[bprm])
    P.dma("sync", pf_[:], prm_f, writes=[bprm])
    P.dma("sync", ct_[:], ct, writes=[bprm])
    P.dma("sync", gm_[:], gmask, writes=[bprm])
    hpi = P.sbuf("hpi", [128, 1], F32)
    bS = Buf("setup")
    P.op(V, lambda e: e.memset(hpi[:], math.pi / 2), writes=[bS])

    def sincos_inplace(eng, xs_, xc_, r, w):
        ki = xc_.bitcast(I32)
        TS(eng, ki, xs_, 1.0 / TWO_PI, None, ALU.mult, None, r, w)
        P.op(eng, lambda e: e.tensor_copy(out=xc_, in_=ki), reads=r, writes=w)
        STT(eng, xs_, xc_, -TWO_PI, xs_, ALU.mult, ALU.add, r, w)
        TS(eng, xs_, xs_, math.pi, -math.pi, ALU.min, ALU.max, r, w)
        TS(eng, xc_, xs_, math.pi / 2, None, ALU.is_gt, None, r, w)
        STT(eng, xc_, xc_, -TWO_PI, xs_, ALU.mult, ALU.add, r, w)
        ACT(xs_, xs_, AF.Sin, r, w)
        ACT(xc_, xc_, AF.Sin, r, w, bias=hpi[:, 0:1], scale=1.0)

    dts = P.sbuf("dts", [128, 8], F32)
    r_s = P.sbuf("r_s", [128, 8], F32)
    th_s = P.sbuf("th_s", [128, 8], F32)
    ki_s = P.sbuf("ki_s", [128, 8], I32)
    kf_s = P.sbuf("kf_s", [128, 8], F32)
    ACT(dts[:], ps_[:, 2, :], AF.Exp, [bprm], [bS])
    TT(V, r_s[:], dts[:], ps_[:, 0, :], ALU.mult, [bS, bprm], [bS])
    ACT(r_s[:], r_s[:], AF.Exp, [bS], [bS])
    TT(V, th_s[:], dts[:], ps_[:, 1, :], ALU.mult, [bS, bprm], [bS])
    TS(V, ki_s[:], th_s[:], 1.0 / TWO_PI, None, ALU.mult, None, [bS], [bS])
    P.op(V, lambda e: e.tensor_copy(out=kf_s[:], in_=ki_s[:]), reads=[bS], writes=[bS])
    STT(V, th_s[:], kf_s[:], -TWO_PI, th_s[:], ALU.mult, ALU.add, [bS], [bS])

    sh = [128, 2, 64]
    dtf = P.sbuf("dtf", sh, F32); mag = P.sbuf("mag", sh, F32)
    snf = P.sbuf("snf", sh, F32); csf = P.sbuf("csf", sh, F32)
    AR = pf_[:, 0]; AI = pf_[:, 1]; LDT = pf_[:, 2]; BTR = pf_[:, 3]; BTI = pf_[:, 4]
    ACT(dtf[:], LDT, AF.Exp, [bprm], [bS])
    TT(V, mag[:], dtf[:], AR, ALU.mult, [bS, bprm], [bS])
    ACT(mag[:], mag[:], AF.Exp, [bS], [bS])
    TT(V, snf[:], dtf[:], AI, ALU.mult, [bS, bprm], [bS])
    sincos_inplace(V, snf[:], csf[:], [bS], [bS])
    abr = P.sbuf("abr", sh, F32); abi = P.sbuf("abi", sh, F32); den = P.sbuf("den", sh, F32)
    t1 = P.sbuf("t1s", sh, F32); t2 = P.sbuf("t2s", sh, F32)
    cor = P.sbuf("cor", sh, F32); coi = P.sbuf("coi", sh, F32)
    TT(V, abr[:], mag[:], csf[:], ALU.mult, [bS], [bS])
    TT(V, abi[:], mag[:], snf[:], ALU.mult, [bS], [bS])
    TS(V, abr[:], abr[:], -1.0, None, ALU.add, None, [bS], [bS])
    TT(V, den[:], AR, AR, ALU.mult, [bprm, bS], [bS])
    TT(V, t1[:], AI, AI, ALU.mult, [bprm, bS], [bS])
    TT(V, den[:], den[:], t1[:], ALU.add, [bS], [bS])
    P.op(V, lambda e: e.reciprocal(out=den[:], in_=den[:]), reads=[bS], writes=[bS])
    TT(V, t1[:], abr[:], AR, ALU.mult, [bS, bprm], [bS])
    TT(V, t2[:], abi[:], AI, ALU.mult, [bS, bprm], [bS])
    TT(V, cor[:], t1[:], t2[:], ALU.add, [bS], [bS])
    TT(V, cor[:], cor[:], den[:], ALU.mult, [bS], [bS])
    TT(V, t1[:], abi[:], AR, ALU.mult, [bS, bprm], [bS])
    TT(V, t2[:], abr[:], AI, ALU.mult, [bS, bprm], [bS])
    TT(V, coi[:], t1[:], t2[:], ALU.subtract, [bS], [bS])
    TT(V, coi[:], coi[:], den[:], ALU.mult, [bS], [bS])
    bbr = P.sbuf("bbr", sh, F32); bbi = P.sbuf("bbi", sh, F32)
    TT(V, t1[:], cor[:], BTR, ALU.mult, [bS, bprm], [bS])
    TT(V, t2[:], coi[:], BTI, ALU.mult, [bS, bprm], [bS])
    TT(V, bbr[:], t1[:], t2[:], ALU.subtract, [bS], [bS])
    TT(V, t1[:], cor[:], BTI, ALU.mult, [bS, bprm], [bS])
    TT(V, t2[:], coi[:], BTR, ALU.mult, [bS, bprm], [bS])
    TT(V, bbi[:], t1[:], t2[:], ALU.add, [bS], [bS])
    WB = P.sbuf("WB", [128, 2, 4, 2, 128], F32)
    for d in range(2):
        for g in range(8):
            for ri, src in enumerate((bbr, bbi)):
                TS(V, WB[:, d, g // 2, ri, (g % 2) * 64:(g % 2 + 1) * 64], src[:, d, :], gm_[:, g:g + 1], None, ALU.mult, None, [bS, bprm], [bS])
    WC = P.sbuf("WC", [128, 2, 4, 2, 128], BF16)
    P.op(V, lambda e: e.memset(WC[:], 0.0), reads=[bS], writes=[bS])
    for d in range(2):
        for gp in range(4):
            for g2 in range(2):
                c0 = (2 * gp + g2) * 16
                lo = g2 * 64
                P.op(V, lambda e, d=d, gp=gp, c0=c0, lo=lo: e.tensor_copy(out=WC[lo:lo + 64, d, gp, 0, c0:c0 + 16], in_=ct_[lo:lo + 64, 0, d * 4 + gp, :]), reads=[bS, bprm], writes=[bS])
                TS(V, WC[lo:lo + 64, d, gp, 1, c0:c0 + 16], ct_[lo:lo + 64, 1, d * 4 + gp, :], -1.0, None, ALU.mult, None, [bS, bprm], [bS])

    iot = P.sbuf("iot", [128, NS], F32)
    P.op(G, lambda e: e.iota(iot[:], pattern=[[1, NS]], base=0, channel_multiplier=0, allow_small_or_imprecise_dtypes=True), writes=[bS], reads=[bS])

    tabS = P.sbuf("tabS", [128, NS], F32); tabC = P.sbuf("tabC", [128, NS], F32); btab = Buf("tab")
    TW = 1024 if fused else 512
    rt = P.sbuf("rt", [128, TW], F32); brt = Buf("rt")
    ub = [(P.sbuf("ub%d" % i, [128, TW], F32), Buf("ub%d" % i)) for i in range(4 if not fused else 2)]
    NUB = len(ub)
    ur = [(P.sbuf("ur%d" % i, [128, TW], F32), Buf("ur%d" % i)) for i in range(1)]

    def nat0(t0, N):
        if t0 < NCTX:
            return 0
        return NCTX + (NS - NCTX) - (t0 - NCTX) - N
    NPB = 4 if TW == 512 else 2
    psB = [(P.psum("psB%d" % i, [128, TW]), Buf("psB%d" % i)) for i in range(NPB)]
    psY = [(P.psum("psY%d" % i, [128, TW]), Buf("psY%d" % i)) for i in range(2)]
    W4 = lambda nm, n=2, dt_=F32: [(P.sbuf("%s%d" % (nm, i), [128, TW], dt_), Buf("%s%d" % (nm, i))) for i in range(n)]
    tA, tB, tC_, tD = W4("tA", 1), W4("tB", 1), W4("tC", 1), W4("tD", 1)
    NM = 2 if TW == 512 else 1
    mre, mim = W4("mre", NM), W4("mim", NM)
    gre, gim = W4("gre"), W4("gim")
    dA, dB = W4("dA", 1), W4("dB", 1)
    hre, him = W4("hre", 2, BF16), W4("him", 2, BF16)
    ysb = W4("ysb", NM)
    car = P.sbuf("car", [128, 2], F32); bcar = Buf("car")
    evs = []
    its = []
    tiles = [(0, NCTX)] + [(NCTX + i * TW, TW) for i in range((NS - NCTX) // TW)]
    for j in range(ndg):
        d, gp = j // 4, j % 4
        for b in range(nb):
            for (t0, N) in tiles:
                its.append((d, gp, b, t0, N))
    loaded = {}

    def emit_load(i):
        if i >= len(its) or i in loaded:
            return
        d, gp, b, t0, N = its[i]
        ut, but = ub[i % NUB]
        if fused:
            c0 = t0 if d == 0 else nat0(t0, N)
            P.dma("sync", ut[:, 0:N], hTc[:, c0:c0 + N], writes=[but])
        else:
            P.dma("sync", ut[:, 0:N], u[d, b, :, t0:t0 + N], writes=[but])
        loaded[i] = True

    PD = 2 if NUB >= 4 else 1
    for j0 in range(PD):
        emit_load(j0)
    cur = None
    for i, (d, gp, b, t0, N) in enumerate(its):
        emit_load(i + PD)
        j = d * 4 + gp
        if cur != j:
            cur = j
            ACT(tabS[:], iot[:], AF.Identity, [bS, btab], [btab], scale=th_s[:, j:j + 1])
            sincos_inplace(V, tabS[:], tabC[:], [btab, bS], [btab])
            P.op(V, lambda e, j=j: e.tensor_copy(out=rt[:], in_=r_s[:, j:j + 1].to_broadcast([128, TW])), reads=[bS, brt], writes=[brt])
        if t0 == 0:
            P.op(V, lambda e: e.memset(car[:], 0.0), reads=[bcar], writes=[bcar])
        s = i % 2
        sm = i % NM
        ut, but = ub[i % NUB]
        pr, bpr = psB[(2 * i) % NPB]
        pi_, bpi = psB[(2 * i + 1) % NPB]
        if fused and d == 1:
            urt, burt = ur[0]
            ACT(urt[:, 0:N], ut[:, 0:N][:, ::-1], AF.Copy, [but], [burt])
            ut, but = urt, burt
        for h0 in range(0, N, 512):
            h1 = min(N, h0 + 512)
            P.op("tensor", lambda e, pr=pr, ut=ut, h0=h0, h1=h1, d=d, gp=gp: e.matmul(pr[:, h0:h1], lhsT=WB[:, d, gp, 0, :], rhs=ut[:, h0:h1], start=True, stop=True), reads=[bS, but], writes=[bpr])
            P.op("tensor", lambda e, pi_=pi_, ut=ut, h0=h0, h1=h1, d=d, gp=gp: e.matmul(pi_[:, h0:h1], lhsT=WB[:, d, gp, 1, :], rhs=ut[:, h0:h1], start=True, stop=True), reads=[bS, but], writes=[bpi])
        Cs = tabC[:, t0:t0 + N]
        Ss = tabS[:, t0:t0 + N]
        (a_, ba), (b_, bb), (c_, bc), (d_, bd) = tA[0], tB[0], tC_[0], tD[0]
        (mr, bmr), (mi, bmi) = mre[sm], mim[sm]
        (gr, bgr), (gi, bgi) = gre[s], gim[s]
        TT(V, a_[:, 0:N], pr[:, 0:N], Cs, ALU.mult, [bpr, btab], [ba])
        TT(V, b_[:, 0:N], pi_[:, 0:N], Ss, ALU.mult, [bpi, btab], [bb])
        TT(V, mr[:, 0:N], a_[:, 0:N], b_[:, 0:N], ALU.add, [ba, bb], [bmr])
        TT(V, c_[:, 0:N], pi_[:, 0:N], Cs, ALU.mult, [bpi, btab], [bc])
        TT(V, d_[:, 0:N], pr[:, 0:N], Ss, ALU.mult, [bpr, btab], [bd])
        TT(V, mi[:, 0:N], c_[:, 0:N], d_[:, 0:N], ALU.subtract, [bc, bd], [bmi])
        P.op(V, lambda e, gr=gr, mr=mr, N=N: e.tensor_tensor_scan(out=gr[:, 0:N], data0=rt[:, 0:N], data1=mr[:, 0:N], initial=car[:, 0:1], op0=ALU.mult, op1=ALU.add),
             reads=[brt, bmr, bcar], writes=[bgr])
        P.op(V, lambda e, gi=gi, mi=mi, N=N: e.tensor_tensor_scan(out=gi[:, 0:N], data0=rt[:, 0:N], data1=mi[:, 0:N], initial=car[:, 1:2], op0=ALU.mult, op1=ALU.add),
             reads=[brt, bmi, bcar], writes=[bgi])
        ACT(car[:, 0:1], gr[:, N - 1:N], AF.Copy, [bgr, bcar], [bcar])
        ACT(car[:, 1:2], gi[:, N - 1:N], AF.Copy, [bgi, bcar], [bcar])
        if t0 < NCTX:
            continue
        (e_, be), (f_, bf) = dA[0], dB[0]
        (hr, bhr), (hi, bhi) = hre[s], him[s]
        DE = V if fused else G
        TT(DE, e_[:, 0:N], gr[:, 0:N], Cs, ALU.mult, [bgr, btab], [be])
        TT(DE, f_[:, 0:N], gi[:, 0:N], Ss, ALU.mult, [bgi, btab], [bf])
        TT(DE, hr[:, 0:N], e_[:, 0:N], f_[:, 0:N], ALU.subtract, [be, bf], [bhr])
        TT(DE, e_[:, 0:N], gi[:, 0:N], Cs, ALU.mult, [bgi, btab, be], [be])
        TT(DE, f_[:, 0:N], gr[:, 0:N], Ss, ALU.mult, [bgr, btab, bf], [bf])
        TT(DE, hi[:, 0:N], e_[:, 0:N], f_[:, 0:N], ALU.add, [be, bf], [bhi])
        py, bpy = psY[i % 2]
        for h0 in range(0, N, 512):
            h1 = min(N, h0 + 512)
            P.op("tensor", lambda e, py=py, hr=hr, h0=h0, h1=h1, d=d, gp=gp: e.matmul(py[:, h0:h1], lhsT=WC[:, d, gp, 0, :], rhs=hr[:, h0:h1], start=True, stop=False), reads=[bS, bhr], writes=[bpy], inc=False)
            P.op("tensor", lambda e, py=py, hi=hi, h0=h0, h1=h1, d=d, gp=gp: e.matmul(py[:, h0:h1], lhsT=WC[:, d, gp, 1, :], rhs=hi[:, h0:h1], start=False, stop=True), reads=[bS, bhi], writes=[bpy])
        ys, bys = ysb[sm]
        if fused and d == 1:
            ACT(ys[:, 0:N], py[:, 0:N][:, ::-1], AF.Copy, [bpy], [bys])
            c0 = nat0(t0, N) - NCTX
            evs.append(P.dma("scalar", ySc[1, gp * 32:(gp + 1) * 32, c0:c0 + N], ys[gp * 32:(gp + 1) * 32, 0:N], reads=[bys]))
        elif fused:
            ACT(ys[:, 0:N], py[:, 0:N], AF.Copy, [bpy], [bys])
            evs.append(P.dma("scalar", ySc[0, gp * 32:(gp + 1) * 32, t0 - NCTX:t0 - NCTX + N], ys[gp * 32:(gp + 1) * 32, 0:N], reads=[bys]))
        else:
            ACT(ys[:, 0:N], py[:, 0:N], AF.Copy, [bpy], [bys])
            evs.append(P.dma("scalar", y[d, b, gp * 32:(gp + 1) * 32, t0 - NCTX:t0 - NCTX + N], ys[gp * 32:(gp + 1) * 32, 0:N], reads=[bys]))
    _finish(P, evs, F)
    print("s5 program: insts", P.ninst, "sems", P.nsem)
    return nc


def s5_host_params(inp, pfx, k):
    gs = slice(8 * k, 8 * k + 8)
    a_re, a_im, ldt = inp[pfx + "ssm_a_re"][:, gs], inp[pfx + "ssm_a_im"][:, gs], inp[pfx + "ssm_log_dt"][:, gs]
    b_re, b_im = inp[pfx + "ssm_b_re"][:, gs], inp[pfx + "ssm_b_im"][:, gs]
    c_re, c_im = inp[pfx + "ssm_c_re"][:, gs], inp[pfx + "ssm_c_im"][:, gs]
    def st(a):
        return a.reshape(2, 4, 2, 64).transpose(2, 3, 0, 1).reshape(128, 8)
    ldt_s = np.broadcast_to(ldt[:, :, None], (2, 8, 64))
    prm_s = np.stack([st(a_re), st(a_im), st(ldt_s)], axis=1).astype(np.float32)
    def ft(a):
        return np.broadcast_to(a.transpose(1, 0, 2)[:, None], (8, 16, 2, 64)).reshape(128, 2, 64)
    btr = b_re.transpose(1, 3, 0, 2).reshape(128, 2, 64)
    bti = b_im.transpose(1, 3, 0, 2).reshape(128, 2, 64)
    prm_f = np.stack([ft(a_re), ft(a_im), ft(ldt_s), btr, bti], axis=1).astype(np.float32)
    def ctl(c):
        return c.reshape(2, 4, 2, 16, 64).transpose(2, 4, 0, 1, 3).reshape(128, 8, 16)
    ct = np.stack([ctl(c_re), ctl(c_im)], axis=1).astype(np.float32)
    gm = np.zeros((128, 8), np.float32)
    for g in range(8):
        gm[g * 16:(g + 1) * 16, g] = 1.0
    return {"prm_s": np.ascontiguousarray(prm_s), "prm_f": np.ascontiguousarray(prm_f), "ct": np.ascontiguousarray(ct), "gmask": gm}


def build_glu_ffn(ntiles=None, F=None):
    nc, sh, dt = _ctx(F)
    if ntiles is None:
        ntiles = NT // 512
    xT = dt("xT", [D, NT]); hlT = dt("hlT", [D, NT]); yfT = dt("yfT", [D, NT]); ybT = dt("ybT", [D, NT])
    dskT = dt("dskT", [128, 8])
    condT = dt("condT", [D, 2]); ada_w = dt("ada_w", [D, 6 * D]); ada_bT = dt("ada_bT", [128, 48])
    n1T = dt("n1T", [128, 8]); n2T = dt("n2T", [128, 8])
    w_glu = dt("w_glu", [D, 2 * D])
    w_gu = dt("w_gu", [D, 2 * DFF]); w_down = dt("w_down", [DFF, D])
    outT = dt("outT", [D, NT], "ExternalOutput")
    P = Prog(nc, sh)
    L = LB(P, nw=4)
    L.alloc_common(512)
    L.alloc_ffn()
    L.alloc_scr(4)
    L.setup_ada(condT, ada_w, ada_bT, n1T, n2T)
    xs = P.sbuf("xs", [128, 8, 512], F32); bxs = Buf("xs")
    x1 = P.sbuf("x1", [128, 8, 512], F32); bx1 = Buf("x1")
    A = P.sbuf("A", [128, 8, 512], F32); bA = Buf("A")
    B = P.sbuf("B", [128, 8, 512], F32); bB = Buf("B")
    C = P.sbuf("C", [128, 8, 512], F32); bC = Buf("C")
    ob = P.sbuf("ob", [128, 8, 512], BF16); bob = Buf("ob")
    dsk = P.sbuf("dsk", [128, 8], F32); bdsk = Buf("dsk")
    P.dma("sync", dsk[:], dskT, writes=[bdsk])
    evs = []
    V = "vector"

    def body(t0, N, c):
        mod = L.mod[c]
        ld = lambda t, bt, src: P.dma("sync", t[:, :, 0:N], src[:, t0:t0 + N].rearrange("(k p) n -> p k n", p=128), writes=[bt])
        ld(xs, bxs, xT); ld(A, bA, hlT); ld(B, bB, yfT); ld(C, bC, ybT)
        for k in range(8):
            P.op(V, lambda e, k=k: e.scalar_tensor_tensor(out=A[:, k, 0:N], in0=A[:, k, 0:N], scalar=dsk[:, k:k + 1], in1=B[:, k, 0:N], op0=ALU.mult, op1=ALU.add), reads=[bA, bB, bdsk], writes=[bA])
            P.op(V, lambda e, k=k: e.tensor_tensor(out=A[:, k, 0:N], in0=A[:, k, 0:N], in1=C[:, k, 0:N], op=ALU.add), reads=[bA, bC], writes=[bA])
            s1, bs1 = L.scr[(2 * k) % 4]
            s2, bs2 = L.scr[(2 * k + 1) % 4]
            P.op("scalar", lambda e, k=k, s1=s1: e.activation(out=s1[:, 0:N], in_=A[:, k, 0:N], func=AF.Square), reads=[bA], writes=[bs1])
            P.op(V, lambda e, s1=s1: e.tensor_scalar(out=s1[:, 0:N], in0=s1[:, 0:N], scalar1=0.044715, scalar2=1.0, op0=ALU.mult, op1=ALU.add), reads=[bs1], writes=[bs1])
            P.op(V, lambda e, k=k, s1=s1: e.tensor_tensor(out=s1[:, 0:N], in0=s1[:, 0:N], in1=A[:, k, 0:N], op=ALU.mult), reads=[bs1, bA], writes=[bs1])
            P.op("scalar", lambda e, s1=s1, s2=s2: e.activation(out=s2[:, 0:N], in_=s1[:, 0:N], func=AF.Sigmoid, scale=1.5957691216057308), reads=[bs1], writes=[bs2])
            P.op(V, lambda e, k=k, s2=s2: e.tensor_tensor(out=ob[:, k, 0:N], in0=s2[:, 0:N], in1=A[:, k, 0:N], op=ALU.mult), reads=[bs2, bA], writes=[bob])
        for q in range(2):
            wv, bwv = L.load_w(w_glu, 0, 8, q * 512, 512)
            wg, bwg = L.load_w(w_glu, 0, 8, D + q * 512, 512)
            for m in range(4):
                k3 = q * 4 + m
                ps, bps = L.next_ps()
                L.mm(ps[:, 0:N], bps, [(wv[:, k, m * 128:(m + 1) * 128], ob[:, k, 0:N], [bwv, bob]) for k in range(8)])
                ps2, bps2 = L.next_ps()
                L.mm(ps2[:, 0:N], bps2, [(wg[:, k, m * 128:(m + 1) * 128], ob[:, k, 0:N], [bwg, bob]) for k in range(8)])
                sg, bsg = L.scr[k3 % 2]
                P.op("scalar", lambda e, sg=sg, ps2=ps2: e.activation(out=sg[:, 0:N], in_=ps2[:, 0:N], func=AF.Sigmoid), reads=[bps2], writes=[bsg])
                P.op(V, lambda e, sg=sg, ps=ps: e.tensor_tensor(out=sg[:, 0:N], in0=sg[:, 0:N], in1=ps[:, 0:N], op=ALU.mult), reads=[bsg, bps], writes=[bsg])
                P.op(V, lambda e, sg=sg, k3=k3: e.scalar_tensor_tensor(out=x1[:, k3, 0:N], in0=sg[:, 0:N], scalar=mod[:, 16 + k3:17 + k3], in1=xs[:, k3, 0:N], op0=ALU.mult, op1=ALU.add),
                     reads=[bsg, bxs, L.bmod], writes=[bx1])
        L.ffn_dense(x1, bx1, N, c, w_gu, w_down, x1, bx1)
        evs.append(P.dma("sync", outT[:, t0:t0 + N].rearrange("(k p) n -> p k n", p=128), x1[:, :, 0:N], reads=[bx1]))
    for i in range(ntiles):
        body(i * 512, 512, 0)
    _finish(P, evs, F)
    print("glu_ffn program: insts", P.ninst, "sems", P.nsem)
    return nc


def run_layer2(xfull, ctxfull, inp, progs):
    pfx = "l2_"
    c, c_ctx = inp["c"], inp["c_ctx"]
    maps = []
    for core in range(8):
        b, h = core // 2, core % 2
        m = ada_maps(inp, pfx, c, c_ctx, b)
        m.update({"xT": T(xfull[b, h * NT:(h + 1) * NT]), "cT": T(ctxfull[b])})
        maps.append(m)
    r1 = run_bass_kernel_spmd(progs["hl"], maps, core_ids=list(range(8))).results
    hlT = [np.concatenate([r1[2 * b]["hlT"], r1[2 * b + 1]["hlT"]], axis=1) for b in range(4)]
    hcT = [r1[2 * b]["hcT"] for b in range(4)]
    maps = []
    for k in range(8):
        fs = slice(128 * k, 128 * k + 128)
        uf = np.stack([np.concatenate([hcT[b][fs], hlT[b][fs]], axis=1) for b in range(4)])
        ub_ = np.stack([np.concatenate([hcT[b][fs][:, ::-1], hlT[b][fs][:, ::-1]], axis=1) for b in range(4)])
        m = s5_host_params(inp, pfx, k)
        m["u"] = np.ascontiguousarray(np.stack([uf, ub_], axis=0))
        maps.append(m)
    r2 = run_bass_kernel_spmd(progs["s5"], maps, core_ids=list(range(8))).results
    yf = [np.concatenate([r2[k]["y"][0, b] for k in range(8)], axis=0) for b in range(4)]
    yb = [np.concatenate([r2[k]["y"][1, b][:, ::-1] for k in range(8)], axis=0) for b in range(4)]
    maps = []
    for core in range(8):
        b, h = core // 2, core % 2
        sl = slice(h * NT, (h + 1) * NT)
        m = ada_maps(inp, pfx, c, c_ctx, b)
        m.update({"xT": T(xfull[b, sl]), "hlT": np.ascontiguousarray(hlT[b][:, sl]), "yfT": np.ascontiguousarray(yf[b][:, sl]),
                  "ybT": np.ascontiguousarray(yb[b][:, sl]), "dskT": fm_vec(inp[pfx + "ssm_d"], 8),
                  "w_glu": inp[pfx + "ssm_w_glu"], "w_gu": inp[pfx + "ffn_w_gu"], "w_down": inp[pfx + "ffn_w_down"]})
        maps.append(m)
    r3 = run_bass_kernel_spmd(progs["glu_ffn"], maps, core_ids=list(range(8))).results
    xo = np.empty_like(xfull)
    for core in range(8):
        b, h = core // 2, core % 2
        xo[b, h * NT:(h + 1) * NT] = r3[core]["outT"].T
    return xo

NT = 8192
_FUSED = {}


def build_fused():
    global NT
    NT = 8192
    nc = bass.Bass("TRN2", target_bir_lowering=False)
    sh = Shared(nc)
    ext = {}

    def E(n, s, d=F32):
        ext[n] = nc.dram_tensor(n, list(s), d, kind="ExternalInput").ap()
        return ext[n]

    def I(n, s, d=F32):
        return nc.dram_tensor(n, list(s), d, kind="Internal").ap()
    xT = E("xT", [D, NT + 2]); cT = E("cT", [D, NCTX + 2])
    condT = E("condT", [D, 2]); mask = E("mask", [128, 4])
    ropeC = E("ropeC", [128, NT]); ropeS = E("ropeS", [128, NT])
    lay = []
    for i in range(4):
        p = "l%d_" % i
        lay.append({"ada_w": E(p + "ada_w", [D, 6 * D]), "ada_bT": E(p + "ada_bT", [128, 48]), "n1T": E(p + "n1T", [128, 8]), "n2T": E(p + "n2T", [128, 8]), "condT": condT})
    for i in (0, 3):
        p = "l%d_" % i
        lay[i].update({"conv_wT": E(p + "conv_wT", [128, 3, 8]), "w_in": E(p + "w_in", [D, 3 * D]), "w_out": E(p + "w_out", [D, D]), "mask": mask})
    for i in (0, 2):
        p = "l%d_" % i
        lay[i].update({"w_gu": E(p + "w_gu", [D, 2 * DFF]), "w_down": E(p + "w_down", [DFF, D])})
    for i in (1, 3):
        p = "l%d_" % i
        lay[i].update({"w_gu": E(p + "w_gu", [8, D, 2 * DFF]), "w_down": E(p + "w_down", [8, DFF, D]), "router": E(p + "router", [D, 8])})
    lay[1].update({"w_qkv": E("l1_w_qkv", [D, 3 * D]), "w_perm": E("l1_w_perm", [D, 2 * D]), "w_o": E("l1_w_o", [D, D]),
                   "lamb": E("l1_lamb", [128, 4, HD]), "sublnT": E("l1_sublnT", [128, 1]), "ropeC": ropeC, "ropeS": ropeS})
    prm_s = E("l2_prm_s", [8, 128, 3, 8]); prm_f = E("l2_prm_f", [8, 128, 5, 2, 64]); ctp = E("l2_ct", [8, 128, 2, 8, 16]); gmask = E("l2_gmask", [128, 8])
    lay[2].update({"dskT": E("l2_dskT", [128, 8]), "w_glu": E("l2_w_glu", [D, 2 * D])})
    fnT = E("fnT", [128, 8])
    out = nc.dram_tensor("outT", [D, NT], F32, kind="ExternalOutput").ap()
    x1T = I("x1T", [D, NT]); c1T = I("c1T", [D, NCTX])
    qT = I("qT", [D, NT], BF16); qcT = I("qcT", [D, NCTX], BF16); kall = I("kall", [D, NCTX + NT], BF16); v_tm = I("v_tm", [NCTX + NT, D], BF16)
    oT = I("oT", [D, NT], BF16); ocT = I("ocT", [D, NCTX], BF16)
    x2T = I("x2T", [D, NT]); c2T = I("c2T", [D, NCTX])
    hT = I("hT", [D, NCTX + NT]); yS = I("yS", [2, D, NT])
    x3e = I("x3e", [D, NT + 2])

    P = Prog(nc, sh)
    keys = [Buf("wk%d" % i) for i in range(4)]
    kc = [0]

    def precast(layer, name, chunks=1):
        src = lay[layer][name]
        dst = nc.dram_tensor("bf_l%d_%s" % (layer, name), list(src.shape), BF16, kind="Internal").ap()
        if len(src.shape) == 3:
            parts = [(dst[e], src[e]) for e in range(src.shape[0])]
        else:
            parts = [(dst, src)]
        for d_, s_ in parts:
            R_ = d_.shape[0]
            for r0 in range(0, R_, 256):
                r1 = min(R_, r0 + 256)
                k = keys[kc[0] % 4]
                kc[0] += 1
                P.dma("gpsimd", d_[r0:r1], s_[r0:r1], key=k, writes=[k])
        lay[layer][name] = dst
    for layer, names in ((0, ("w_in", "w_out", "w_gu", "w_down")), (1, ("w_qkv", "w_perm", "w_o", "w_gu", "w_down")),
                         (2, ("w_glu", "w_gu", "w_down")), (3, ("w_in", "w_out", "w_gu", "w_down"))):
        for n in names:
            precast(layer, n)
    P.barrier()
    P.emit(final=False)

    def st(io_extra, layer):
        io = dict(lay[layer])
        io.update(io_extra)
        return Fuse(nc, sh, io)
    P = Prog(nc, sh)
    z = P.sbuf("zz", [128, 8, 1], F32); bz = Buf("zz")
    P.op("vector", lambda e: e.memset(z[:], 0.0), writes=[bz])
    for ci, col in enumerate((0, NT + 1)):
        dst = x3e[:, col:col + 1].rearrange("(k p) n -> p k n", p=128)
        P.dma("sync", None, None, reads=[bz], key=Buf("zk%d" % ci), fn=lambda e, dst=dst: e.dma_start(out=dst, in_=z[:], allow_slow_non_contiguous=True))
    P.barrier()
    P.emit(final=False)
    build_conv_layer(moe=False, with_ctx=True, F=st({"xT": xT, "cT": cT, "oT": x1T, "coT": c1T}, 0))
    build_qkv(F=st({"xT": x1T, "cT": c1T, "qT": qT, "qcT": qcT, "kall": kall, "v_tm": v_tm}, 1), fused=True)
    build_attn(0.8 - 0.6 * math.exp(-0.3 * 1), F=st({"qT": qT, "qcT": qcT, "kT": kall, "v_tm": v_tm, "oT": oT, "ocT": ocT}, 1), fused=True)
    build_oproj_ffn(moe=True, with_ctx=True, F=st({"xT": x1T, "cT": c1T, "oT": oT, "ocT": ocT, "outT": x2T, "coT": c2T}, 1))
    build_hl(F=st({"xT": x2T, "cT": c2T, "hT": hT}, 2), fused=True)
    for c in range(8):
        build_s5(F=Fuse(nc, sh, {"hTc": hT[c * 128:(c + 1) * 128, :], "ySc": yS[:, c * 128:(c + 1) * 128, :], "prm_s": prm_s[c], "prm_f": prm_f[c], "ct": ctp[c], "gmask": gmask}), fused=True)
    build_glu_ffn(F=st({"xT": x2T, "hlT": hT[:, NCTX:], "yfT": yS[0], "ybT": yS[1], "outT": x3e[:, 1:NT + 1]}, 2))
    build_conv_layer(moe=True, with_ctx=False, final_norm=True, F=st({"xT": x3e, "oT": out, "fnT": fnT}, 3))
    sh.close()
    print("fused program built; sems", sh.nsem)
    return nc


_INPUT_NAMES = (
    "x", "c", "ctx", "c_ctx",
    "l0_ada_w", "l0_ada_b", "l0_norm_mix", "l0_norm_ffn", "l0_conv_w_in", "l0_conv_w", "l0_conv_w_out", "l0_ffn_w_gu", "l0_ffn_w_down",
    "l1_ada_w", "l1_ada_b", "l1_norm_mix", "l1_norm_ffn", "l1_attn_w_qkv", "l1_attn_lam", "l1_attn_subln", "l1_attn_w_o", "l1_moe_router", "l1_moe_w_gu", "l1_moe_w_down",
    "l2_ada_w", "l2_ada_b", "l2_norm_mix", "l2_norm_ffn", "l2_ssm_a_re", "l2_ssm_a_im", "l2_ssm_log_dt", "l2_ssm_b_re", "l2_ssm_b_im", "l2_ssm_c_re", "l2_ssm_c_im", "l2_ssm_d", "l2_ssm_w_glu", "l2_ffn_w_gu", "l2_ffn_w_down",
    "l3_ada_w", "l3_ada_b", "l3_norm_mix", "l3_norm_ffn", "l3_conv_w_in", "l3_conv_w", "l3_conv_w_out", "l3_moe_router", "l3_moe_w_gu", "l3_moe_w_down",
    "final_norm",
)


def kernel(**inputs):
    inp = {k: np.ascontiguousarray(np.asarray(inputs[k])) for k in _INPUT_NAMES}
    if "nc" not in _FUSED:
        _FUSED["nc"] = build_fused()
    nc = _FUSED["nc"]
    x, ctx, c, c_ctx = inp["x"], inp["ctx"], inp["c"], inp["c_ctx"]
    perm, lo = rope_partner_perm()
    wq = inp["l1_attn_w_qkv"]
    colperm = np.concatenate([part * 1024 + h * 128 + perm for part in range(2) for h in range(8)])
    w_perm = np.ascontiguousarray(wq[:, colperm])
    C, S = rope_tables(0, NT)
    sp = [s5_host_params(inp, "l2_", k) for k in range(8)]
    shared = {
        "mask": np.ascontiguousarray(np.tile(np.array([[0.0, 0.0, 1.0, 0.0]], np.float32), (128, 1))),
        "ropeC": C, "ropeS": S,
        "l1_w_qkv": wq, "l1_w_perm": w_perm, "l1_w_o": inp["l1_attn_w_o"],
        "l1_lamb": np.ascontiguousarray(np.broadcast_to(inp["l1_attn_lam"][None], (128, 4, HD))).astype(np.float32),
        "l1_sublnT": np.ascontiguousarray(inp["l1_attn_subln"].reshape(128, 1)),
        "l2_prm_s": np.stack([p["prm_s"] for p in sp]), "l2_prm_f": np.stack([p["prm_f"] for p in sp]), "l2_ct": np.stack([p["ct"] for p in sp]), "l2_gmask": sp[0]["gmask"],
        "l2_dskT": fm_vec(inp["l2_ssm_d"], 8), "l2_w_glu": inp["l2_ssm_w_glu"],
        "fnT": fm_vec(inp["final_norm"], 8),
    }
    for i in range(4):
        p = "l%d_" % i
        shared.update({p + "ada_w": inp[p + "ada_w"], p + "ada_bT": fm_vec(inp[p + "ada_b"], 48), p + "n1T": fm_vec(inp[p + "norm_mix"], 8), p + "n2T": fm_vec(inp[p + "norm_ffn"], 8)})
    for i in (0, 3):
        p = "l%d_" % i
        shared.update({p + "conv_wT": np.ascontiguousarray(inp[p + "conv_w"].reshape(3, 8, 128).transpose(2, 0, 1)), p + "w_in": inp[p + "conv_w_in"], p + "w_out": inp[p + "conv_w_out"]})
    for i in (0, 2):
        p = "l%d_" % i
        shared.update({p + "w_gu": inp[p + "ffn_w_gu"], p + "w_down": inp[p + "ffn_w_down"]})
    for i in (1, 3):
        p = "l%d_" % i
        shared.update({p + "w_gu": inp[p + "moe_w_gu"], p + "w_down": inp[p + "moe_w_down"], p + "router": inp[p + "moe_router"]})
    active = [0, 1, 4, 5]
    big = [k for k, v in shared.items() if v.size >= (1 << 16)]
    idle = dict(shared)
    for k in big:
        idle[k] = np.zeros_like(shared[k])
    idle.update({"xT": np.zeros((D, NT + 2), np.float32), "cT": np.zeros((D, NCTX + 2), np.float32), "condT": np.zeros((D, 2), np.float32)})
    maps = []
    for core in range(8):
        if core not in active:
            maps.append(idle)
            continue
        b = active.index(core)
        xe = np.zeros((NT + 2, D), np.float32)
        xe[1:NT + 1] = x[b]
        ce = np.zeros((NCTX + 2, D), np.float32)
        ce[1:NCTX + 1] = ctx[b]
        m = dict(shared)
        m.update({"xT": np.ascontiguousarray(xe.T), "cT": np.ascontiguousarray(ce.T), "condT": np.ascontiguousarray(np.stack([c[b], c_ctx], axis=1))})
        maps.append(m)
    r = run_bass_kernel_spmd(nc, maps, core_ids=list(range(8))).results
    out = np.empty_like(x)
    for b in range(4):
        out[b] = r[active[b]]["outT"].T
    return out
```

```python
import contextlib
import numpy as np
import concourse.bass as bass
import concourse.mybir as mybir
from concourse.bass_utils import run_bass_kernel_spmd

F32 = mybir.dt.float32
BF16 = mybir.dt.bfloat16
I32 = mybir.dt.int32
AF = mybir.ActivationFunctionType
ALU = mybir.AluOpType
AX = mybir.AxisListType

ENGS = ["tensor", "vector", "scalar", "gpsimd", "sync"]


class Ev:
    __slots__ = ("sem", "val", "eng")

    def __init__(self, sem, val, eng):
        self.sem = sem
        self.val = val
        self.eng = eng


class Buf:
    __slots__ = ("name", "w", "r", "dsem", "dcnt")

    def __init__(self, name=""):
        self.name = name
        self.w = None
        self.r = []
        self.dsem = None
        self.dcnt = 0


class Shared:
    def __init__(self, nc):
        self.nc = nc
        self.es = contextlib.ExitStack()
        self.esem = {e: self.es.enter_context(nc.semaphore("s_" + e)) for e in ENGS}
        self.cnt = {e: 0 for e in ENGS}
        self.pool = []
        self.nstage = 0
        self.nsem = len(ENGS)

    def close(self):
        self.es.close()


class Prog:
    def __init__(self, nc, shared=None):
        self.nc = nc
        self.es = contextlib.ExitStack()
        self.own_shared = shared is None
        self.sh = shared if shared is not None else Shared(nc)
        self.q = {e: [] for e in ENGS}
        self.cnt = self.sh.cnt
        self.pending = {e: [] for e in ENGS}
        self.known = {e: {} for e in ENGS}
        self.esem = self.sh.esem
        self.ninst = 0
        self.dbufs = []
        self.pfx = "g%d_" % self.sh.nstage
        self.sh.nstage += 1

    @property
    def nsem(self):
        return self.sh.nsem

    def sbuf(self, name, shape, dtype):
        return self.es.enter_context(self.nc.sbuf_tensor(self.pfx + name, list(shape), dtype))

    def psum(self, name, shape, dtype=F32):
        return self.es.enter_context(self.nc.psum_tensor(self.pfx + name, list(shape), dtype))

    def newsem(self, buf):
        if self.sh.pool:
            h, c = self.sh.pool.pop()
        else:
            self.sh.nsem += 1
            h, c = self.sh.es.enter_context(self.nc.semaphore("d%d" % self.sh.nsem)), 0
        buf.dsem = h
        buf.dcnt = c
        self.dbufs.append(buf)

    @staticmethod
    def _flat(bs):
        out = []
        for b in bs:
            if isinstance(b, (list, tuple)):
                out.extend(Prog._flat(b))
            else:
                out.append(b)
        return out

    def _collect(self, eng, reads, writes):
        evs = []
        for b in reads:
            if b.w is not None:
                evs.append(b.w)
        for b in writes:
            if b.w is not None:
                evs.append(b.w)
            evs.extend(b.r)
        need = {}
        for ev in evs:
            if ev.eng == eng and eng == "tensor":
                continue
            if ev.val is None:
                raise RuntimeError("waiting on unclosed event (inc=False group not closed)")
            key = id(ev.sem)
            if self.known[eng].get(key, 0) >= ev.val:
                continue
            if key not in need or need[key][1] < ev.val:
                need[key] = (ev.sem, ev.val)
        waits = list(need.values())
        for s, v in waits:
            self.known[eng][id(s)] = v
        return waits

    def op(self, eng, fn, reads=(), writes=(), inc=True):
        reads = self._flat(reads)
        writes = self._flat(writes)
        waits = self._collect(eng, reads, writes)
        if inc:
            self.cnt[eng] += 1
            k = self.cnt[eng]
            ev = Ev(self.esem[eng], k, eng)
            for p in self.pending[eng]:
                p.val = k
            self.pending[eng] = []
        else:
            ev = Ev(self.esem[eng], None, eng)
            self.pending[eng].append(ev)
        sem = self.esem[eng]

        def thunk(e, waits=waits, fn=fn, inc=inc, sem=sem):
            for s, v in waits:
                e.wait_ge(s, v)
            ins = fn(e)
            if inc:
                ins.then_inc(sem, 1)
        self.q[eng].append(thunk)
        self.ninst += 1
        for b in writes:
            b.w = ev
            b.r = []
        for b in reads:
            b.r.append(ev)
            if len(b.r) > 64:
                b.r = self._prune(b.r)
        return ev

    def _prune(self, evs):
        best = {}
        for ev in evs:
            if ev.val is None:
                best[id(ev)] = ev
                continue
            key = id(ev.sem)
            if key not in best or best[key].val < ev.val:
                best[key] = ev
        return list(best.values())

    def dma(self, eng, out_ap, in_ap, reads=(), writes=(), key=None, fn=None):
        if key is None:
            key = writes[0] if writes else reads[0]
        reads = self._flat(reads)
        writes = self._flat(writes)
        if key.dsem is None:
            self.newsem(key)
        waits = self._collect(eng, reads, writes)
        key.dcnt += 16
        ev = Ev(key.dsem, key.dcnt, "dma")
        sem = key.dsem
        if fn is None:
            fn = lambda e: e.dma_start(out=out_ap, in_=in_ap)

        def thunk(e, waits=waits, sem=sem, fn=fn):
            for s, v in waits:
                e.wait_ge(s, v)
            fn(e).then_inc(sem, 16)
        self.q[eng].append(thunk)
        self.ninst += 1
        for b in writes:
            b.w = ev
            b.r = []
        for b in reads:
            b.r.append(ev)
        return ev

    def wait_all(self, eng, evs):
        need = {}
        for ev in evs:
            key = id(ev.sem)
            if key not in need or need[key][1] < ev.val:
                need[key] = (ev.sem, ev.val)
        waits = list(need.values())

        def thunk(e, waits=waits):
            for s, v in waits:
                e.wait_ge(s, v)
        self.q[eng].append(thunk)

    def barrier(self):
        waits = [(self.esem[e], self.cnt[e]) for e in ENGS if self.cnt[e] > 0]
        for b in self.dbufs:
            waits.append((b.dsem, b.dcnt))
        for eng in ENGS:
            def thunk(e, waits=waits):
                for s, v in waits:
                    e.wait_ge(s, v)
            self.q[eng].append(thunk)
        for b in self.dbufs:
            self.sh.pool.append((b.dsem, b.dcnt))
        self.dbufs = []

    def emit(self, final=True):
        nc = self.nc
        with nc.Block() as block:
            @block.tensor
            def _(e):
                for t in self.q["tensor"]:
                    t(e)

            @block.vector
            def _(e):
                for t in self.q["vector"]:
                    t(e)

            @block.scalar
            def _(e):
                for t in self.q["scalar"]:
                    t(e)

            @block.gpsimd
            def _(e):
                for t in self.q["gpsimd"]:
                    t(e)

            @block.sync
            def _(e):
                for t in self.q["sync"]:
                    t(e)
        self.es.close()
        if self.own_shared and final:
            self.sh.close()


D = 1024
KC = 8
DFF = 2816
FC = 22
EPS = 1e-6


class LB:
    def __init__(self, P, nw=5, wslot=4096):
        self.P = P
        self.nw = nw
        self.ps = [(P.psum("ps%d" % i, [128, 1024]), Buf("ps%d" % i)) for i in range(4)]
        self.psi = 0
        self.wr = [(P.sbuf("w%d" % i, [128, wslot], BF16), Buf("w%d" % i)) for i in range(nw)]
        self.wi = 0
        self.ones = P.sbuf("ones", [128, 128], BF16)
        self.bones = Buf("ones")
        P.op("vector", lambda e: e.memset(self.ones[:], 1.0), writes=[self.bones])
        self.dq = 0

    def next_ps(self):
        r = self.ps[self.psi % 4]
        self.psi += 1
        return r

    def load_w(self, W, k0, kch, c0, ncols):
        P = self.P
        t, b = self.wr[self.wi % self.nw]
        self.wi += 1
        view = t[:, 0:kch * ncols].rearrange("p (k n) -> p k n", k=kch)
        src = W[k0 * 128:(k0 + kch) * 128, c0:c0 + ncols].rearrange("(k p) n -> p k n", p=128)
        P.dma("sync" if W.dtype == BF16 else "gpsimd", view, src, writes=[b])
        return view, b

    def mm(self, out_ap, bout, pairs):
        P = self.P
        n = len(pairs)
        for i, (l, r, bufs) in enumerate(pairs):
            P.op("tensor",
                 lambda e, l=l, r=r, i=i: e.matmul(out_ap, lhsT=l, rhs=r, start=(i == 0), stop=(i == n - 1)),
                 reads=bufs, writes=[bout], inc=(i == n - 1))

    def mm_wide(self, ps, bps, W, pairs_fn):
        c0 = 0
        while c0 < W:
            c1 = min(W, c0 + 512)
            self.mm(ps[:, c0:c1], bps, pairs_fn(c0, c1))
            c0 = c1

    def setup_ada(self, condT, ada_w, ada_bT, norm1T, norm2T, ncond=2):
        P = self.P
        cs = P.sbuf("ada_cs", [128, 8, ncond], F32); bcs = Buf("ada_cs")
        csb = P.sbuf("ada_csb", [128, 8, ncond], BF16); bcsb = Buf("ada_csb")
        adab = P.sbuf("ada_b", [128, 48], F32); badab = Buf("ada_b")
        n1 = P.sbuf("n1", [128, 8], F32); bn1 = Buf("n1")
        n2 = P.sbuf("n2", [128, 8], F32); bn2 = Buf("n2")
        P.dma("sync", cs[:], condT.rearrange("(k p) c -> p k c", p=128), writes=[bcs])
        P.dma("sync", adab[:], ada_bT, writes=[badab])
        P.dma("sync", n1[:], norm1T, writes=[bn1])
        P.dma("sync", n2[:], norm2T, writes=[bn2])
        P.op("scalar", lambda e: e.activation(out=csb[:], in_=cs[:], func=AF.Silu), reads=[bcs], writes=[bcsb])
        psA, bpsA = self.next_ps()
        for cg in range(12):
            w, bw = self.load_w(ada_w, 0, 8, cg * 512, 512)
            for m in range(4):
                j = cg * 4 + m
                self.mm(psA[:, j * ncond:(j + 1) * ncond], bpsA,
                        [(w[:, k, m * 128:(m + 1) * 128], csb[:, k, :], [bw, bcsb]) for k in range(8)])
        self.mod = []
        self.gs1 = []
        self.gs2 = []
        self.bmod = Buf("mod")
        psv = psA[:, 0:48 * ncond].rearrange("p (j c) -> p j c", c=ncond)
        for c in range(ncond):
            mod = P.sbuf("mod%d" % c, [128, 48], F32)
            g1 = P.sbuf("gs1_%d" % c, [128, 8], F32)
            g2 = P.sbuf("gs2_%d" % c, [128, 8], F32)
            P.op("vector", lambda e, mod=mod, c=c: e.tensor_tensor(out=mod[:], in0=psv[:, :, c], in1=adab[:], op=ALU.add),
                 reads=[bpsA, badab], writes=[self.bmod])
            P.op("vector", lambda e, mod=mod, g1=g1: e.scalar_tensor_tensor(out=g1[:], in0=mod[:, 8:16], scalar=1.0, in1=n1[:], op0=ALU.add, op1=ALU.mult),
                 reads=[self.bmod, bn1], writes=[self.bmod])
            P.op("vector", lambda e, mod=mod, g2=g2: e.scalar_tensor_tensor(out=g2[:], in0=mod[:, 32:40], scalar=1.0, in1=n2[:], op0=ALU.add, op1=ALU.mult),
                 reads=[self.bmod, bn2], writes=[self.bmod])
            self.mod.append(mod)
            self.gs1.append(g1)
            self.gs2.append(g2)

    def alloc_common(self, WMAX=514):
        P = self.P
        self.WMAX = WMAX
        self.sq = P.sbuf("sq", [128, 8, WMAX], BF16); self.bsq = Buf("sq")
        self.rs = P.sbuf("rs", [128, WMAX], F32); self.brs = Buf("rs")
        self.tmp = [(P.sbuf("tmp%d" % i, [128, WMAX], F32), Buf("tmp%d" % i)) for i in range(2)]
        self.ti = 0
        self.hl = P.sbuf("hl", [128, 8, WMAX], BF16); self.bhl = Buf("hl")

    def alloc_scr(self, n):
        if not hasattr(self, "scr"):
            self.scr = []
        while len(self.scr) < n:
            i = len(self.scr)
            self.scr.append((self.P.sbuf("scr%d" % i, [128, self.WMAX], F32), Buf("scr%d" % i)))

    def next_tmp(self):
        r = self.tmp[self.ti % 2]
        self.ti += 1
        return r

    def rstd(self, xin, bxin, W):
        P = self.P
        sq, bsq, rs, brs = self.sq, self.bsq, self.rs, self.brs
        for k in range(8):
            P.op("scalar", lambda e, k=k: e.activation(out=sq[:, k, 0:W], in_=xin(k), func=AF.Square),
                 reads=[bxin], writes=[bsq])
        ps, bps = self.next_ps()
        self.mm_wide(ps, bps, W, lambda c0, c1: [(self.ones[:], sq[:, k, c0:c1], [self.bones, bsq]) for k in range(8)])
        P.op("vector", lambda e: e.tensor_scalar(out=rs[:, 0:W], in0=ps[:, 0:W], scalar1=1.0 / D, scalar2=EPS, op0=ALU.mult, op1=ALU.add),
             reads=[bps], writes=[brs])
        P.op("scalar", lambda e: e.activation(out=rs[:, 0:W], in_=rs[:, 0:W], func=AF.Sqrt), reads=[brs], writes=[brs])
        P.op("vector", lambda e: e.reciprocal(out=rs[:, 0:W], in_=rs[:, 0:W]), reads=[brs], writes=[brs])

    def norm_mod(self, xin, bxin, W, gs, shift, out=None, bout=None, out32=None, bout32=None):
        P = self.P
        rs, brs = self.rs, self.brs
        if out is None:
            out, bout = self.hl, self.bhl
        self.rstd(xin, bxin, W)
        for k in range(8):
            tmp, btmp = self.next_tmp()
            P.op("vector", lambda e, k=k, tmp=tmp: e.scalar_tensor_tensor(out=tmp[:, 0:W], in0=xin(k), scalar=gs[:, k:k + 1], in1=rs[:, 0:W], op0=ALU.mult, op1=ALU.mult),
                 reads=[bxin, brs, self.bmod], writes=[btmp])
            if out32 is None:
                P.op("scalar", lambda e, k=k, tmp=tmp: e.activation(out=out[:, k, 0:W], in_=tmp[:, 0:W], func=AF.Identity, bias=shift[:, k:k + 1], scale=1.0),
                     reads=[btmp, self.bmod], writes=[bout])
            else:
                P.op("scalar", lambda e, k=k, tmp=tmp: e.activation(out=out32[:, k, 0:W], in_=tmp[:, 0:W], func=AF.Identity, bias=shift[:, k:k + 1], scale=1.0),
                     reads=[btmp, self.bmod], writes=[bout32])
                P.op("vector", lambda e, k=k: e.tensor_copy(out=out[:, k, 0:W], in_=out32[:, k, 0:W]), reads=[bout32], writes=[bout])

    def final_norm(self, x1, bx1, N, fn, bfn, xo, bxo):
        P = self.P
        rs, brs = self.rs, self.brs
        self.rstd(lambda k: x1[:, k, 0:N], bx1, N)
        for k in range(8):
            P.op("vector", lambda e, k=k: e.scalar_tensor_tensor(out=xo[:, k, 0:N], in0=x1[:, k, 0:N], scalar=fn[:, k:k + 1], in1=rs[:, 0:N], op0=ALU.mult, op1=ALU.mult),
                 reads=[bx1, brs, bfn], writes=[bxo])

    def alloc_conv(self):
        P = self.P
        W = self.WMAX
        self.alloc_scr(6)
        self.zc = self.scr[0:2]
        self.z = self.scr[2:4]
        self.yc = self.scr[4:6]
        self.tt = P.sbuf("tt", [128, 8, 512], BF16); self.btt = Buf("tt")
        self.cw = P.sbuf("cw", [128, 3, 8], F32); self.bcw = Buf("cw")
        self.ci = 0

    def load_conv_w(self, conv_wT):
        self.P.dma("sync", self.cw[:], conv_wT, writes=[self.bcw])

    def conv_mixer(self, xs, bxs, N, c, w_in, w_out, maskL, maskR, bmask, x1, bx1):
        P = self.P
        W = N + 2
        mod = self.mod[c]
        self.norm_mod(lambda k: xs[:, k, 0:W], bxs, W, self.gs1[c], mod[:, 0:8])
        hl, bhl = self.hl, self.bhl
        tt, btt = self.tt, self.btt
        for q in range(2):
            wc, bwc = self.load_w(w_in, 0, 8, 1024 + q * 512, 512)
            wv, bwv = self.load_w(w_in, 0, 8, 2048 + q * 512, 512)
            wb, bwb = self.load_w(w_in, 0, 8, q * 512, 512)
            for m in range(4):
                k2 = q * 4 + m
                psC, bpsC = self.next_ps()
                self.mm_wide(psC, bpsC, W, lambda c0, c1: [(wc[:, k, m * 128:(m + 1) * 128], hl[:, k, c0:c1], [bwc, bhl]) for k in range(8)])
                psV, bpsV = self.next_ps()
                self.mm_wide(psV, bpsV, W, lambda c0, c1: [(wv[:, k, m * 128:(m + 1) * 128], hl[:, k, c0:c1], [bwv, bhl]) for k in range(8)])
                zc, bzc = self.zc[self.ci % 2]
                z, bz = self.z[self.ci % 2]
                yc, byc = self.yc[self.ci % 2]
                self.ci += 1
                P.op("scalar", lambda e, zc=zc, psC=psC: e.activation(out=zc[:, 0:W], in_=psC[:, 0:W], func=AF.Copy), reads=[bpsC], writes=[bzc])
                P.op("vector", lambda e, z=z, zc=zc, psV=psV: e.tensor_tensor(out=z[:, 0:W], in0=zc[:, 0:W], in1=psV[:, 0:W], op=ALU.mult), reads=[bzc, bpsV], writes=[bz])
                P.op("vector", lambda e, z=z: e.tensor_scalar(out=z[:, 0:1], in0=z[:, 0:1], scalar1=maskL, scalar2=None, op0=ALU.mult), reads=[bz, bmask], writes=[bz])
                P.op("vector", lambda e, z=z: e.tensor_scalar(out=z[:, W - 1:W], in0=z[:, W - 1:W], scalar1=maskR, scalar2=None, op0=ALU.mult), reads=[bz, bmask], writes=[bz])
                cw = self.cw
                P.op("vector", lambda e, z=z, yc=yc, k2=k2: e.tensor_scalar(out=yc[:, 0:N], in0=z[:, 0:N], scalar1=cw[:, 0, k2:k2 + 1], scalar2=None, op0=ALU.mult), reads=[bz, self.bcw], writes=[byc])
                P.op("vector", lambda e, z=z, yc=yc, k2=k2: e.scalar_tensor_tensor(out=yc[:, 0:N], in0=z[:, 1:N + 1], scalar=cw[:, 1, k2:k2 + 1], in1=yc[:, 0:N], op0=ALU.mult, op1=ALU.add), reads=[bz, byc, self.bcw], writes=[byc])
                P.op("vector", lambda e, z=z, yc=yc, k2=k2: e.scalar_tensor_tensor(out=yc[:, 0:N], in0=z[:, 2:N + 2], scalar=cw[:, 2, k2:k2 + 1], in1=yc[:, 0:N], op0=ALU.mult, op1=ALU.add), reads=[bz, byc, self.bcw], writes=[byc])
                psB, bpsB = self.next_ps()
                self.mm(psB[:, 0:N], bpsB, [(wb[:, k, m * 128:(m + 1) * 128], hl[:, k, 1:N + 1], [bwb, bhl]) for k in range(8)])
                P.op("vector", lambda e, yc=yc, psB=psB, k2=k2: e.tensor_tensor(out=tt[:, k2, 0:N], in0=yc[:, 0:N], in1=psB[:, 0:N], op=ALU.mult), reads=[byc, bpsB], writes=[btt])
        for q in range(2):
            wo, bwo = self.load_w(w_out, 0, 8, q * 512, 512)
            for m in range(4):
                k3 = q * 4 + m
                ps, bps = self.next_ps()
                self.mm(ps[:, 0:N], bps, [(wo[:, k, m * 128:(m + 1) * 128], tt[:, k, 0:N], [bwo, btt]) for k in range(8)])
                P.op("vector", lambda e, ps=ps, k3=k3: e.scalar_tensor_tensor(out=x1[:, k3, 0:N], in0=ps[:, 0:N], scalar=mod[:, 16 + k3:17 + k3], in1=xs[:, k3, 1:N + 1], op0=ALU.mult, op1=ALU.add),
                     reads=[bps, bxs, self.bmod], writes=[bx1])

    def alloc_ffn(self):
        P = self.P
        self.sg = [(P.sbuf("sg%d" % i, [128, 512], F32), Buf("sg%d" % i)) for i in range(2)]
        self.sgi = 0
        self.act = P.sbuf("act", [128, FC, 512], BF16); self.bact = Buf("act")

    def ffn_dense(self, x1, bx1, N, c, w_gu, w_down, x2, bx2):
        P = self.P
        mod = self.mod[c]
        self.norm_mod(lambda k: x1[:, k, 0:N], bx1, N, self.gs2[c], mod[:, 24:32])
        hl, bhl = self.hl, self.bhl
        act, bact = self.act, self.bact
        for jq in range(6):
            nch = 4 if jq < 5 else 2
            wg, bwg = self.load_w(w_gu, 0, 8, jq * 512, nch * 128)
            wu, bwu = self.load_w(w_gu, 0, 8, DFF + jq * 512, nch * 128)
            for m in range(nch):
                j = jq * 4 + m
                psG, bpsG = self.next_ps()
                self.mm(psG[:, 0:N], bpsG, [(wg[:, k, m * 128:(m + 1) * 128], hl[:, k, 0:N], [bwg, bhl]) for k in range(8)])
                psU, bpsU = self.next_ps()
                self.mm(psU[:, 0:N], bpsU, [(wu[:, k, m * 128:(m + 1) * 128], hl[:, k, 0:N], [bwu, bhl]) for k in range(8)])
                sg, bsg = self.sg[self.sgi % 2]
                self.sgi += 1
                P.op("scalar", lambda e, sg=sg, psG=psG: e.activation(out=sg[:, 0:N], in_=psG[:, 0:N], func=AF.Silu), reads=[bpsG], writes=[bsg])
                P.op("vector", lambda e, sg=sg, psU=psU, j=j: e.tensor_tensor(out=act[:, j, 0:N], in0=sg[:, 0:N], in1=psU[:, 0:N], op=ALU.mult), reads=[bsg, bpsU], writes=[bact])
        for dp in range(4):
            wa, bwa = self.load_w(w_down, 0, 11, dp * 256, 256)
            wb, bwb = self.load_w(w_down, 11, 11, dp * 256, 256)
            for h in range(2):
                d = dp * 2 + h
                ps, bps = self.next_ps()
                pairs = [(wa[:, f, h * 128:(h + 1) * 128], act[:, f, 0:N], [bwa, bact]) for f in range(11)]
                pairs += [(wb[:, f, h * 128:(h + 1) * 128], act[:, 11 + f, 0:N], [bwb, bact]) for f in range(11)]
                self.mm(ps[:, 0:N], bps, pairs)
                P.op("vector", lambda e, ps=ps, d=d: e.scalar_tensor_tensor(out=x2[:, d, 0:N], in0=ps[:, 0:N], scalar=mod[:, 40 + d:41 + d], in1=x1[:, d, 0:N], op0=ALU.mult, op1=ALU.add),
                     reads=[bps, bx1, self.bmod], writes=[bx2])

    def alloc_moe(self, router_dram):
        P = self.P
        self.alloc_scr(20)
        self.wr32 = P.sbuf("wr32", [128, 8, 8], F32); self.bwr32 = Buf("wr32")
        P.dma("sync", self.wr32[:], router_dram.rearrange("(k p) e -> p k e", p=128), writes=[self.bwr32])

    def ffn_moe(self, x1, bx1, N, c, w_gu, w_down, hl2f, bhl2f):
        P = self.P
        mod = self.mod[c]
        self.norm_mod(lambda k: x1[:, k, 0:N], bx1, N, self.gs2[c], mod[:, 24:32], out32=hl2f, bout32=bhl2f)
        hl, bhl = self.hl, self.bhl
        act, bact = self.act, self.bact
        wr32, bwr32 = self.wr32, self.bwr32
        Ls = []
        for e_ in range(8):
            ps, bps = self.ps[e_ // 2]
            o = (e_ % 2) * 512
            self.mm(ps[:, o:o + N], bps, [(wr32[:, k, e_:e_ + 1].to_broadcast([128, 128]), hl2f[:, k, 0:N], [bwr32, bhl2f]) for k in range(8)])
            Ls.append((ps[:, o:o + N], bps))
        self.psi = 0
        G = self.scr[0:8]
        LM = self.scr[8:16]
        m1, bm1 = self.scr[16]
        m2, bm2 = self.scr[17]
        w1, bw1 = self.scr[18]
        w2, bw2 = self.scr[19]
        TT = lambda o, a, b, op, r, w: P.op("vector", lambda e: e.tensor_tensor(out=o, in0=a, in1=b, op=op), reads=r, writes=w)
        P.op("scalar", lambda e: e.activation(out=m1[:, 0:N], in_=Ls[0][0], func=AF.Copy), reads=[Ls[0][1]], writes=[bm1])
        for e_ in range(1, 8):
            TT(m1[:, 0:N], m1[:, 0:N], Ls[e_][0], ALU.max, [bm1, Ls[e_][1]], [bm1])
        for e_ in range(8):
            g, bg = G[e_]
            lm, blm = LM[e_]
            TT(g[:, 0:N], Ls[e_][0], m1[:, 0:N], ALU.is_ge, [Ls[e_][1], bm1], [bg])
            P.op("vector", lambda e, g=g, lm=lm, e_=e_: e.scalar_tensor_tensor(out=lm[:, 0:N], in0=g[:, 0:N], scalar=-1e30, in1=Ls[e_][0], op0=ALU.mult, op1=ALU.add),
                 reads=[bg, Ls[e_][1]], writes=[blm])
        P.op("vector", lambda e: e.tensor_copy(out=m2[:, 0:N], in_=LM[0][0][:, 0:N]), reads=[LM[0][1]], writes=[bm2])
        for e_ in range(1, 8):
            TT(m2[:, 0:N], m2[:, 0:N], LM[e_][0][:, 0:N], ALU.max, [bm2, LM[e_][1]], [bm2])
        for e_ in range(8):
            lm, blm = LM[e_]
            TT(lm[:, 0:N], lm[:, 0:N], m2[:, 0:N], ALU.is_ge, [blm, bm2], [blm])
        TT(w1[:, 0:N], m1[:, 0:N], m2[:, 0:N], ALU.subtract, [bm1, bm2], [bw1])
        P.op("scalar", lambda e: e.activation(out=w1[:, 0:N], in_=w1[:, 0:N], func=AF.Sigmoid), reads=[bw1], writes=[bw1])
        P.op("vector", lambda e: e.tensor_scalar(out=w2[:, 0:N], in0=w1[:, 0:N], scalar1=-1.0, scalar2=1.0, op0=ALU.mult, op1=ALU.add), reads=[bw1], writes=[bw2])
        for e_ in range(8):
            g, bg = G[e_]
            lm, blm = LM[e_]
            TT(g[:, 0:N], g[:, 0:N], w1[:, 0:N], ALU.mult, [bg, bw1], [bg])
            TT(lm[:, 0:N], lm[:, 0:N], w2[:, 0:N], ALU.mult, [blm, bw2], [blm])
            TT(g[:, 0:N], g[:, 0:N], lm[:, 0:N], ALU.add, [bg, blm], [bg])
        for e_ in range(8):
            g, bg = G[e_]
            wgu = w_gu[e_]
            wdn = w_down[e_]
            for jq in range(6):
                nch = 4 if jq < 5 else 2
                wg, bwg = self.load_w(wgu, 0, 8, jq * 512, nch * 128)
                wu, bwu = self.load_w(wgu, 0, 8, DFF + jq * 512, nch * 128)
                for m in range(nch):
                    j = jq * 4 + m
                    psG, bpsG = self.next_ps()
                    self.mm(psG[:, 0:N], bpsG, [(wg[:, k, m * 128:(m + 1) * 128], hl[:, k, 0:N], [bwg, bhl]) for k in range(8)])
                    psU, bpsU = self.next_ps()
                    self.mm(psU[:, 0:N], bpsU, [(wu[:, k, m * 128:(m + 1) * 128], hl[:, k, 0:N], [bwu, bhl]) for k in range(8)])
                    sg, bsg = self.sg[self.sgi % 2]
                    self.sgi += 1
                    P.op("scalar", lambda e, sg=sg, psG=psG: e.activation(out=sg[:, 0:N], in_=psG[:, 0:N], func=AF.Silu), reads=[bpsG], writes=[bsg])
                    TT(sg[:, 0:N], sg[:, 0:N], g[:, 0:N], ALU.mult, [bsg, bg], [bsg])
                    TT(act[:, j, 0:N], sg[:, 0:N], psU[:, 0:N], ALU.mult, [bsg, bpsU], [bact])
            for dp in range(4):
                wa, bwa = self.load_w(wdn, 0, 11, dp * 256, 256)
                wb, bwb = self.load_w(wdn, 11, 11, dp * 256, 256)
                for h in range(2):
                    d = dp * 2 + h
                    ps, bps = self.next_ps()
                    pairs = [(wa[:, f, h * 128:(h + 1) * 128], act[:, f, 0:N], [bwa, bact]) for f in range(11)]
                    pairs += [(wb[:, f, h * 128:(h + 1) * 128], act[:, 11 + f, 0:N], [bwb, bact]) for f in range(11)]
                    self.mm(ps[:, 0:N], bps, pairs)
                    P.op("vector", lambda e, ps=ps, d=d: e.scalar_tensor_tensor(out=x1[:, d, 0:N], in0=ps[:, 0:N], scalar=mod[:, 40 + d:41 + d], in1=x1[:, d, 0:N], op0=ALU.mult, op1=ALU.add),
                         reads=[bps, bx1, self.bmod], writes=[bx1])


NT = 4096
NCTX = 256


class Fuse:
    def __init__(self, nc, sh, io):
        self.nc = nc
        self.sh = sh
        self.io = io


def _ctx(F):
    if F is None:
        nc = bass.Bass("TRN2", target_bir_lowering=False)
        dt = lambda n, s, k="ExternalInput", d=F32: nc.dram_tensor(n, list(s), d, kind=k).ap()
        return nc, None, dt

    def dt(n, s, k="ExternalInput", d=F32):
        ap = F.io[n]
        assert list(ap.shape) == list(s), (n, list(ap.shape), list(s))
        return ap
    return F.nc, F.sh, dt


def _finish(P, evs, F):
    if F is None:
        P.wait_all("sync", evs)
        P.emit()
    else:
        P.barrier()
        P.emit(final=False)


def build_conv_layer(moe=False, with_ctx=True, final_norm=False, ntiles=None, F=None, pre_hook=None):
    nc, sh, dt = _ctx(F)
    if ntiles is None:
        ntiles = NT // 512
    xT = dt("xT", [D, NT + 2])
    condT = dt("condT", [D, 2])
    ada_w = dt("ada_w", [D, 6 * D])
    ada_bT = dt("ada_bT", [128, 48])
    n1T = dt("n1T", [128, 8])
    n2T = dt("n2T", [128, 8])
    conv_wT = dt("conv_wT", [128, 3, 8])
    w_in = dt("w_in", [D, 3 * D])
    w_out = dt("w_out", [D, D])
    mask = dt("mask", [128, 4])
    if with_ctx:
        cT = dt("cT", [D, NCTX + 2])
        coT = dt("coT", [D, NCTX], "ExternalOutput")
    if moe:
        w_gu = dt("w_gu", [8, D, 2 * DFF])
        w_down = dt("w_down", [8, DFF, D])
        router = dt("router", [D, 8])
    else:
        w_gu = dt("w_gu", [D, 2 * DFF])
        w_down = dt("w_down", [DFF, D])
    oT = dt("oT", [D, NT], "ExternalOutput")

    P = Prog(nc, sh)
    if pre_hook is not None:
        pre_hook(P)
    L = LB(P, nw=4)
    L.alloc_common(514)
    L.alloc_conv()
    L.alloc_ffn()
    msk = P.sbuf("msk", [128, 4], F32); bmsk = Buf("msk")
    P.dma("sync", msk[:], mask, writes=[bmsk])
    L.load_conv_w(conv_wT)
    L.setup_ada(condT, ada_w, ada_bT, n1T, n2T)
    xs = P.sbuf("xs", [128, 8, 514], F32); bxs = Buf("xs")
    x1 = P.sbuf("x1", [128, 8, 512], F32); bx1 = Buf("x1")
    if moe:
        L.alloc_moe(router)
    if final_norm:
        fnT = dt("fnT", [128, 8])
        fn = P.sbuf("fn", [128, 8], F32); bfn = Buf("fn")
        P.dma("sync", fn[:], fnT, writes=[bfn])
    outs = []
    tiles = []
    if with_ctx:
        tiles.append((cT, coT, 0, NCTX, 1, msk[:, 3:4], msk[:, 3:4]))
    for i in range(ntiles):
        mL = msk[:, 0:1] if i == 0 else msk[:, 2:3]
        mR = msk[:, 1:2] if i == NT // 512 - 1 else msk[:, 2:3]
        tiles.append((xT, oT, i * 512, 512, 0, mL, mR))
    for (src, dst, t0, N, c, mL, mR) in tiles:
        W = N + 2
        P.dma("sync", xs[:, :, 0:W], src[:, t0:t0 + W].rearrange("(k p) n -> p k n", p=128), writes=[bxs])
        L.conv_mixer(xs, bxs, N, c, w_in, w_out, mL, mR, bmsk, x1, bx1)
        if moe:
            L.ffn_moe(x1, bx1, N, c, w_gu, w_down, xs, bxs)
        else:
            L.ffn_dense(x1, bx1, N, c, w_gu, w_down, x1, bx1)
        if final_norm:
            L.final_norm(x1, bx1, N, fn, bfn, xs, bxs)
            ev = P.dma("sync", dst[:, t0:t0 + N].rearrange("(k p) n -> p k n", p=128), xs[:, :, 0:N], reads=[bxs])
        else:
            ev = P.dma("sync", dst[:, t0:t0 + N].rearrange("(k p) n -> p k n", p=128), x1[:, :, 0:N], reads=[bx1])
        outs.append(ev)
    _finish(P, outs, F)
    print("conv layer program: insts", P.ninst, "sems", P.nsem)
    return nc


def fm_vec(v, n):
    return np.ascontiguousarray(v.reshape(n, 128).T)


def host_inputs_conv(xfull, ctxfull, c, c_ctx, pfx, inp, with_ctx=True):
    maps = []
    for core in range(8):
        b, h = core // 2, core % 2
        xe = np.zeros((NT + 2, D), np.float32)
        lo = h * NT - 1
        hi = h * NT + NT + 1
        slo, shi = max(lo, 0), min(hi, xfull.shape[1])
        xe[slo - lo:shi - lo] = xfull[b, slo:shi]
        m = {
            "xT": np.ascontiguousarray(xe.T),
            "condT": np.ascontiguousarray(np.stack([c[b], c_ctx], axis=1)),
            "ada_w": inp[pfx + "ada_w"], "ada_bT": fm_vec(inp[pfx + "ada_b"], 48),
            "n1T": fm_vec(inp[pfx + "norm_mix"], 8), "n2T": fm_vec(inp[pfx + "norm_ffn"], 8),
            "conv_wT": np.ascontiguousarray(inp[pfx + "conv_w"].reshape(3, 8, 128).transpose(2, 0, 1)),
            "w_in": inp[pfx + "conv_w_in"], "w_out": inp[pfx + "conv_w_out"],
            "mask": np.ascontiguousarray(np.tile(np.array([[float(h == 1), float(h == 0), 1.0, 0.0]], np.float32), (128, 1))),
        }
        if with_ctx:
            ce = np.zeros((NCTX + 2, D), np.float32)
            ce[1:NCTX + 1] = ctxfull[b]
            m["cT"] = np.ascontiguousarray(ce.T)
        maps.append(m)
    return maps

import math

NK = 8448
NKT = 66
HD = 64


def rope_partner_perm():
    p = np.arange(128)
    d = p % 64
    lo = (d % 32) < 16
    return np.where(lo, p + 16, p - 16), lo


def rope_tables(tok0, n):
    t = np.arange(tok0, tok0 + n)
    row = (t // 64).astype(np.float32)
    col = (t % 64).astype(np.float32)
    inv = (np.float32(10000.0) ** (-np.arange(16, dtype=np.float32) / np.float32(16))).astype(np.float32)
    ang = np.concatenate([row[:, None] * inv, col[:, None] * inv], axis=-1).astype(np.float32)
    p = np.arange(128)
    d = p % 64
    lo = (d % 32) < 16
    fi = (d % 16) + 16 * (d >= 32)
    C = np.cos(ang[:, fi]).T.astype(np.float32)
    S = np.sin(ang[:, fi]).T.astype(np.float32)
    S = np.where(lo[:, None], -S, S).astype(np.float32)
    return np.ascontiguousarray(C), np.ascontiguousarray(S)


def build_qkv(F=None, fused=False):
    nc, sh, dt = _ctx(F)
    xT = dt("xT", [D, NT]); cT = dt("cT", [D, NCTX])
    condT = dt("condT", [D, 2]); ada_w = dt("ada_w", [D, 6 * D]); ada_bT = dt("ada_bT", [128, 48])
    n1T = dt("n1T", [128, 8]); n2T = dt("n2T", [128, 8])
    w_qkv = dt("w_qkv", [D, 3 * D]); w_perm = dt("w_perm", [D, 2 * D])
    ropeC = dt("ropeC", [128, NT]); ropeS = dt("ropeS", [128, NT])
    if fused:
        kall = dt("kall", [D, NCTX + NT], "ExternalOutput", BF16)
        v_tm = dt("v_tm", [NCTX + NT, D], "ExternalOutput", BF16)
        outs_l = [dt("qT", [D, NT], "ExternalOutput", BF16), kall[:, NCTX:], v_tm[NCTX:, :]]
        outs_c = [dt("qcT", [D, NCTX], "ExternalOutput", BF16), kall[:, 0:NCTX], v_tm[0:NCTX, :]]
    else:
        outs_l = [dt(n, [D, NT], "ExternalOutput", BF16) for n in ("qT", "kT", "vT")]
        outs_c = [dt(n, [D, NCTX], "ExternalOutput", BF16) for n in ("qcT", "kcT", "vcT")]
    P = Prog(nc, sh)
    L = LB(P, nw=4)
    L.alloc_common(512)
    L.alloc_scr(4)
    L.setup_ada(condT, ada_w, ada_bT, n1T, n2T)
    xs = P.sbuf("xs", [128, 8, 512], F32); bxs = Buf("xs")
    rc = P.sbuf("rc", [128, 512], F32); brc = Buf("rc")
    rsn = P.sbuf("rsn", [128, 512], F32); brsn = Buf("rsn")
    ob = [(P.sbuf("ob%d" % i, [128, 8, 512], BF16), Buf("ob%d" % i)) for i in range(3)]
    if fused:
        vb = P.sbuf("vb", [128, 4, 1024], BF16); bvb = Buf("vb")
    evs = []
    tiles = [(cT, outs_c, 0, NCTX, 1, False)] + [(xT, outs_l, i * 512, 512, 0, True) for i in range(NT // 512)]
    def body(src, dsts, t0, N, c, rope):
        P.dma("sync", xs[:, :, 0:N], src[:, t0:t0 + N].rearrange("(k p) n -> p k n", p=128), writes=[bxs])
        if rope:
            P.dma("sync", rc[:, 0:N], ropeC[:, t0:t0 + N], writes=[brc])
            P.dma("sync", rsn[:, 0:N], ropeS[:, t0:t0 + N], writes=[brsn])
        L.norm_mod(lambda k: xs[:, k, 0:N], bxs, N, L.gs1[c], L.mod[c][:, 0:8])
        hl, bhl = L.hl, L.bhl
        for part in range(3):
            o, bo = ob[part]
            if fused and part == 2:
                for q in range(2):
                    w, bw = L.load_w(w_qkv, 0, 8, 2048 + q * 512, 512)
                    for blk in range(N // 128):
                        ps, bps = L.next_ps()
                        L.mm(ps[:, 0:512], bps, [(hl[:, k, blk * 128:(blk + 1) * 128], w[:, k, :], [bw, bhl]) for k in range(8)])
                        P.op("scalar", lambda e, ps=ps, blk=blk, q=q: e.activation(out=vb[:, blk, q * 512:(q + 1) * 512], in_=ps[:, 0:512], func=AF.Copy), reads=[bps], writes=[bvb])
                nb_ = N // 128
                evs.append(P.dma("sync", dsts[2][t0:t0 + N, :].rearrange("(b p) c -> p b c", p=128), vb[:, 0:nb_, :], reads=[bvb]))
                continue
            for q in range(2):
                w, bw = L.load_w(w_qkv, 0, 8, part * 1024 + q * 512, 512)
                if rope and part < 2:
                    wp, bwp = L.load_w(w_perm, 0, 8, part * 1024 + q * 512, 512)
                for m in range(4):
                    k2 = q * 4 + m
                    ps, bps = L.next_ps()
                    L.mm(ps[:, 0:N], bps, [(w[:, k, m * 128:(m + 1) * 128], hl[:, k, 0:N], [bw, bhl]) for k in range(8)])
                    if rope and part < 2:
                        ps2, bps2 = L.next_ps()
                        L.mm(ps2[:, 0:N], bps2, [(wp[:, k, m * 128:(m + 1) * 128], hl[:, k, 0:N], [bwp, bhl]) for k in range(8)])
                        t1, bt1 = L.scr[(2 * k2) % 4]
                        t2, bt2 = L.scr[(2 * k2 + 1) % 4]
                        P.op("vector", lambda e, t1=t1, ps=ps: e.tensor_tensor(out=t1[:, 0:N], in0=ps[:, 0:N], in1=rc[:, 0:N], op=ALU.mult), reads=[bps, brc], writes=[bt1])
                        P.op("vector", lambda e, t2=t2, ps2=ps2: e.tensor_tensor(out=t2[:, 0:N], in0=ps2[:, 0:N], in1=rsn[:, 0:N], op=ALU.mult), reads=[bps2, brsn], writes=[bt2])
                        P.op("vector", lambda e, t1=t1, t2=t2, o=o, k2=k2: e.tensor_tensor(out=o[:, k2, 0:N], in0=t1[:, 0:N], in1=t2[:, 0:N], op=ALU.add), reads=[bt1, bt2], writes=[bo])
                    else:
                        P.op("scalar", lambda e, ps=ps, o=o, k2=k2: e.activation(out=o[:, k2, 0:N], in_=ps[:, 0:N], func=AF.Copy), reads=[bps], writes=[bo])
            evs.append(P.dma("sync", dsts[part][:, t0:t0 + N].rearrange("(k p) n -> p k n", p=128), o[:, :, 0:N], reads=[bo]))
    for tl in tiles:
        body(*tl)
    _finish(P, evs, F)
    print("qkv program: insts", P.ninst, "sems", P.nsem)
    return nc


def build_attn(lam_init, F=None, fused=False):
    nc, sh, dt = _ctx(F)
    qT = dt("qT", [D, NT], d=BF16)
    qcT = dt("qcT", [D, NCTX], d=BF16)
    kT = dt("kT", [D, NK], d=BF16)
    if fused:
        v_tm = dt("v_tm", [NK, D], d=BF16)
    else:
        vr = dt("vr", [8, 128, NKT * 128], d=BF16)
    lamb = dt("lamb", [128, 4, HD])
    sublnT = dt("sublnT", [128, 1])
    oT = dt("oT", [D, NT], "ExternalOutput", BF16)
    ocT = dt("ocT", [D, NCTX], "ExternalOutput", BF16)
    P = Prog(nc, sh)
    ones = P.sbuf("ones", [128, 128], BF16); bones = Buf("ones")
    P.op("vector", lambda e: e.memset(ones[:], 1.0), writes=[bones])
    lv = P.sbuf("lv", [128, 4, HD], F32); blv = Buf("lv")
    sl = P.sbuf("sl", [128, 1], F32); bsl = Buf("sl")
    P.dma("sync", lv[:], lamb, writes=[blv])
    P.dma("sync", sl[:], sublnT, writes=[bsl])
    pr = P.sbuf("pr", [128, 2, HD], F32); bpr = Buf("pr")
    sm = P.sbuf("sm", [128, 2], F32); bsm = Buf("sm")
    nlam = P.sbuf("nlam", [128, 1], F32); bnl = Buf("nlam")
    P.op("vector", lambda e: e.tensor_tensor(out=pr[:, 0, :], in0=lv[:, 0, :], in1=lv[:, 1, :], op=ALU.mult), reads=[blv], writes=[bpr])
    P.op("vector", lambda e: e.tensor_tensor(out=pr[:, 1, :], in0=lv[:, 2, :], in1=lv[:, 3, :], op=ALU.mult), reads=[blv, bpr], writes=[bpr])
    P.op("vector", lambda e: e.reduce_sum(out=sm[:], in_=pr[:], axis=AX.X), reads=[bpr], writes=[bsm])
    P.op("scalar", lambda e: e.activation(out=sm[:], in_=sm[:], func=AF.Exp), reads=[bsm], writes=[bsm])
    P.op("vector", lambda e: e.tensor_tensor(out=nlam[:], in0=sm[:, 1:2], in1=sm[:, 0:1], op=ALU.subtract), reads=[bsm], writes=[bnl])
    P.op("vector", lambda e: e.tensor_scalar(out=nlam[:], in0=nlam[:], scalar1=-float(lam_init), scalar2=None, op0=ALU.add), reads=[bnl], writes=[bnl])
    P.op("vector", lambda e: e.tensor_scalar(out=sl[:], in0=sl[:], scalar1=float(1.0 - lam_init), scalar2=None, op0=ALU.mult), reads=[bsl], writes=[bsl])

    psS = [(P.psum("pS%d" % i, [128, 512]), Buf("pS%d" % i)) for i in range(4)]
    psA = [(P.psum("pA%d" % i, [128, 512]), Buf("pA%d" % i)) for i in range(4)]
    E = [(P.sbuf("E%d" % i, [128, 512], BF16), Buf("E%d" % i)) for i in range(4)]
    kh = [(P.sbuf("kh%d" % i, [128, NK], BF16), Buf("kh%d" % i)) for i in range(2)]
    vh = [(P.sbuf("vh%d" % i, [128, NKT * 128], BF16), Buf("vh%d" % i)) for i in range(2)]
    qh = [(P.sbuf("qh%d" % i, [128, NT], BF16), Buf("qh%d" % i)) for i in range(2)]
    qch = [(P.sbuf("qch%d" % i, [128, NCTX], BF16), Buf("qch%d" % i)) for i in range(2)]
    f = [(P.sbuf("f%d" % i, [128, 512], F32), Buf("f%d" % i)) for i in range(4)]
    sqb = P.sbuf("sqb", [128, 512], BF16); bsqb = Buf("sqb")
    obuf = [(P.sbuf("obf%d" % i, [128, 512], BF16), Buf("obf%d" % i)) for i in range(2)]
    acc0 = [(P.sbuf("acc0_%d" % i, [128, 512], F32), Buf("acc0_%d" % i)) for i in range(2)]
    ones32 = P.sbuf("ones32", [128, 128], F32); bones32 = Buf("ones32")
    P.op("vector", lambda e: e.memset(ones32[:], 1.0), writes=[bones32])
    cnt = {"s": 0, "o": 0, "a": 0}
    evs = []

    def attend(h, q_ap, bq, N, kts, k_t, bk, v_t, bv, dst):
        nk = len(kts)

        def emit_S(i):
            kt = kts[i]
            res = []
            for m in range(2):
                ps, bps = psS[cnt["s"] % 4]
                e_, be = E[cnt["s"] % 4]
                cnt["s"] += 1
                lo = m * 64
                P.op("tensor", lambda e, ps=ps, lo=lo, kt=kt: e.matmul(ps[:, 0:N], lhsT=k_t[lo:lo + 64, kt * 128:(kt + 1) * 128], rhs=q_ap[lo:lo + 64, :], start=True, stop=True),
                     reads=[bk, bq], writes=[bps])
                P.op("scalar", lambda e, ps=ps, e_=e_: e.activation(out=e_[:, 0:N], in_=ps[:, 0:N], func=AF.Exp, scale=0.125), reads=[bps], writes=[be])
                res.append((e_, be))
            return res

        ac, bac = acc0[cnt["a"] % 2]
        cnt["a"] += 1
        cur = emit_S(0)
        for i in range(nk):
            nxt = emit_S(i + 1) if i + 1 < nk else None
            kt = kts[i]
            for m in range(2):
                e_, be = cur[m]
                po, bpo = psA[2 * m]
                pd, bpd = psA[2 * m + 1]
                P.op("tensor", lambda e, po=po, e_=e_, kt=kt, i=i: e.matmul(po[:, 0:N], lhsT=v_t[:, kt * 128:(kt + 1) * 128], rhs=e_[:, 0:N], start=(i == 0), stop=(i == nk - 1)),
                     reads=[bv, be], writes=[bpo], inc=(i == nk - 1))
                if m == 0:
                    if i == 0:
                        P.op("vector", lambda e, e_=e_: e.tensor_copy(out=ac[:, 0:N], in_=e_[:, 0:N]), reads=[be], writes=[bac])
                    else:
                        P.op("vector", lambda e, e_=e_: e.tensor_tensor(out=ac[:, 0:N], in0=ac[:, 0:N], in1=e_[:, 0:N], op=ALU.add), reads=[be, bac], writes=[bac])
                    if i == nk - 1:
                        P.op("tensor", lambda e, pd=pd: e.matmul(pd[:, 0:N], lhsT=ones32[:], rhs=ac[:, 0:N], start=True, stop=True), reads=[bones32, bac], writes=[bpd])
                else:
                    P.op("tensor", lambda e, pd=pd, e_=e_, i=i: e.matmul(pd[:, 0:N], lhsT=ones[:], rhs=e_[:, 0:N], start=(i == 0), stop=(i == nk - 1)),
                         reads=[bones, be], writes=[bpd], inc=True)
            cur = nxt
        (r0, br0), (r1, br1), (a, ba), (b, bb) = f
        P.op("vector", lambda e: e.reciprocal(out=r0[:, 0:N], in_=psA[1][0][:, 0:N]), reads=[psA[1][1]], writes=[br0])
        P.op("vector", lambda e: e.reciprocal(out=r1[:, 0:N], in_=psA[3][0][:, 0:N]), reads=[psA[3][1]], writes=[br1])
        P.op("vector", lambda e: e.tensor_tensor(out=a[:, 0:N], in0=psA[0][0][:, 0:N], in1=r0[:, 0:N], op=ALU.mult), reads=[psA[0][1], br0], writes=[ba])
        P.op("vector", lambda e: e.tensor_tensor(out=b[:, 0:N], in0=psA[2][0][:, 0:N], in1=r1[:, 0:N], op=ALU.mult), reads=[psA[2][1], br1], writes=[bb])
        P.op("vector", lambda e: e.scalar_tensor_tensor(out=a[:, 0:N], in0=b[:, 0:N], scalar=nlam[:, 0:1], in1=a[:, 0:N], op0=ALU.mult, op1=ALU.add), reads=[bb, ba, bnl], writes=[ba])
        P.op("scalar", lambda e: e.activation(out=sqb[:, 0:N], in_=a[:, 0:N], func=AF.Square), reads=[ba], writes=[bsqb])
        ps, bps = psS[cnt["s"] % 4]
        cnt["s"] += 1
        P.op("tensor", lambda e, ps=ps: e.matmul(ps[:, 0:N], lhsT=ones[:], rhs=sqb[:, 0:N], start=True, stop=True), reads=[bones, bsqb], writes=[bps])
        P.op("vector", lambda e, ps=ps: e.tensor_scalar(out=r0[:, 0:N], in0=ps[:, 0:N], scalar1=1.0 / 128, scalar2=EPS, op0=ALU.mult, op1=ALU.add), reads=[bps, br0], writes=[br0])
        P.op("scalar", lambda e: e.activation(out=r0[:, 0:N], in_=r0[:, 0:N], func=AF.Sqrt), reads=[br0], writes=[br0])
        P.op("vector", lambda e: e.reciprocal(out=r0[:, 0:N], in_=r0[:, 0:N]), reads=[br0], writes=[br0])
        ob_, bob = obuf[cnt["o"] % 2]
        cnt["o"] += 1
        P.op("vector", lambda e, ob_=ob_: e.scalar_tensor_tensor(out=ob_[:, 0:N], in0=a[:, 0:N], scalar=sl[:, 0:1], in1=r0[:, 0:N], op0=ALU.mult, op1=ALU.mult), reads=[ba, br0, bsl], writes=[bob])
        evs.append(P.dma("sync", dst, ob_[:, 0:N], reads=[bob]))

    for h in range(8):
        k_t, bk = kh[h % 2]
        v_t, bv = vh[h % 2]
        q_t, bq = qh[h % 2]
        qc_t, bqc = qch[h % 2]
        P.dma("sync", k_t[:], kT[h * 128:(h + 1) * 128, :], writes=[bk])
        if fused:
            P.dma("sync", v_t[:].rearrange("p (kt e) -> p kt e", e=128), v_tm[:, h * 128:(h + 1) * 128].rearrange("(kt p) e -> p kt e", p=128), writes=[bv])
        else:
            P.dma("sync", v_t[:], vr[h], writes=[bv])
        P.dma("sync", q_t[:], qT[h * 128:(h + 1) * 128, :], writes=[bq])
        P.dma("sync", qc_t[:], qcT[h * 128:(h + 1) * 128, :], writes=[bqc])
        attend(h, qc_t[:, 0:NCTX], bqc, NCTX, [0, 1], k_t, bk, v_t, bv, ocT[h * 128:(h + 1) * 128, :])
        for i in range(NT // 512):
            attend(h, q_t[:, i * 512:(i + 1) * 512], bq, 512, list(range(NKT)), k_t, bk, v_t, bv, oT[h * 128:(h + 1) * 128, i * 512:(i + 1) * 512])
    _finish(P, evs, F)
    print("attn program: insts", P.ninst, "sems", P.nsem)
    return nc


def build_oproj_ffn(moe=True, with_ctx=True, ntiles=None, in_bf16=True, name_w="w_o", glu=False, F=None):
    nc, sh, dt = _ctx(F)
    if ntiles is None:
        ntiles = NT // 512
    xT = dt("xT", [D, NT])
    oT = dt("oT", [D, NT], d=BF16 if in_bf16 else F32)
    if with_ctx:
        cT = dt("cT", [D, NCTX]); ocT = dt("ocT", [D, NCTX], d=BF16 if in_bf16 else F32)
        coT = dt("coT", [D, NCTX], "ExternalOutput")
    condT = dt("condT", [D, 2]); ada_w = dt("ada_w", [D, 6 * D]); ada_bT = dt("ada_bT", [128, 48])
    n1T = dt("n1T", [128, 8]); n2T = dt("n2T", [128, 8])
    w_o = dt("w_o", [D, 2 * D if glu else D])
    if moe:
        w_gu = dt("w_gu", [8, D, 2 * DFF]); w_down = dt("w_down", [8, DFF, D]); router = dt("router", [D, 8])
    else:
        w_gu = dt("w_gu", [D, 2 * DFF]); w_down = dt("w_down", [DFF, D])
    outT = dt("outT", [D, NT], "ExternalOutput")
    P = Prog(nc, sh)
    L = LB(P, nw=4)
    L.alloc_common(512)
    L.alloc_ffn()
    L.setup_ada(condT, ada_w, ada_bT, n1T, n2T)
    xs = P.sbuf("xs", [128, 8, 512], F32); bxs = Buf("xs")
    x1 = P.sbuf("x1", [128, 8, 512], F32); bx1 = Buf("x1")
    ob = P.sbuf("ob", [128, 8, 512], BF16); bob = Buf("ob")
    if not in_bf16:
        ob32 = P.sbuf("ob32", [128, 8, 512], F32); bob32 = Buf("ob32")
    if moe:
        L.alloc_moe(router)
    else:
        L.alloc_scr(2)
    evs = []
    tiles = []
    if with_ctx:
        tiles.append((cT, ocT, coT, 0, NCTX, 1))
    tiles += [(xT, oT, outT, i * 512, 512, 0) for i in range(ntiles)]
    def body(src, osrc, dst, t0, N, c):
        mod = L.mod[c]
        P.dma("sync", xs[:, :, 0:N], src[:, t0:t0 + N].rearrange("(k p) n -> p k n", p=128), writes=[bxs])
        if in_bf16:
            P.dma("sync", ob[:, :, 0:N], osrc[:, t0:t0 + N].rearrange("(k p) n -> p k n", p=128), writes=[bob])
        else:
            P.dma("sync", ob32[:, :, 0:N], osrc[:, t0:t0 + N].rearrange("(k p) n -> p k n", p=128), writes=[bob32])
            for k in range(8):
                P.op("scalar", lambda e, k=k: e.activation(out=ob[:, k, 0:N], in_=ob32[:, k, 0:N], func=AF.Gelu), reads=[bob32], writes=[bob])
        for q in range(2):
            if glu:
                wv, bwv = L.load_w(w_o, 0, 8, q * 512, 512)
                wg, bwg = L.load_w(w_o, 0, 8, D + q * 512, 512)
            else:
                wv, bwv = L.load_w(w_o, 0, 8, q * 512, 512)
            for m in range(4):
                k3 = q * 4 + m
                ps, bps = L.next_ps()
                L.mm(ps[:, 0:N], bps, [(wv[:, k, m * 128:(m + 1) * 128], ob[:, k, 0:N], [bwv, bob]) for k in range(8)])
                if glu:
                    ps2, bps2 = L.next_ps()
                    L.mm(ps2[:, 0:N], bps2, [(wg[:, k, m * 128:(m + 1) * 128], ob[:, k, 0:N], [bwg, bob]) for k in range(8)])
                    sg, bsg = L.scr[k3 % 2]
                    P.op("scalar", lambda e, sg=sg, ps2=ps2: e.activation(out=sg[:, 0:N], in_=ps2[:, 0:N], func=AF.Sigmoid), reads=[bps2], writes=[bsg])
                    P.op("vector", lambda e, sg=sg, ps=ps: e.tensor_tensor(out=sg[:, 0:N], in0=sg[:, 0:N], in1=ps[:, 0:N], op=ALU.mult), reads=[bsg, bps], writes=[bsg])
                    P.op("vector", lambda e, sg=sg, k3=k3: e.scalar_tensor_tensor(out=x1[:, k3, 0:N], in0=sg[:, 0:N], scalar=mod[:, 16 + k3:17 + k3], in1=xs[:, k3, 0:N], op0=ALU.mult, op1=ALU.add),
                         reads=[bsg, bxs, L.bmod], writes=[bx1])
                else:
                    P.op("vector", lambda e, ps=ps, k3=k3: e.scalar_tensor_tensor(out=x1[:, k3, 0:N], in0=ps[:, 0:N], scalar=mod[:, 16 + k3:17 + k3], in1=xs[:, k3, 0:N], op0=ALU.mult, op1=ALU.add),
                         reads=[bps, bxs, L.bmod], writes=[bx1])
        if moe:
            L.ffn_moe(x1, bx1, N, c, w_gu, w_down, xs, bxs)
        else:
            L.ffn_dense(x1, bx1, N, c, w_gu, w_down, x1, bx1)
        evs.append(P.dma("sync", dst[:, t0:t0 + N].rearrange("(k p) n -> p k n", p=128), x1[:, :, 0:N], reads=[bx1]))
    for tl in tiles:
        body(*tl)
    _finish(P, evs, F)
    print("oproj_ffn program: insts", P.ninst, "sems", P.nsem)
    return nc


def T(a):
    return np.ascontiguousarray(a.T)


def ada_maps(inp, pfx, c, c_ctx, b):
    return {"condT": np.ascontiguousarray(np.stack([c[b], c_ctx], axis=1)),
            "ada_w": inp[pfx + "ada_w"], "ada_bT": fm_vec(inp[pfx + "ada_b"], 48),
            "n1T": fm_vec(inp[pfx + "norm_mix"], 8), "n2T": fm_vec(inp[pfx + "norm_ffn"], 8)}


def run_layer1(xfull, ctxfull, inp, progs, ntiles=8):
    pfx = "l1_"
    c, c_ctx = inp["c"], inp["c_ctx"]
    perm, lo = rope_partner_perm()
    wq = inp[pfx + "attn_w_qkv"]
    colperm = np.concatenate([part * 1024 + h * 128 + perm for part in range(2) for h in range(8)])
    w_perm = np.ascontiguousarray(wq[:, colperm])
    maps = []
    for core in range(8):
        b, h = core // 2, core % 2
        C, S = rope_tables(h * NT, NT)
        m = ada_maps(inp, pfx, c, c_ctx, b)
        m.update({"xT": T(xfull[b, h * NT:(h + 1) * NT]), "cT": T(ctxfull[b]), "w_qkv": wq, "w_perm": w_perm, "ropeC": C, "ropeS": S})
        maps.append(m)
    r1 = run_bass_kernel_spmd(progs["qkv"], maps, core_ids=list(range(8))).results
    maps = []
    lamb = np.ascontiguousarray(np.broadcast_to(inp[pfx + "attn_lam"][None], (128, 4, HD))).astype(np.float32)
    for core in range(8):
        b, h = core // 2, core % 2
        kall = np.concatenate([r1[2 * b]["kcT"], r1[2 * b]["kT"], r1[2 * b + 1]["kT"]], axis=1)
        vall = np.concatenate([r1[2 * b]["vcT"], r1[2 * b]["vT"], r1[2 * b + 1]["vT"]], axis=1)
        vr = np.ascontiguousarray(vall.reshape(8, 128, NKT, 128).transpose(0, 3, 2, 1)).reshape(8, 128, NKT * 128)
        maps.append({"qT": r1[core]["qT"], "qcT": r1[core]["qcT"], "kT": np.ascontiguousarray(kall), "vr": vr,
                     "lamb": lamb, "sublnT": np.ascontiguousarray(inp[pfx + "attn_subln"].reshape(128, 1))})
    r2 = run_bass_kernel_spmd(progs["attn"], maps, core_ids=list(range(8))).results
    maps = []
    for core in range(8):
        b, h = core // 2, core % 2
        m = ada_maps(inp, pfx, c, c_ctx, b)
        m.update({"xT": T(xfull[b, h * NT:(h + 1) * NT]), "cT": T(ctxfull[b]), "oT": r2[core]["oT"], "ocT": r2[core]["ocT"],
                  "w_o": inp[pfx + "attn_w_o"], "w_gu": inp[pfx + "moe_w_gu"], "w_down": inp[pfx + "moe_w_down"], "router": inp[pfx + "moe_router"]})
        maps.append(m)
    r3 = run_bass_kernel_spmd(progs["oproj_moe"], maps, core_ids=list(range(8))).results
    xo = np.empty_like(xfull)
    co = np.empty_like(ctxfull)
    for core in range(8):
        b, h = core // 2, core % 2
        xo[b, h * NT:(h + 1) * NT] = r3[core]["outT"].T
        if h == 0:
            co[b] = r3[core]["coT"].T
    return xo, co, (r1, r2)


NS = 8448
TWO_PI = 2.0 * math.pi


def build_hl(with_ctx=True, F=None, fused=False):
    nc, sh, dt = _ctx(F)
    xT = dt("xT", [D, NT]); cT = dt("cT", [D, NCTX])
    condT = dt("condT", [D, 2]); ada_w = dt("ada_w", [D, 6 * D]); ada_bT = dt("ada_bT", [128, 48])
    n1T = dt("n1T", [128, 8]); n2T = dt("n2T", [128, 8])
    if fused:
        hT = dt("hT", [D, NCTX + NT], "ExternalOutput")
        hlT = hT[:, NCTX:]; hcT = hT[:, 0:NCTX]
    else:
        hlT = dt("hlT", [D, NT], "ExternalOutput"); hcT = dt("hcT", [D, NCTX], "ExternalOutput")
    P = Prog(nc, sh)
    L = LB(P, nw=2)
    L.alloc_common(512)
    L.setup_ada(condT, ada_w, ada_bT, n1T, n2T)
    xs = P.sbuf("xs", [128, 8, 512], F32); bxs = Buf("xs")
    ho = P.sbuf("ho", [128, 8, 512], F32); bho = Buf("ho")
    evs = []

    def body(src, dst, t0, N, c):
        P.dma("sync", xs[:, :, 0:N], src[:, t0:t0 + N].rearrange("(k p) n -> p k n", p=128), writes=[bxs])
        L.norm_mod(lambda k: xs[:, k, 0:N], bxs, N, L.gs1[c], L.mod[c][:, 0:8], out32=ho, bout32=bho)
        evs.append(P.dma("sync", dst[:, t0:t0 + N].rearrange("(k p) n -> p k n", p=128), ho[:, :, 0:N], reads=[bho]))
    body(cT, hcT, 0, NCTX, 1)
    for i in range(NT // 512):
        body(xT, hlT, i * 512, 512, 0)
    _finish(P, evs, F)
    print("hl program: insts", P.ninst, "sems", P.nsem)
    return nc


def build_s5(nb=4, ndg=8, F=None, fused=False):
    nc, sh, dt = _ctx(F)
    if fused:
        hTc = dt("hTc", [128, NS])
        ySc = dt("ySc", [2, 128, NS - NCTX], "ExternalOutput")
        nb = 1
    else:
        u = dt("u", [2, nb, 128, NS])
    prm_s = dt("prm_s", [128, 3, 8])
    prm_f = dt("prm_f", [128, 5, 2, 64])
    ct = dt("ct", [128, 2, 8, 16])
    gmask = dt("gmask", [128, 8])
    if not fused:
        y = dt("y", [2, nb, 128, NS - NCTX], "ExternalOutput")
    P = Prog(nc, sh)
    TT = lambda eng, o, a, b, op, r, w: P.op(eng, lambda e: e.tensor_tensor(out=o, in0=a, in1=b, op=op), reads=r, writes=w)
    STT = lambda eng, o, a, sc, b, op0, op1, r, w: P.op(eng, lambda e: e.scalar_tensor_tensor(out=o, in0=a, scalar=sc, in1=b, op0=op0, op1=op1), reads=r, writes=w)

    def TS(eng, o, a, s1, s2, op0, op1, r, w):
        if s2 is None:
            return P.op(eng, lambda e: e.tensor_scalar(out=o, in0=a, scalar1=s1, scalar2=None, op0=op0), reads=r, writes=w)
        return P.op(eng, lambda e: e.tensor_scalar(out=o, in0=a, scalar1=s1, scalar2=s2, op0=op0, op1=op1), reads=r, writes=w)
    ACT = lambda o, a, func, r, w, **kw: P.op("scalar", lambda e: e.activation(out=o, in_=a, func=func, **kw), reads=r, writes=w)
    V = "vector"
    G = "gpsimd"

    bprm = Buf("prm")
    ps_ = P.sbuf("ps_", [128, 3, 8], F32)
    pf_ = P.sbuf("pf_", [128, 5, 2, 64], F32)
    ct_ = P.sbuf("ct_", [128, 2, 8, 16], F32)
    gm_ = P.sbuf("gm_", [128, 8], F32)
    P.dma("sync", ps_[:], prm_s, writes=[bprm])
    P.dma("sync", pf_[:], prm_f, writes=[bprm])
    P.dma("sync", ct_[:], ct, writes=[bprm])
    P.dma("sync", gm_[:], gmask, writes=[bprm])
    hpi = P.sbuf("hpi", [128, 1], F32)
    bS = Buf("setup")
    P.op(V, lambda e: e.memset(hpi[:], math.pi / 2), writes=[bS])

    def sincos_inplace(eng, xs_, xc_, r, w):
        ki = xc_.bitcast(I32)
        TS(eng, ki, xs_, 1.0 / TWO_PI, None, ALU.mult, None, r, w)
        P.op(eng, lambda e: e.tensor_copy(out=xc_, in_=ki), reads=r, writes=w)
        STT(eng, xs_, xc_, -TWO_PI, xs_, ALU.mult, ALU.add, r, w)
        TS(eng, xs_, xs_, math.pi, -math.pi, ALU.min, ALU.max, r, w)
        ACT(xc_, xs_, AF.Abs, r, w)
        ACT(xs_, xs_, AF.Sin, r, w)
        ACT(xc_, xc_, AF.Sin, r, w, bias=hpi[:, 0:1], scale=-1.0)

    dts = P.sbuf("dts", [128, 8], F32)
    r_s = P.sbuf("r_s", [128, 8], F32)
    th_s = P.sbuf("th_s", [128, 8], F32)
    ki_s = P.sbuf("ki_s", [128, 8], I32)
    kf_s = P.sbuf("kf_s", [128, 8], F32)
    ACT(dts[:], ps_[:, 2, :], AF.Exp, [bprm], [bS])
    TT(V, r_s[:], dts[:], ps_[:, 0, :], ALU.mult, [bS, bprm], [bS])
    ACT(r_s[:], r_s[:], AF.Exp, [bS], [bS])
    TT(V, th_s[:], dts[:], ps_[:, 1, :], ALU.mult, [bS, bprm], [bS])
    TS(V, ki_s[:], th_s[:], 1.0 / TWO_PI, None, ALU.mult, None, [bS], [bS])
    P.op(V, lambda e: e.tensor_copy(out=kf_s[:], in_=ki_s[:]), reads=[bS], writes=[bS])
    STT(V, th_s[:], kf_s[:], -TWO_PI, th_s[:], ALU.mult, ALU.add, [bS], [bS])

    sh = [128, 2, 64]
    dtf = P.sbuf("dtf", sh, F32); mag = P.sbuf("mag", sh, F32)
    snf = P.sbuf("snf", sh, F32); csf = P.sbuf("csf", sh, F32)
    AR = pf_[:, 0]; AI = pf_[:, 1]; LDT = pf_[:, 2]; BTR = pf_[:, 3]; BTI = pf_[:, 4]
    ACT(dtf[:], LDT, AF.Exp, [bprm], [bS])
    TT(V, mag[:], dtf[:], AR, ALU.mult, [bS, bprm], [bS])
    ACT(mag[:], mag[:], AF.Exp, [bS], [bS])
    TT(V, snf[:], dtf[:], AI, ALU.mult, [bS, bprm], [bS])
    sincos_inplace(V, snf[:], csf[:], [bS], [bS])
    abr = P.sbuf("abr", sh, F32); abi = P.sbuf("abi", sh, F32); den = P.sbuf("den", sh, F32)
    t1 = P.sbuf("t1s", sh, F32); t2 = P.sbuf("t2s", sh, F32)
    cor = P.sbuf("cor", sh, F32); coi = P.sbuf("coi", sh, F32)
    TT(V, abr[:], mag[:], csf[:], ALU.mult, [bS], [bS])
    TT(V, abi[:], mag[:], snf[:], ALU.mult, [bS], [bS])
    TS(V, abr[:], abr[:], -1.0, None, ALU.add, None, [bS], [bS])
    TT(V, den[:], AR, AR, ALU.mult, [bprm, bS], [bS])
    TT(V, t1[:], AI, AI, ALU.mult, [bprm, bS], [bS])
    TT(V, den[:], den[:], t1[:], ALU.add, [bS], [bS])
    P.op(V, lambda e: e.reciprocal(out=den[:], in_=den[:]), reads=[bS], writes=[bS])
    TT(V, t1[:], abr[:], AR, ALU.mult, [bS, bprm], [bS])
    TT(V, t2[:], abi[:], AI, ALU.mult, [bS, bprm], [bS])
    TT(V, cor[:], t1[:], t2[:], ALU.add, [bS], [bS])
    TT(V, cor[:], cor[:], den[:], ALU.mult, [bS], [bS])
    TT(V, t1[:], abi[:], AR, ALU.mult, [bS, bprm], [bS])
    TT(V, t2[:], abr[:], AI, ALU.mult, [bS, bprm], [bS])
    TT(V, coi[:], t1[:], t2[:], ALU.subtract, [bS], [bS])
    TT(V, coi[:], coi[:], den[:], ALU.mult, [bS], [bS])
    bbr = P.sbuf("bbr", sh, F32); bbi = P.sbuf("bbi", sh, F32)
    TT(V, t1[:], cor[:], BTR, ALU.mult, [bS, bprm], [bS])
    TT(V, t2[:], coi[:], BTI, ALU.mult, [bS, bprm], [bS])
    TT(V, bbr[:], t1[:], t2[:], ALU.subtract, [bS], [bS])
    TT(V, t1[:], cor[:], BTI, ALU.mult, [bS, bprm], [bS])
    TT(V, t2[:], coi[:], BTR, ALU.mult, [bS, bprm], [bS])
    TT(V, bbi[:], t1[:], t2[:], ALU.add, [bS], [bS])
    WB = P.sbuf("WB", [128, 2, 4, 2, 128], F32)
    for d in range(2):
        for g in range(8):
            for ri, src in enumerate((bbr, bbi)):
                TS(V, WB[:, d, g // 2, ri, (g % 2) * 64:(g % 2 + 1) * 64], src[:, d, :], gm_[:, g:g + 1], None, ALU.mult, None, [bS, bprm], [bS])
    WC = P.sbuf("WC", [128, 2, 4, 2, 128], BF16)
    P.op(V, lambda e: e.memset(WC[:], 0.0), reads=[bS], writes=[bS])
    for d in range(2):
        for gp in range(4):
            for g2 in range(2):
                c0 = (2 * gp + g2) * 16
                lo = g2 * 64
                P.op(V, lambda e, d=d, gp=gp, c0=c0, lo=lo: e.tensor_copy(out=WC[lo:lo + 64, d, gp, 0, c0:c0 + 16], in_=ct_[lo:lo + 64, 0, d * 4 + gp, :]), reads=[bS, bprm], writes=[bS])
                TS(V, WC[lo:lo + 64, d, gp, 1, c0:c0 + 16], ct_[lo:lo + 64, 1, d * 4 + gp, :], -1.0, None, ALU.mult, None, [bS, bprm], [bS])

    iot = P.sbuf("iot", [128, NS], F32)
    P.op(G, lambda e: e.iota(iot[:], pattern=[[1, NS]], base=0, channel_multiplier=0, allow_small_or_imprecise_dtypes=True), writes=[bS], reads=[bS])

    tabS = P.sbuf("tabS", [128, NS], F32); tabC = P.sbuf("tabC", [128, NS], F32); btab = Buf("tab")
    TW = 1024 if fused else 512
    rt = P.sbuf("rt", [128, TW], F32); brt = Buf("rt")
    ub = [(P.sbuf("ub%d" % i, [128, TW], F32), Buf("ub%d" % i)) for i in range(4 if not fused else 2)]
    NUB = len(ub)
    ur = [(P.sbuf("ur%d" % i, [128, TW], F32), Buf("ur%d" % i)) for i in range(1)]

    def nat0(t0, N):
        if t0 < NCTX:
            return 0
        return NCTX + (NS - NCTX) - (t0 - NCTX) - N
    NPB = 4 if TW == 512 else 2
    psB = [(P.psum("psB%d" % i, [128, TW]), Buf("psB%d" % i)) for i in range(NPB)]
    psY = [(P.psum("psY%d" % i, [128, TW]), Buf("psY%d" % i)) for i in range(2)]
    W4 = lambda nm, n=2, dt_=F32: [(P.sbuf("%s%d" % (nm, i), [128, TW], dt_), Buf("%s%d" % (nm, i))) for i in range(n)]
    tA, tB, tC_, tD = W4("tA", 1), W4("tB", 1), W4("tC", 1), W4("tD", 1)
    NM = 2 if TW == 512 else 1
    mre, mim = W4("mre", NM), W4("mim", NM)
    gre, gim = W4("gre"), W4("gim")
    dA, dB = W4("dA", 1), W4("dB", 1)
    hre, him = W4("hre", 2, BF16), W4("him", 2, BF16)
    ysb = W4("ysb", NM)
    car = P.sbuf("car", [128, 2], F32); bcar = Buf("car")
    evs = []
    its = []
    tiles = [(0, NCTX)] + [(NCTX + i * TW, TW) for i in range((NS - NCTX) // TW)]
    for j in range(ndg):
        d, gp = j // 4, j % 4
        for b in range(nb):
            for (t0, N) in tiles:
                its.append((d, gp, b, t0, N))
    loaded = {}

    def emit_load(i):
        if i >= len(its) or i in loaded:
            return
        d, gp, b, t0, N = its[i]
        ut, but = ub[i % NUB]
        if fused:
            c0 = t0 if d == 0 else nat0(t0, N)
            P.dma("sync", ut[:, 0:N], hTc[:, c0:c0 + N], writes=[but])
        else:
            P.dma("sync", ut[:, 0:N], u[d, b, :, t0:t0 + N], writes=[but])
        loaded[i] = True

    PD = 2 if NUB >= 4 else 1
    for j0 in range(PD):
        emit_load(j0)
    cur = None
    for i, (d, gp, b, t0, N) in enumerate(its):
        emit_load(i + PD)
        j = d * 4 + gp
        if cur != j:
            cur = j
            ACT(tabS[:], iot[:], AF.Identity, [bS, btab], [btab], scale=th_s[:, j:j + 1])
            sincos_inplace(V, tabS[:], tabC[:], [btab, bS], [btab])
            P.op(V, lambda e, j=j: e.tensor_copy(out=rt[:], in_=r_s[:, j:j + 1].to_broadcast([128, TW])), reads=[bS, brt], writes=[brt])
        if t0 == 0:
            P.op(V, lambda e: e.memset(car[:], 0.0), reads=[bcar], writes=[bcar])
        s = i % 2
        sm = i % NM
        ut, but = ub[i % NUB]
        pr, bpr = psB[(2 * i) % NPB]
        pi_, bpi = psB[(2 * i + 1) % NPB]
        if fused and d == 1:
            urt, burt = ur[0]
            ACT(urt[:, 0:N], ut[:, 0:N][:, ::-1], AF.Copy, [but], [burt])
            ut, but = urt, burt
        for h0 in range(0, N, 512):
            h1 = min(N, h0 + 512)
            P.op("tensor", lambda e, pr=pr, ut=ut, h0=h0, h1=h1, d=d, gp=gp: e.matmul(pr[:, h0:h1], lhsT=WB[:, d, gp, 0, :], rhs=ut[:, h0:h1], start=True, stop=True), reads=[bS, but], writes=[bpr])
            P.op("tensor", lambda e, pi_=pi_, ut=ut, h0=h0, h1=h1, d=d, gp=gp: e.matmul(pi_[:, h0:h1], lhsT=WB[:, d, gp, 1, :], rhs=ut[:, h0:h1], start=True, stop=True), reads=[bS, but], writes=[bpi])
        Cs = tabC[:, t0:t0 + N]
        Ss = tabS[:, t0:t0 + N]
        (a_, ba), (b_, bb), (c_, bc), (d_, bd) = tA[0], tB[0], tC_[0], tD[0]
        (mr, bmr), (mi, bmi) = mre[sm], mim[sm]
        (gr, bgr), (gi, bgi) = gre[s], gim[s]
        TT(V, a_[:, 0:N], pr[:, 0:N], Cs, ALU.mult, [bpr, btab], [ba])
        TT(V, b_[:, 0:N], pi_[:, 0:N], Ss, ALU.mult, [bpi, btab], [bb])
        TT(V, mr[:, 0:N], a_[:, 0:N], b_[:, 0:N], ALU.add, [ba, bb], [bmr])
        TT(V, c_[:, 0:N], pi_[:, 0:N], Cs, ALU.mult, [bpi, btab], [bc])
        TT(V, d_[:, 0:N], pr[:, 0:N], Ss, ALU.mult, [bpr, btab], [bd])
        TT(V, mi[:, 0:N], c_[:, 0:N], d_[:, 0:N], ALU.subtract, [bc, bd], [bmi])
        P.op(V, lambda e, gr=gr, mr=mr, N=N: e.tensor_tensor_scan(out=gr[:, 0:N], data0=rt[:, 0:N], data1=mr[:, 0:N], initial=car[:, 0:1], op0=ALU.mult, op1=ALU.add),
             reads=[brt, bmr, bcar], writes=[bgr])
        P.op(V, lambda e, gi=gi, mi=mi, N=N: e.tensor_tensor_scan(out=gi[:, 0:N], data0=rt[:, 0:N], data1=mi[:, 0:N], initial=car[:, 1:2], op0=ALU.mult, op1=ALU.add),
             reads=[brt, bmi, bcar], writes=[bgi])
        ACT(car[:, 0:1], gr[:, N - 1:N], AF.Copy, [bgr, bcar], [bcar])
        ACT(car[:, 1:2], gi[:, N - 1:N], AF.Copy, [bgi, bcar], [bcar])
        if t0 < NCTX:
            continue
        (e_, be), (f_, bf) = dA[0], dB[0]
        (hr, bhr), (hi, bhi) = hre[s], him[s]
        DE = V if fused else G
        TT(DE, e_[:, 0:N], gr[:, 0:N], Cs, ALU.mult, [bgr, btab], [be])
        TT(DE, f_[:, 0:N], gi[:, 0:N], Ss, ALU.mult, [bgi, btab], [bf])
        TT(DE, hr[:, 0:N], e_[:, 0:N], f_[:, 0:N], ALU.subtract, [be, bf], [bhr])
        TT(DE, e_[:, 0:N], gi[:, 0:N], Cs, ALU.mult, [bgi, btab, be], [be])
        TT(DE, f_[:, 0:N], gr[:, 0:N], Ss, ALU.mult, [bgr, btab, bf], [bf])
        TT(DE, hi[:, 0:N], e_[:, 0:N], f_[:, 0:N], ALU.add, [be, bf], [bhi])
        py, bpy = psY[i % 2]
        for h0 in range(0, N, 512):
            h1 = min(N, h0 + 512)
            P.op("tensor", lambda e, py=py, hr=hr, h0=h0, h1=h1, d=d, gp=gp: e.matmul(py[:, h0:h1], lhsT=WC[:, d, gp, 0, :], rhs=hr[:, h0:h1], start=True, stop=False), reads=[bS, bhr], writes=[bpy], inc=False)
            P.op("tensor", lambda e, py=py, hi=hi, h0=h0, h1=h1, d=d, gp=gp: e.matmul(py[:, h0:h1], lhsT=WC[:, d, gp, 1, :], rhs=hi[:, h0:h1], start=False, stop=True), reads=[bS, bhi], writes=[bpy])
        ys, bys = ysb[sm]
        if fused and d == 1:
            ACT(ys[:, 0:N], py[:, 0:N][:, ::-1], AF.Copy, [bpy], [bys])
            c0 = nat0(t0, N) - NCTX
            evs.append(P.dma("scalar", ySc[1, gp * 32:(gp + 1) * 32, c0:c0 + N], ys[gp * 32:(gp + 1) * 32, 0:N], reads=[bys]))
        elif fused:
            ACT(ys[:, 0:N], py[:, 0:N], AF.Copy, [bpy], [bys])
            evs.append(P.dma("scalar", ySc[0, gp * 32:(gp + 1) * 32, t0 - NCTX:t0 - NCTX + N], ys[gp * 32:(gp + 1) * 32, 0:N], reads=[bys]))
        else:
            ACT(ys[:, 0:N], py[:, 0:N], AF.Copy, [bpy], [bys])
            evs.append(P.dma("scalar", y[d, b, gp * 32:(gp + 1) * 32, t0 - NCTX:t0 - NCTX + N], ys[gp * 32:(gp + 1) * 32, 0:N], reads=[bys]))
    _finish(P, evs, F)
    print("s5 program: insts", P.ninst, "sems", P.nsem)
    return nc


def s5_host_params(inp, pfx, k):
    gs = slice(8 * k, 8 * k + 8)
    a_re, a_im, ldt = inp[pfx + "ssm_a_re"][:, gs], inp[pfx + "ssm_a_im"][:, gs], inp[pfx + "ssm_log_dt"][:, gs]
    b_re, b_im = inp[pfx + "ssm_b_re"][:, gs], inp[pfx + "ssm_b_im"][:, gs]
    c_re, c_im = inp[pfx + "ssm_c_re"][:, gs], inp[pfx + "ssm_c_im"][:, gs]
    def st(a):
        return a.reshape(2, 4, 2, 64).transpose(2, 3, 0, 1).reshape(128, 8)
    ldt_s = np.broadcast_to(ldt[:, :, None], (2, 8, 64))
    prm_s = np.stack([st(a_re), st(a_im), st(ldt_s)], axis=1).astype(np.float32)
    def ft(a):
        return np.broadcast_to(a.transpose(1, 0, 2)[:, None], (8, 16, 2, 64)).reshape(128, 2, 64)
    btr = b_re.transpose(1, 3, 0, 2).reshape(128, 2, 64)
    bti = b_im.transpose(1, 3, 0, 2).reshape(128, 2, 64)
    prm_f = np.stack([ft(a_re), ft(a_im), ft(ldt_s), btr, bti], axis=1).astype(np.float32)
    def ctl(c):
        return c.reshape(2, 4, 2, 16, 64).transpose(2, 4, 0, 1, 3).reshape(128, 8, 16)
    ct = np.stack([ctl(c_re), ctl(c_im)], axis=1).astype(np.float32)
    gm = np.zeros((128, 8), np.float32)
    for g in range(8):
        gm[g * 16:(g + 1) * 16, g] = 1.0
    return {"prm_s": np.ascontiguousarray(prm_s), "prm_f": np.ascontiguousarray(prm_f), "ct": np.ascontiguousarray(ct), "gmask": gm}


def build_glu_ffn(ntiles=None, F=None):
    nc, sh, dt = _ctx(F)
    if ntiles is None:
        ntiles = NT // 512
    xT = dt("xT", [D, NT]); hlT = dt("hlT", [D, NT]); yfT = dt("yfT", [D, NT]); ybT = dt("ybT", [D, NT])
    dskT = dt("dskT", [128, 8])
    condT = dt("condT", [D, 2]); ada_w = dt("ada_w", [D, 6 * D]); ada_bT = dt("ada_bT", [128, 48])
    n1T = dt("n1T", [128, 8]); n2T = dt("n2T", [128, 8])
    w_glu = dt("w_glu", [D, 2 * D])
    w_gu = dt("w_gu", [D, 2 * DFF]); w_down = dt("w_down", [DFF, D])
    outT = dt("outT", [D, NT], "ExternalOutput")
    P = Prog(nc, sh)
    L = LB(P, nw=4)
    L.alloc_common(512)
    L.alloc_ffn()
    L.alloc_scr(4)
    L.setup_ada(condT, ada_w, ada_bT, n1T, n2T)
    xs = P.sbuf("xs", [128, 8, 512], F32); bxs = Buf("xs")
    x1 = P.sbuf("x1", [128, 8, 512], F32); bx1 = Buf("x1")
    A = P.sbuf("A", [128, 8, 512], F32); bA = Buf("A")
    B = P.sbuf("B", [128, 8, 512], F32); bB = Buf("B")
    C = P.sbuf("C", [128, 8, 512], F32); bC = Buf("C")
    ob = P.sbuf("ob", [128, 8, 512], BF16); bob = Buf("ob")
    dsk = P.sbuf("dsk", [128, 8], F32); bdsk = Buf("dsk")
    P.dma("sync", dsk[:], dskT, writes=[bdsk])
    evs = []
    V = "vector"

    def body(t0, N, c):
        mod = L.mod[c]
        ld = lambda t, bt, src: P.dma("sync", t[:, :, 0:N], src[:, t0:t0 + N].rearrange("(k p) n -> p k n", p=128), writes=[bt])
        ld(xs, bxs, xT); ld(A, bA, hlT); ld(B, bB, yfT); ld(C, bC, ybT)
        for k in range(8):
            P.op(V, lambda e, k=k: e.scalar_tensor_tensor(out=A[:, k, 0:N], in0=A[:, k, 0:N], scalar=dsk[:, k:k + 1], in1=B[:, k, 0:N], op0=ALU.mult, op1=ALU.add), reads=[bA, bB, bdsk], writes=[bA])
            P.op(V, lambda e, k=k: e.tensor_tensor(out=A[:, k, 0:N], in0=A[:, k, 0:N], in1=C[:, k, 0:N], op=ALU.add), reads=[bA, bC], writes=[bA])
            s1, bs1 = L.scr[(2 * k) % 4]
            s2, bs2 = L.scr[(2 * k + 1) % 4]
            P.op("scalar", lambda e, k=k, s1=s1: e.activation(out=s1[:, 0:N], in_=A[:, k, 0:N], func=AF.Square), reads=[bA], writes=[bs1])
            P.op(V, lambda e, s1=s1: e.tensor_scalar(out=s1[:, 0:N], in0=s1[:, 0:N], scalar1=0.044715, scalar2=1.0, op0=ALU.mult, op1=ALU.add), reads=[bs1], writes=[bs1])
            P.op(V, lambda e, k=k, s1=s1: e.tensor_tensor(out=s1[:, 0:N], in0=s1[:, 0:N], in1=A[:, k, 0:N], op=ALU.mult), reads=[bs1, bA], writes=[bs1])
            P.op("scalar", lambda e, s1=s1, s2=s2: e.activation(out=s2[:, 0:N], in_=s1[:, 0:N], func=AF.Sigmoid, scale=1.5957691216057308), reads=[bs1], writes=[bs2])
            P.op(V, lambda e, k=k, s2=s2: e.tensor_tensor(out=ob[:, k, 0:N], in0=s2[:, 0:N], in1=A[:, k, 0:N], op=ALU.mult), reads=[bs2, bA], writes=[bob])
        for q in range(2):
            wv, bwv = L.load_w(w_glu, 0, 8, q * 512, 512)
            wg, bwg = L.load_w(w_glu, 0, 8, D + q * 512, 512)
            for m in range(4):
                k3 = q * 4 + m
                ps, bps = L.next_ps()
                L.mm(ps[:, 0:N], bps, [(wv[:, k, m * 128:(m + 1) * 128], ob[:, k, 0:N], [bwv, bob]) for k in range(8)])
                ps2, bps2 = L.next_ps()
                L.mm(ps2[:, 0:N], bps2, [(wg[:, k, m * 128:(m + 1) * 128], ob[:, k, 0:N], [bwg, bob]) for k in range(8)])
                sg, bsg = L.scr[k3 % 2]
                P.op("scalar", lambda e, sg=sg, ps2=ps2: e.activation(out=sg[:, 0:N], in_=ps2[:, 0:N], func=AF.Sigmoid), reads=[bps2], writes=[bsg])
                P.op(V, lambda e, sg=sg, ps=ps: e.tensor_tensor(out=sg[:, 0:N], in0=sg[:, 0:N], in1=ps[:, 0:N], op=ALU.mult), reads=[bsg, bps], writes=[bsg])
                P.op(V, lambda e, sg=sg, k3=k3: e.scalar_tensor_tensor(out=x1[:, k3, 0:N], in0=sg[:, 0:N], scalar=mod[:, 16 + k3:17 + k3], in1=xs[:, k3, 0:N], op0=ALU.mult, op1=ALU.add),
                     reads=[bsg, bxs, L.bmod], writes=[bx1])
        L.ffn_dense(x1, bx1, N, c, w_gu, w_down, x1, bx1)
        evs.append(P.dma("sync", outT[:, t0:t0 + N].rearrange("(k p) n -> p k n", p=128), x1[:, :, 0:N], reads=[bx1]))
    for i in range(ntiles):
        body(i * 512, 512, 0)
    _finish(P, evs, F)
    print("glu_ffn program: insts", P.ninst, "sems", P.nsem)
    return nc


def run_layer2(xfull, ctxfull, inp, progs):
    pfx = "l2_"
    c, c_ctx = inp["c"], inp["c_ctx"]
    maps = []
    for core in range(8):
        b, h = core // 2, core % 2
        m = ada_maps(inp, pfx, c, c_ctx, b)
        m.update({"xT": T(xfull[b, h * NT:(h + 1) * NT]), "cT": T(ctxfull[b])})
        maps.append(m)
    r1 = run_bass_kernel_spmd(progs["hl"], maps, core_ids=list(range(8))).results
    hlT = [np.concatenate([r1[2 * b]["hlT"], r1[2 * b + 1]["hlT"]], axis=1) for b in range(4)]
    hcT = [r1[2 * b]["hcT"] for b in range(4)]
    maps = []
    for k in range(8):
        fs = slice(128 * k, 128 * k + 128)
        uf = np.stack([np.concatenate([hcT[b][fs], hlT[b][fs]], axis=1) for b in range(4)])
        ub_ = np.stack([np.concatenate([hcT[b][fs][:, ::-1], hlT[b][fs][:, ::-1]], axis=1) for b in range(4)])
        m = s5_host_params(inp, pfx, k)
        m["u"] = np.ascontiguousarray(np.stack([uf, ub_], axis=0))
        maps.append(m)
    r2 = run_bass_kernel_spmd(progs["s5"], maps, core_ids=list(range(8))).results
    yf = [np.concatenate([r2[k]["y"][0, b] for k in range(8)], axis=0) for b in range(4)]
    yb = [np.concatenate([r2[k]["y"][1, b][:, ::-1] for k in range(8)], axis=0) for b in range(4)]
    maps = []
    for core in range(8):
        b, h = core // 2, core % 2
        sl = slice(h * NT, (h + 1) * NT)
        m = ada_maps(inp, pfx, c, c_ctx, b)
        m.update({"xT": T(xfull[b, sl]), "hlT": np.ascontiguousarray(hlT[b][:, sl]), "yfT": np.ascontiguousarray(yf[b][:, sl]),
                  "ybT": np.ascontiguousarray(yb[b][:, sl]), "dskT": fm_vec(inp[pfx + "ssm_d"], 8),
                  "w_glu": inp[pfx + "ssm_w_glu"], "w_gu": inp[pfx + "ffn_w_gu"], "w_down": inp[pfx + "ffn_w_down"]})
        maps.append(m)
    r3 = run_bass_kernel_spmd(progs["glu_ffn"], maps, core_ids=list(range(8))).results
    xo = np.empty_like(xfull)
    for core in range(8):
        b, h = core // 2, core % 2
        xo[b, h * NT:(h + 1) * NT] = r3[core]["outT"].T
    return xo

NT = 8192
_FUSED = {}


def build_fused():
    global NT
    NT = 8192
    nc = bass.Bass("TRN2", target_bir_lowering=False)
    sh = Shared(nc)
    ext = {}

    def E(n, s, d=F32):
        ext[n] = nc.dram_tensor(n, list(s), d, kind="ExternalInput").ap()
        return ext[n]

    def I(n, s, d=F32):
        return nc.dram_tensor(n, list(s), d, kind="Internal").ap()
    xT = E("xT", [D, NT + 2]); cT = E("cT", [D, NCTX + 2])
    condT = E("condT", [D, 2]); mask = E("mask", [128, 4])
    ropeC = E("ropeC", [128, NT]); ropeS = E("ropeS", [128, NT])
    lay = []
    for i in range(4):
        p = "l%d_" % i
        lay.append({"ada_w": E(p + "ada_w", [D, 6 * D]), "ada_bT": E(p + "ada_bT", [128, 48]), "n1T": E(p + "n1T", [128, 8]), "n2T": E(p + "n2T", [128, 8]), "condT": condT})
    for i in (0, 3):
        p = "l%d_" % i
        lay[i].update({"conv_wT": E(p + "conv_wT", [128, 3, 8]), "w_in": E(p + "w_in", [D, 3 * D]), "w_out": E(p + "w_out", [D, D]), "mask": mask})
    for i in (0, 2):
        p = "l%d_" % i
        lay[i].update({"w_gu": E(p + "w_gu", [D, 2 * DFF]), "w_down": E(p + "w_down", [DFF, D])})
    for i in (1, 3):
        p = "l%d_" % i
        lay[i].update({"w_gu": E(p + "w_gu", [8, D, 2 * DFF]), "w_down": E(p + "w_down", [8, DFF, D]), "router": E(p + "router", [D, 8])})
    lay[1].update({"w_qkv": E("l1_w_qkv", [D, 3 * D]), "w_perm": E("l1_w_perm", [D, 2 * D]), "w_o": E("l1_w_o", [D, D]),
                   "lamb": E("l1_lamb", [128, 4, HD]), "sublnT": E("l1_sublnT", [128, 1]), "ropeC": ropeC, "ropeS": ropeS})
    prm_s = E("l2_prm_s", [8, 128, 3, 8]); prm_f = E("l2_prm_f", [8, 128, 5, 2, 64]); ctp = E("l2_ct", [8, 128, 2, 8, 16]); gmask = E("l2_gmask", [128, 8])
    lay[2].update({"dskT": E("l2_dskT", [128, 8]), "w_glu": E("l2_w_glu", [D, 2 * D])})
    fnT = E("fnT", [128, 8])
    out = nc.dram_tensor("outT", [D, NT], F32, kind="ExternalOutput").ap()
    x1T = I("x1T", [D, NT]); c1T = I("c1T", [D, NCTX])
    qT = I("qT", [D, NT], BF16); qcT = I("qcT", [D, NCTX], BF16); kall = I("kall", [D, NCTX + NT], BF16); v_tm = I("v_tm", [NCTX + NT, D], BF16)
    oT = I("oT", [D, NT], BF16); ocT = I("ocT", [D, NCTX], BF16)
    x2T = I("x2T", [D, NT]); c2T = I("c2T", [D, NCTX])
    hT = I("hT", [D, NCTX + NT]); yS = I("yS", [2, D, NT])
    x3e = I("x3e", [D, NT + 2])

    kc = [0]

    def precast(P_, keys, layer, name):
        src = lay[layer][name]
        dst = nc.dram_tensor("bf_l%d_%s" % (layer, name), list(src.shape), BF16, kind="Internal").ap()
        if len(src.shape) == 3:
            parts = [(dst[e], src[e]) for e in range(src.shape[0])]
        else:
            parts = [(dst, src)]
        for d_, s_ in parts:
            R_ = d_.shape[0]
            for r0 in range(0, R_, 256):
                r1 = min(R_, r0 + 256)
                k = keys[kc[0] % 4]
                kc[0] += 1
                P_.dma("gpsimd", d_[r0:r1], s_[r0:r1], key=k, writes=[k])
        lay[layer][name] = dst
    P = Prog(nc, sh)
    keys0 = [Buf("wk%d" % i) for i in range(4)]
    for n in ("w_in", "w_out", "w_gu", "w_down"):
        precast(P, keys0, 0, n)
    P.barrier()
    P.emit(final=False)

    def bg_precast(P_):
        keys1 = [Buf("wkb%d" % i) for i in range(4)]
        for layer, names in ((1, ("w_qkv", "w_perm", "w_o", "w_gu", "w_down")), (2, ("w_glu", "w_gu", "w_down")), (3, ("w_in", "w_out", "w_gu", "w_down"))):
            for n in names:
                precast(P_, keys1, layer, n)

    def st(io_extra, layer):
        io = dict(lay[layer])
        io.update(io_extra)
        return Fuse(nc, sh, io)
    P = Prog(nc, sh)
    z = P.sbuf("zz", [128, 8, 1], F32); bz = Buf("zz")
    P.op("vector", lambda e: e.memset(z[:], 0.0), writes=[bz])
    for ci, col in enumerate((0, NT + 1)):
        dst = x3e[:, col:col + 1].rearrange("(k p) n -> p k n", p=128)
        P.dma("sync", None, None, reads=[bz], key=Buf("zk%d" % ci), fn=lambda e, dst=dst: e.dma_start(out=dst, in_=z[:], allow_slow_non_contiguous=True))
    P.barrier()
    P.emit(final=False)
    f0 = st({"xT": xT, "cT": cT, "oT": x1T, "coT": c1T}, 0)
    build_conv_layer(moe=False, with_ctx=True, F=f0, pre_hook=bg_precast)
    build_qkv(F=st({"xT": x1T, "cT": c1T, "qT": qT, "qcT": qcT, "kall": kall, "v_tm": v_tm}, 1), fused=True)
    build_attn(0.8 - 0.6 * math.exp(-0.3 * 1), F=st({"qT": qT, "qcT": qcT, "kT": kall, "v_tm": v_tm, "oT": oT, "ocT": ocT}, 1), fused=True)
    build_oproj_ffn(moe=True, with_ctx=True, F=st({"xT": x1T, "cT": c1T, "oT": oT, "ocT": ocT, "outT": x2T, "coT": c2T}, 1))
    build_hl(F=st({"xT": x2T, "cT": c2T, "hT": hT}, 2), fused=True)
    for c in range(8):
        build_s5(F=Fuse(nc, sh, {"hTc": hT[c * 128:(c + 1) * 128, :], "ySc": yS[:, c * 128:(c + 1) * 128, :], "prm_s": prm_s[c], "prm_f": prm_f[c], "ct": ctp[c], "gmask": gmask}), fused=True)
    build_glu_ffn(F=st({"xT": x2T, "hlT": hT[:, NCTX:], "yfT": yS[0], "ybT": yS[1], "outT": x3e[:, 1:NT + 1]}, 2))
    build_conv_layer(moe=True, with_ctx=False, final_norm=True, F=st({"xT": x3e, "oT": out, "fnT": fnT}, 3))
    sh.close()
    print("fused program built; sems", sh.nsem)
    return nc


_INPUT_NAMES = (
    "x", "c", "ctx", "c_ctx",
    "l0_ada_w", "l0_ada_b", "l0_norm_mix", "l0_norm_ffn", "l0_conv_w_in", "l0_conv_w", "l0_conv_w_out", "l0_ffn_w_gu", "l0_ffn_w_down",
    "l1_ada_w", "l1_ada_b", "l1_norm_mix", "l1_norm_ffn", "l1_attn_w_qkv", "l1_attn_lam", "l1_attn_subln", "l1_attn_w_o", "l1_moe_router", "l1_moe_w_gu", "l1_moe_w_down",
    "l2_ada_w", "l2_ada_b", "l2_norm_mix", "l2_norm_ffn", "l2_ssm_a_re", "l2_ssm_a_im", "l2_ssm_log_dt", "l2_ssm_b_re", "l2_ssm_b_im", "l2_ssm_c_re", "l2_ssm_c_im", "l2_ssm_d", "l2_ssm_w_glu", "l2_ffn_w_gu", "l2_ffn_w_down",
    "l3_ada_w", "l3_ada_b", "l3_norm_mix", "l3_norm_ffn", "l3_conv_w_in", "l3_conv_w", "l3_conv_w_out", "l3_moe_router", "l3_moe_w_gu", "l3_moe_w_down",
    "final_norm",
)


def kernel(**inputs):
    inp = {k: np.ascontiguousarray(np.asarray(inputs[k])) for k in _INPUT_NAMES}
    if "nc" not in _FUSED:
        _FUSED["nc"] = build_fused()
    nc = _FUSED["nc"]
    x, ctx, c, c_ctx = inp["x"], inp["ctx"], inp["c"], inp["c_ctx"]
    perm, lo = rope_partner_perm()
    wq = inp["l1_attn_w_qkv"]
    colperm = np.concatenate([part * 1024 + h * 128 + perm for part in range(2) for h in range(8)])
    w_perm = np.ascontiguousarray(wq[:, colperm])
    C, S = rope_tables(0, NT)
    sp = [s5_host_params(inp, "l2_", k) for k in range(8)]
    shared = {
        "mask": np.ascontiguousarray(np.tile(np.array([[0.0, 0.0, 1.0, 0.0]], np.float32), (128, 1))),
        "ropeC": C, "ropeS": S,
        "l1_w_qkv": wq, "l1_w_perm": w_perm, "l1_w_o": inp["l1_attn_w_o"],
        "l1_lamb": np.ascontiguousarray(np.broadcast_to(inp["l1_attn_lam"][None], (128, 4, HD))).astype(np.float32),
        "l1_sublnT": np.ascontiguousarray(inp["l1_attn_subln"].reshape(128, 1)),
        "l2_prm_s": np.stack([p["prm_s"] for p in sp]), "l2_prm_f": np.stack([p["prm_f"] for p in sp]), "l2_ct": np.stack([p["ct"] for p in sp]), "l2_gmask": sp[0]["gmask"],
        "l2_dskT": fm_vec(inp["l2_ssm_d"], 8), "l2_w_glu": inp["l2_ssm_w_glu"],
        "fnT": fm_vec(inp["final_norm"], 8),
    }
    for i in range(4):
        p = "l%d_" % i
        shared.update({p + "ada_w": inp[p + "ada_w"], p + "ada_bT": fm_vec(inp[p + "ada_b"], 48), p + "n1T": fm_vec(inp[p + "norm_mix"], 8), p + "n2T": fm_vec(inp[p + "norm_ffn"], 8)})
    for i in (0, 3):
        p = "l%d_" % i
        shared.update({p + "conv_wT": np.ascontiguousarray(inp[p + "conv_w"].reshape(3, 8, 128).transpose(2, 0, 1)), p + "w_in": inp[p + "conv_w_in"], p + "w_out": inp[p + "conv_w_out"]})
    for i in (0, 2):
        p = "l%d_" % i
        shared.update({p + "w_gu": inp[p + "ffn_w_gu"], p + "w_down": inp[p + "ffn_w_down"]})
    for i in (1, 3):
        p = "l%d_" % i
        shared.update({p + "w_gu": inp[p + "moe_w_gu"], p + "w_down": inp[p + "moe_w_down"], p + "router": inp[p + "moe_router"]})
    active = [0, 1, 4, 5]
    big = [k for k, v in shared.items() if v.size >= (1 << 16)]
    idle = dict(shared)
    for k in big:
        idle[k] = np.zeros_like(shared[k])
    idle.update({"xT": np.zeros((D, NT + 2), np.float32), "cT": np.zeros((D, NCTX + 2), np.float32), "condT": np.zeros((D, 2), np.float32)})
    maps = []
    for core in range(8):
        if core not in active:
            maps.append(idle)
            continue
        b = active.index(core)
        xe = np.zeros((NT + 2, D), np.float32)
        xe[1:NT + 1] = x[b]
        ce = np.zeros((NCTX + 2, D), np.float32)
        ce[1:NCTX + 1] = ctx[b]
        m = dict(shared)
        m.update({"xT": np.ascontiguousarray(xe.T), "cT": np.ascontiguousarray(ce.T), "condT": np.ascontiguousarray(np.stack([c[b], c_ctx], axis=1))})
        maps.append(m)
    r = run_bass_kernel_spmd(nc, maps, core_ids=list(range(8))).results
    out = np.empty_like(x)
    for b in range(4):
        out[b] = r[active[b]]["outT"].T
    return out
```
